# Optimizing a Trainium2 kernel written in Bass

```python
import jax
import jax.numpy as jnp
from jax import lax
import numpy as np

D_MODEL = 1024
BATCH = 8
SEQ = 4096
DEPTH = 4

CTX_LEN = 256
GRID_W = 64
N_MIXERS = 3
Q_BLOCK = 128
ROPE_THETA = 10000.0
NORM_EPS = 1e-6
NEG_INF = -1e30

A_HEADS = 8
A_KV_HEADS = 2
A_HEAD_DIM = D_MODEL // A_HEADS
B_HEADS = 16
B_KV_HEADS = 4
B_HEAD_DIM = D_MODEL // B_HEADS
WINDOW = 128
C_HEAD_DIM = 64
C_HEADS = D_MODEL // C_HEAD_DIM
C_DECAY_LORA = 64
C_AAA_LORA = 64
C_GATE_LORA = 128
C_GN_EPS = C_HEAD_DIM * 1e-5
N_EXPERTS = 16
EXPERT_FF = 2 * D_MODEL
CAPACITY_FACTOR = 2

N_A = (DEPTH + 2) // N_MIXERS
N_B = (DEPTH + 1) // N_MIXERS
N_C = DEPTH // N_MIXERS

kernel_name = 'hybrid_diffusion_trunk_gqa_swa_rwkv7_ecmoe'


def rms_norm(x):
    x32 = x.astype(jnp.float32)
    return (x32 * lax.rsqrt(jnp.mean(x32 * x32, axis=-1, keepdims=True) + NORM_EPS)).astype(x.dtype)


def modulate(x, shift, scale):
    return rms_norm(x) * (1 + scale) + shift


def grid_positions(n_tokens):
    n_rows = n_tokens // GRID_W
    rows = jnp.repeat(jnp.arange(n_rows, dtype=jnp.int32), GRID_W)
    cols = jnp.tile(jnp.arange(GRID_W, dtype=jnp.int32), n_rows)
    return rows, cols


def axial_rope_tables(rows, cols, head_dim):
    n_freq = head_dim // 4
    inv_freq = ROPE_THETA ** (-jnp.arange(n_freq, dtype=jnp.float32) / n_freq)
    ang = jnp.concatenate([rows.astype(jnp.float32)[:, None] * inv_freq,
                           cols.astype(jnp.float32)[:, None] * inv_freq], axis=-1)
    return jnp.cos(ang), jnp.sin(ang)


def apply_rope(x, cos, sin):
    bshape = (1, cos.shape[0]) + (1,) * (x.ndim - 3) + (cos.shape[1],)
    cos, sin = cos.reshape(bshape), sin.reshape(bshape)
    x1, x2 = jnp.split(x.astype(jnp.float32), 2, axis=-1)
    return jnp.concatenate([x1 * cos - x2 * sin, x1 * sin + x2 * cos], axis=-1).astype(x.dtype)


def project_qkv(h, w_qkv, n_heads, n_kv, head_dim):
    b, t, _ = h.shape
    q, k, v = jnp.split(h @ w_qkv, [n_heads * head_dim, (n_heads + n_kv) * head_dim], axis=-1)
    return (q.reshape(b, t, n_kv, n_heads // n_kv, head_dim),
            k.reshape(b, t, n_kv, head_dim), v.reshape(b, t, n_kv, head_dim))


def dense_attend(q, k, v, scale):
    s = jnp.einsum('bqhgd,bkhd->bhgqk', q, k).astype(jnp.float32) * scale
    p = jax.nn.softmax(s, axis=-1).astype(v.dtype)
    return jnp.einsum('bhgqk,bkhd->bqhgd', p, v)


def global_gqa(hc, hl, w_qkv, w_o, q_gain, k_gain, cos, sin, last):
    b, n_lat, _ = hl.shape
    scale = A_HEAD_DIM ** -0.5
    qc, kc, vc = project_qkv(hc, w_qkv, A_HEADS, A_KV_HEADS, A_HEAD_DIM)
    ql, kl, vl = project_qkv(hl, w_qkv, A_HEADS, A_KV_HEADS, A_HEAD_DIM)
    qc, ql = rms_norm(qc) * q_gain, rms_norm(ql) * q_gain
    kc, kl = rms_norm(kc) * k_gain, rms_norm(kl) * k_gain
    ql, kl = apply_rope(ql, cos, sin), apply_rope(kl, cos, sin)
    k_all = jnp.concatenate([kl, kc], axis=1)
    v_all = jnp.concatenate([vl, vc], axis=1)
    n_blk = n_lat // Q_BLOCK
    q_blocks = jnp.moveaxis(ql.reshape(b, n_blk, Q_BLOCK, *ql.shape[2:]), 1, 0)
    o = lax.map(lambda q: dense_attend(q, k_all, v_all, scale), q_blocks)
    yl = jnp.moveaxis(o, 0, 1).reshape(b, n_lat, D_MODEL) @ w_o
    if last:
        return None, yl
    yc = dense_attend(qc, kc, vc, scale).reshape(hc.shape) @ w_o
    return yc, yl


def sink_softmax(scores, sink):
    col = jnp.broadcast_to(sink.astype(jnp.float32)[:, :, None, None], scores[0].shape[:-1] + (1,))
    p = jax.nn.softmax(jnp.concatenate(scores + [col], axis=-1), axis=-1)
    return p[..., :-1]


def window_gqa_sink(hc, hl, w_qkv, w_o, sink, cos, sin, last):
    b, n_lat, _ = hl.shape
    scale = B_HEAD_DIM ** -0.5
    sink = sink.reshape(B_KV_HEADS, B_HEADS // B_KV_HEADS)
    qc, kc, vc = project_qkv(hc, w_qkv, B_HEADS, B_KV_HEADS, B_HEAD_DIM)
    ql, kl, vl = project_qkv(hl, w_qkv, B_HEADS, B_KV_HEADS, B_HEAD_DIM)
    ql, kl = apply_rope(ql, cos, sin), apply_rope(kl, cos, sin)
    pad = ((0, 0), (Q_BLOCK, Q_BLOCK), (0, 0), (0, 0))
    kp, vp = jnp.pad(kl, pad), jnp.pad(vl, pad)
    n_blk = n_lat // Q_BLOCK
    q_blocks = jnp.moveaxis(ql.reshape(b, n_blk, Q_BLOCK, *ql.shape[2:]), 1, 0)

    def block(args):
        blk, q = args
        start = blk * Q_BLOCK
        kw = lax.dynamic_slice_in_dim(kp, start, 3 * Q_BLOCK, axis=1)
        vw = lax.dynamic_slice_in_dim(vp, start, 3 * Q_BLOCK, axis=1)
        q_pos = start + jnp.arange(Q_BLOCK)
        k_pos = start - Q_BLOCK + jnp.arange(3 * Q_BLOCK)
        band = ((jnp.abs(k_pos[None, :] - q_pos[:, None]) <= WINDOW)
                & (k_pos >= 0)[None, :] & (k_pos < n_lat)[None, :])
        s_win = jnp.where(band, jnp.einsum('bqhgd,bkhd->bhgqk', q, kw).astype(jnp.float32) * scale, NEG_INF)
        s_ctx = jnp.einsum('bqhgd,bkhd->bhgqk', q, kc).astype(jnp.float32) * scale
        p = sink_softmax([s_win, s_ctx], sink).astype(vw.dtype)
        return (jnp.einsum('bhgqk,bkhd->bqhgd', p[..., :3 * Q_BLOCK], vw)
                + jnp.einsum('bhgqk,bkhd->bqhgd', p[..., 3 * Q_BLOCK:], vc))

    o = lax.map(block, (jnp.arange(n_blk), q_blocks))
    yl = jnp.moveaxis(o, 0, 1).reshape(b, n_lat, D_MODEL) @ w_o
    if last:
        return None, yl
    s = jnp.einsum('bqhgd,bkhd->bhgqk', qc, kc).astype(jnp.float32) * scale
    p = sink_softmax([s], sink).astype(vc.dtype)
    yc = jnp.einsum('bhgqk,bkhd->bqhgd', p, vc).reshape(hc.shape) @ w_o
    return yc, yl


def centred_shift(x):
    xp = jnp.pad(x, ((0, 0), (1, 1), (0, 0)))
    return 0.5 * (xp[:, :-2] + xp[:, 2:]) - x


def rwkv_features(h, mu, w_rkv, w0, w1, w2, a0, a1, a2, g1, g2, k_k, k_a):
    f32 = jnp.float32
    heads = lambda t: t.reshape(*t.shape[:-1], C_HEADS, C_HEAD_DIM)
    xx = centred_shift(h)
    xr, xw, xk, xv, xa, xg = h[None] + xx[None] * mu[:, None, None, :]
    r, k, v = jnp.einsum('jbtd,jde->jbte', jnp.stack([xr, xk, xv]), w_rkv)
    w_lora = jnp.einsum('jbtr,jrd->jbtd', jnp.tanh(jnp.einsum('btd,jdr->jbtr', xw, w1)), w2)
    log_w = -jax.nn.softplus(-(w0[:, None, None, :] + w_lora).astype(f32)) - 0.5
    decay = jnp.exp(-jnp.exp(log_w))
    a = jax.nn.sigmoid((a0[:, None, None, :]
                        + jnp.einsum('jbtr,jrd->jbtd', jnp.einsum('btd,jdr->jbtr', xa, a1), a2)).astype(f32))
    g = jax.nn.sigmoid(xg @ g1) @ g2
    kk = heads((k * k_k).astype(f32))
    kk = kk * lax.rsqrt(jnp.maximum(jnp.sum(kk * kk, axis=-1, keepdims=True), 1e-24))
    k_dir = heads(k.astype(f32)[None] * (1 + (a - 1) * k_a.astype(f32)))
    return heads(r.astype(f32)), heads(decay), k_dir, heads(v.astype(f32)), -kk, kk[None] * heads(a), g


def wkv_scan(r, w, k, v, a, b, state0, reverse):
    def step(s, inp):
        r_t, w_t, k_t, v_t, a_t, b_t = inp
        sa = jnp.einsum('bhvk,bhk->bhv', s, a_t)
        s = s * w_t[:, :, None, :] + sa[..., None] * b_t[:, :, None, :] + v_t[..., None] * k_t[:, :, None, :]
        return s, jnp.einsum('bhvk,bhk->bhv', s, r_t)
    xs = tuple(jnp.swapaxes(t, 0, 1) for t in (r, w, k, v, a, b))
    s_final, o = lax.scan(step, state0, xs, reverse=reverse)
    return s_final, jnp.swapaxes(o, 0, 1)


def rwkv7_bidir(hc, hl, mu, w_rkv, w_o, w0, w1, w2, a0, a1, a2, g1, g2, k_k, k_a, r_k, ln_w, ln_b, last):
    fc = rwkv_features(hc, mu, w_rkv, w0, w1, w2, a0, a1, a2, g1, g2, k_k, k_a)
    fl = rwkv_features(hl, mu, w_rkv, w0, w1, w2, a0, a1, a2, g1, g2, k_k, k_a)
    zero = jnp.zeros((hl.shape[0], C_HEADS, C_HEAD_DIM, C_HEAD_DIM), jnp.float32)

    def scan_dir(f, d, state0):
        r, w, k, v, a_vec, b_vec, _ = f
        return wkv_scan(r, w[d], k[d], v, a_vec, b_vec[d], state0, reverse=(d == 1))

    s_fc, o_fc = scan_dir(fc, 0, zero)
    s_bc, o_bc = scan_dir(fc, 1, zero)
    _, o_fl = scan_dir(fl, 0, s_fc)
    _, o_bl = scan_dir(fl, 1, s_bc)

    def readout(f, o_f, o_b, dtype):
        r, _, k, v, _, _, g = f
        o = o_f + o_b
        o = o - jnp.mean(o, axis=-1, keepdims=True)
        o = o * lax.rsqrt(jnp.mean(o * o, axis=-1, keepdims=True) + C_GN_EPS)
        o = o.reshape(*o.shape[:2], D_MODEL) * ln_w + ln_b
        bonus = jnp.sum(jnp.sum(r[None] * k * r_k[:, None, None], axis=-1, keepdims=True), axis=0) * v
        return ((o + bonus.reshape(o.shape)) * g).astype(dtype) @ w_o

    yl = readout(fl, o_fl, o_bl, hl.dtype)
    if last:
        return None, yl
    return readout(fc, o_fc, o_bc, hc.dtype), yl


def expert_choice_ffn(h, router_w, w1, w3, w2):
    b, t, d = h.shape
    cap = CAPACITY_FACTOR * t // N_EXPERTS
    aff = jax.nn.softmax((h @ router_w).astype(jnp.float32), axis=-1)
    gate, idx = lax.top_k(jnp.swapaxes(aff, 1, 2), cap)
    xs = jax.vmap(lambda hb, ib: hb[ib])(h, idx)
    hid = jax.nn.silu(jnp.einsum('becd,edf->becf', xs, w1)) * jnp.einsum('becd,edf->becf', xs, w3)
    out = jnp.einsum('becf,efd->becd', hid, w2) * gate[..., None].astype(h.dtype)
    return jax.vmap(lambda ib, ob: jnp.zeros((t, d), h.dtype).at[ib.reshape(-1)].add(ob.reshape(-1, d)))(idx, out)


def setup_inputs(seed: int = 0) -> dict:
    key = jax.random.key(seed)
    ks = iter(jax.random.split(key, 48))
    f32 = jnp.float32
    D = D_MODEL

    def nrm(shape, scale):
        return jax.random.normal(next(ks), shape, f32) * scale

    qkv_a = (A_HEADS + 2 * A_KV_HEADS) * A_HEAD_DIM
    qkv_b = (B_HEADS + 2 * B_KV_HEADS) * B_HEAD_DIM
    return {
        'x': nrm((BATCH, SEQ, D), 1.0),
        'c': nrm((BATCH, D), 1.0),
        'ctx': nrm((BATCH, CTX_LEN, D), 1.0),
        'c_ctx': nrm((D,), 1.0),
        'mod_w': nrm((DEPTH, D, 6 * D), 0.5 * D ** -0.5),
        'mod_b': nrm((DEPTH, 6 * D), 0.02),
        'a_w_qkv': nrm((N_A, D, qkv_a), D ** -0.5),
        'a_w_o': nrm((N_A, D, D), D ** -0.5),
        'a_q_norm': 1.0 + nrm((N_A, A_HEAD_DIM), 0.02),
        'a_k_norm': 1.0 + nrm((N_A, A_HEAD_DIM), 0.02),
        'b_w_qkv': nrm((N_B, D, qkv_b), D ** -0.5),
        'b_w_o': nrm((N_B, D, D), D ** -0.5),
        'b_sink': nrm((N_B, B_HEADS), 0.5),
        'c_mu': jax.random.uniform(next(ks), (N_C, 6, D), f32),
        'c_w_rkv': nrm((N_C, 3, D, D), D ** -0.5),
        'c_w_o': nrm((N_C, D, D), D ** -0.5),
        'c_w0': jax.random.uniform(next(ks), (N_C, 2, D), f32, minval=-6.0, maxval=0.0),
        'c_w1': nrm((N_C, 2, D, C_DECAY_LORA), 0.1 * D ** -0.5),
        'c_w2': nrm((N_C, 2, C_DECAY_LORA, D), 0.1 * C_DECAY_LORA ** -0.5),
        'c_a0': nrm((N_C, 2, D), 0.1),
        'c_a1': nrm((N_C, 2, D, C_AAA_LORA), 0.1 * D ** -0.5),
        'c_a2': nrm((N_C, 2, C_AAA_LORA, D), 0.1 * C_AAA_LORA ** -0.5),
        'c_g1': nrm((N_C, D, C_GATE_LORA), D ** -0.5),
        'c_g2': nrm((N_C, C_GATE_LORA, D), C_GATE_LORA ** -0.5),
        'c_k_k': 0.85 + nrm((N_C, D), 0.02),
        'c_k_a': 1.0 + nrm((N_C, D), 0.02),
        'c_r_k': nrm((N_C, 2, C_HEADS, C_HEAD_DIM), 0.1),
        'c_ln_w': 1.0 + nrm((N_C, D), 0.02),
        'c_ln_b': nrm((N_C, D), 0.02),
        'router_w': nrm((DEPTH, D, N_EXPERTS), D ** -0.5),
        'ffn_w1': nrm((DEPTH, N_EXPERTS, D, EXPERT_FF), D ** -0.5),
        'ffn_w3': nrm((DEPTH, N_EXPERTS, D, EXPERT_FF), D ** -0.5),
        'ffn_w2': nrm((DEPTH, N_EXPERTS, EXPERT_FF, D), EXPERT_FF ** -0.5),
        'final_norm': 1.0 + nrm((D,), 0.02),
    }


def reference(x, c, ctx, c_ctx, mod_w, mod_b, a_w_qkv, a_w_o, a_q_norm, a_k_norm, b_w_qkv, b_w_o, b_sink,
              c_mu, c_w_rkv, c_w_o, c_w0, c_w1, c_w2, c_a0, c_a1, c_a2, c_g1, c_g2, c_k_k, c_k_a, c_r_k,
              c_ln_w, c_ln_b, router_w, ffn_w1, ffn_w3, ffn_w2, final_norm):
    n_lat = x.shape[1]
    rows, cols = grid_positions(n_lat)
    cos_a, sin_a = axial_rope_tables(rows, cols, A_HEAD_DIM)
    cos_b, sin_b = axial_rope_tables(rows, cols, B_HEAD_DIM)
    cond_l = jax.nn.silu(c)
    cond_c = jax.nn.silu(c_ctx)
    xl, xc = x, ctx
    for i in range(DEPTH):
        last = i == DEPTH - 1
        kind, j = i % N_MIXERS, i // N_MIXERS
        mod_l = (cond_l @ mod_w[i] + mod_b[i]).reshape(-1, 6, 1, D_MODEL)
        mod_c = (cond_c @ mod_w[i] + mod_b[i]).reshape(6, D_MODEL)
        hl = modulate(xl, mod_l[:, 0], mod_l[:, 1])
        hc = modulate(xc, mod_c[0], mod_c[1])
        if kind == 0:
            yc, yl = global_gqa(hc, hl, a_w_qkv[j], a_w_o[j], a_q_norm[j], a_k_norm[j], cos_a, sin_a, last)
        elif kind == 1:
            yc, yl = window_gqa_sink(hc, hl, b_w_qkv[j], b_w_o[j], b_sink[j], cos_b, sin_b, last)
        else:
            yc, yl = rwkv7_bidir(hc, hl, c_mu[j], c_w_rkv[j], c_w_o[j], c_w0[j], c_w1[j], c_w2[j],
                                 c_a0[j], c_a1[j], c_a2[j], c_g1[j], c_g2[j], c_k_k[j], c_k_a[j],
                                 c_r_k[j], c_ln_w[j], c_ln_b[j], last)
        xl = xl + mod_l[:, 2] * yl
        hl = modulate(xl, mod_l[:, 3], mod_l[:, 4])
        xl = xl + mod_l[:, 5] * expert_choice_ffn(hl, router_w[i], ffn_w1[i], ffn_w3[i], ffn_w2[i])
        if not last:
            xc = xc + mod_c[2] * yc
            hc = modulate(xc, mod_c[3], mod_c[4])
            xc = xc + mod_c[5] * expert_choice_ffn(hc, router_w[i], ffn_w1[i], ffn_w3[i], ffn_w2[i])
    return rms_norm(xl) * final_norm
```

```python
import contextlib
import numpy as np
import concourse.bass as bass
import concourse.mybir as mybir
from concourse.bass_utils import run_bass_kernel_spmd

F32 = mybir.dt.float32
U32 = mybir.dt.uint32
I32 = mybir.dt.int32
ALU = mybir.AluOpType
AF = mybir.ActivationFunctionType
AX = mybir.AxisListType

D = 1024
NE = 16
EPS = 1e-6
CFG = dict(S=4096, C=256, FF=2048, kinds=[0, 1, 2, 0], ncores=8)


class Buf:
    __slots__ = ("t", "w", "r", "pr", "name")

    def __init__(self, t, name):
        self.t = t
        self.name = name
        self.w = []
        self.r = []
        self.pr = []

    def __getitem__(self, k):
        return self.t[k]


class KB:
    SEM_LIMIT = 30000
    N_DMA_SLOTS = 48

    def __init__(self):
        self.nc = bass.Bass("TRN2", target_bir_lowering=False)
        nc = self.nc
        self.es = contextlib.ExitStack()
        self.engs = {"pe": nc.tensor, "dve": nc.vector, "act": nc.scalar, "pool": nc.gpsimd, "sp": nc.sync}
        self.sem = {}
        self.cnt = {}
        self.nsem = 0
        for e in self.engs:
            self._new_sem(e)
        self.seen = {e: {} for e in self.engs}
        self.slots = []
        for i in range(self.N_DMA_SLOTS):
            s = self.es.enter_context(nc.semaphore(f"dq{i}"))
            self.slots.append([s, 0])
        self.slot_i = 0
        self.uid = 0
        self.ninst = 0

    def _new_sem(self, e):
        self.nsem += 1
        self.sem[e] = self.es.enter_context(self.nc.semaphore(f"s_{e}_{self.nsem}"))
        self.cnt[e] = 0

    def inp(self, name, shape, dtype=F32):
        return Buf(self.nc.dram_tensor(name, list(shape), dtype, kind="ExternalInput").ap(), name)

    def outp(self, name, shape, dtype=F32):
        return Buf(self.nc.dram_tensor(name, list(shape), dtype, kind="ExternalOutput").ap(), name)

    def dram(self, name, shape, dtype=F32):
        return Buf(self.nc.dram_tensor(name, list(shape), dtype, kind="Internal").ap(), name)

    def sb(self, stack, name, shape, dtype=F32):
        self.uid += 1
        t = stack.enter_context(self.nc.sbuf_tensor(f"{name}_{self.uid}", list(shape), dtype))
        return Buf(t, name)

    def ps(self, stack, name, shape, dtype=F32):
        self.uid += 1
        t = stack.enter_context(self.nc.psum_tensor(f"{name}_{self.uid}", list(shape), dtype))
        return Buf(t, name)

    def _wait(self, eng, sem, val):
        d = self.seen[eng]
        k = id(sem)
        if d.get(k, (None, 0))[1] >= val:
            return
        self.engs[eng].wait_ge(sem, val)
        d[k] = (sem, val)
        self.ninst += 1

    @staticmethod
    def _add(lst, ev):
        out = [x for x in lst if not (x[0] is ev[0] and x[1] <= ev[1])]
        out.append(ev)
        return out

    def op(self, eng, fn, R=(), W=(), P=(), dma=False):
        for b in R:
            for ev in b.w:
                self._wait(eng, ev[0], ev[1])
        for b in W:
            for ev in b.w + b.r + b.pr:
                self._wait(eng, ev[0], ev[1])
        for b in P:
            for ev in b.r + b.pr:
                self._wait(eng, ev[0], ev[1])
            for ev in b.w:
                if ev[2]:
                    self._wait(eng, ev[0], ev[1])
        if dma:
            slot = self.slots[self.slot_i]
            self.slot_i = (self.slot_i + 1) % len(self.slots)
            if slot[1] > 0:
                self._wait(eng, slot[0], slot[1])
            inst = fn(self.engs[eng])
            slot[1] += 16
            inst.then_inc(slot[0], 16)
            sem, val = slot[0], slot[1]
        else:
            if self.cnt[eng] >= self.SEM_LIMIT:
                self._new_sem(eng)
            inst = fn(self.engs[eng])
            self.cnt[eng] += 1
            sem, val = self.sem[eng], self.cnt[eng]
            inst.then_inc(sem, 1)
        self.ninst += 1
        for b in R:
            b.r = self._add(b.r, (sem, val))
        for b in W:
            b.w = [(sem, val, True)]
            b.r = []
            b.pr = []
        for b in P:
            if b.r:
                b.pr = b.r
                b.r = []
                b.w = []
            b.w = self._add(b.w, (sem, val, False))
        return (sem, val)

    def dma(self, eng, out_b, out_ap, in_b, in_ap, part=True, **kw):
        def f(e):
            return e.dma_start(out=out_ap, in_=in_ap, **kw)
        if part:
            return self.op(eng, f, R=[in_b], P=[out_b], dma=True)
        return self.op(eng, f, R=[in_b], W=[out_b], dma=True)

    def barrier(self):
        for e in self.engs:
            for e2 in self.engs:
                if e2 != e and self.cnt[e2] > 0:
                    self._wait(e, self.sem[e2], self.cnt[e2])
            for slot in self.slots:
                if slot[1] > 0:
                    self._wait(e, slot[0], slot[1])

    def finish(self, bufs):
        for b in bufs:
            for ev in b.w:
                self._wait("sp", ev[0], ev[1])
        self.es.close()
        return self.nc


def bc_last(ap, n):
    return ap.unsqueeze(2).to_broadcast([ap.shape[0], ap.shape[1], n])


def bc_mid(ap, n):
    return ap.unsqueeze(1).to_broadcast([ap.shape[0], n, ap.shape[1]])


class Prog:
    def __init__(self, cfg):
        self.cfg = cfg
        self.S, self.C, self.FF = cfg["S"], cfg["C"], cfg["FF"]
        self.T = self.S + self.C
        self.kinds = cfg["kinds"]
        self.L = len(self.kinds)
        self.NT = self.T // 128
        self.NTL = self.S // 128
        self.kb = KB()

    def rstd(self, st, ss, n, width, tmp):
        kb = self.kb
        kb.op("dve", lambda e: e.tensor_scalar(tmp[:, 0:n], ss[:, 0:n], 1.0 / width, EPS, op0=ALU.mult, op1=ALU.add),
              R=[ss], W=[tmp])
        kb.op("act", lambda e: e.activation(tmp[:, 0:n], tmp[:, 0:n], AF.Sqrt), R=[tmp], W=[tmp])
        kb.op("dve", lambda e: e.reciprocal(tmp[:, 0:n], tmp[:, 0:n]), R=[tmp], W=[tmp])

    def load_bc(self, dst, src_b, row_ap, plus_one=False):
        kb = self.kb
        kb.dma("sp", dst, dst[:], src_b, row_ap.partition_broadcast(128), part=False)
        if plus_one:
            kb.op("pool", lambda e: e.tensor_scalar(dst[:], dst[:], 1.0, None, op0=ALU.add), R=[dst], W=[dst])

    def declare(self):
        kb, S, C, T, L, FF = self.kb, self.S, self.C, self.T, self.L, self.FF
        nA, nB, nC = max(1, self.kinds.count(0)), max(1, self.kinds.count(1)), max(1, self.kinds.count(2))
        i = {}
        i["x"] = kb.inp("x", [S, D])
        i["c"] = kb.inp("c", [1, D])
        i["ctx"] = kb.inp("ctx", [C, D])
        i["c_ctx"] = kb.inp("c_ctx", [1, D])
        i["mod_w"] = kb.inp("mod_w", [L, D, 6 * D])
        i["mod_b"] = kb.inp("mod_b", [L, 6 * D])
        i["a_w_qkv"] = kb.inp("a_w_qkv", [nA, D, 1536])
        i["a_w_o"] = kb.inp("a_w_o", [nA, D, D])
        i["a_q_norm"] = kb.inp("a_q_norm", [nA, 128])
        i["a_k_norm"] = kb.inp("a_k_norm", [nA, 128])
        i["b_w_qkv"] = kb.inp("b_w_qkv", [nB, D, 1536])
        i["b_w_o"] = kb.inp("b_w_o", [nB, D, D])
        i["b_sink"] = kb.inp("b_sink", [nB, 16])
        i["c_mu"] = kb.inp("c_mu", [nC, 6, D])
        i["c_w_rkv"] = kb.inp("c_w_rkv", [nC, 3, D, D])
        i["c_w_o"] = kb.inp("c_w_o", [nC, D, D])
        i["c_w0"] = kb.inp("c_w0", [nC, 2, D])
        i["c_w1"] = kb.inp("c_w1", [nC, 2, D, 64])
        i["c_w2"] = kb.inp("c_w2", [nC, 2, 64, D])
        i["c_a0"] = kb.inp("c_a0", [nC, 2, D])
        i["c_a1"] = kb.inp("c_a1", [nC, 2, D, 64])
        i["c_a2"] = kb.inp("c_a2", [nC, 2, 64, D])
        i["c_g1"] = kb.inp("c_g1", [nC, D, 128])
        i["c_g2"] = kb.inp("c_g2", [nC, 128, D])
        i["c_k_k"] = kb.inp("c_k_k", [nC, D])
        i["c_k_a"] = kb.inp("c_k_a", [nC, D])
        i["c_r_k"] = kb.inp("c_r_k", [nC, 2, D])
        i["c_ln_w"] = kb.inp("c_ln_w", [nC, D])
        i["c_ln_b"] = kb.inp("c_ln_b", [nC, D])
        i["router_w"] = kb.inp("router_w", [L, D, NE])
        i["ffn_w1"] = kb.inp("ffn_w1", [L, NE, D, FF])
        i["ffn_w3"] = kb.inp("ffn_w3", [L, NE, D, FF])
        i["ffn_w2"] = kb.inp("ffn_w2", [L, NE, FF, D])
        i["final_norm"] = kb.inp("final_norm", [1, D])
        i["k_ident"] = kb.inp("k_ident", [128, 128])
        i["k_ropeA"] = kb.inp("k_ropeA", [S, 128])
        i["k_ropeB"] = kb.inp("k_ropeB", [S, 64])
        i["k_tri"] = kb.inp("k_tri", [2, 128, 128])
        i["k_bd"] = kb.inp("k_bd", [48, 512])
        i["k_sel"] = kb.inp("k_sel", [128, 64, 16])
        i["k_pad"] = kb.inp("k_pad", [128, 1])
        i["k_zero"] = kb.inp("k_zero", [128, D])
        self.i = i
        self.out = kb.outp("out", [S, D])
        d = {}
        d["X"] = kb.dram("X", [T + 128, D])
        d["H"] = kb.dram("H", [T + 128, D])
        d["MODV"] = kb.dram("MODV", [L, 2, 6 * D])
        d["QT"] = kb.dram("QT", [20, 128, T])
        d["V"] = kb.dram("V", [T, 256])
        d["O"] = kb.dram("O", [T, D])
        if 2 in self.kinds:
            d["XJ"] = kb.dram("XJ", [6, T, D])
            for nm in ("RR", "KK", "VV", "GG", "AV"):
                d[nm] = kb.dram(nm, [T, D])
            for nm in ("DEC", "AA", "BB", "KD", "RP"):
                d[nm] = kb.dram(nm, [2, T, D])
            d["SC"] = kb.dram("SC", [T, 80])
            self.QSB = 1024
            d["QS"] = [[kb.dram(f"QS{a}_{b}", [min(2048, T - b * 2048), 32 * 512]) for b in range((T + 2047) // 2048)] for a in range(2)]
        self.d = d

    def phase_init(self):
        kb, i, d, S, C, T = self.kb, self.i, self.d, self.S, self.C, self.T
        with contextlib.ExitStack() as st:
            kb.barrier()
            self.ident = kb.sb(self.gst, "ident", [128, 128])
            kb.dma("sp", self.ident, self.ident[:], i["k_ident"], i["k_ident"][:], part=False)
            tl = [kb.sb(st, f"cp{j}", [128, D]) for j in range(2)]
            for t in range(self.NT):
                b = tl[t % 2]
                src = i["x"][t * 128:(t + 1) * 128, :] if t < self.NTL else i["ctx"][(t - self.NTL) * 128:(t - self.NTL + 1) * 128, :]
                kb.dma("sp", b, b[:], i["x"] if t < self.NTL else i["ctx"], src, part=False)
                kb.dma("sp", d["X"], d["X"][t * 128:(t + 1) * 128, :], b, b[:])
            z = kb.sb(st, "z", [128, D])
            kb.dma("sp", z, z[:], i["k_zero"], i["k_zero"][:], part=False)
            kb.dma("sp", d["X"], d["X"][T:T + 128, :], z, z[:])
            kb.dma("sp", d["H"], d["H"][T:T + 128, :], z, z[:])
            cT = kb.sb(st, "cT", [128, 8, 2])
            kb.dma("sp", cT, cT[:, :, 0], i["c"], i["c"][0, :].rearrange("(k p) -> p k", p=128), allow_slow_non_contiguous=True)
            kb.dma("sp", cT, cT[:, :, 1], i["c_ctx"], i["c_ctx"][0, :].rearrange("(k p) -> p k", p=128), allow_slow_non_contiguous=True)
            kb.op("act", lambda e: e.activation(cT[:], cT[:], AF.Silu), R=[cT], W=[cT])
            wts = [kb.sb(st, f"mw{j}", [128, 8, 512]) for j in range(2)]
            mb = kb.sb(st, "mb", [2, 6 * D])
            mv = kb.sb(st, "mv", [2, 6 * D])
            pm = [kb.ps(st, f"pm{j}", [2, 512]) for j in range(2)]
            n = 0
            for l in range(self.L):
                kb.dma("sp", mb, mb[:], i["mod_b"], i["mod_b"][l, :].partition_broadcast(2), part=False)
                for nb in range(12):
                    wt = wts[n % 2]
                    p = pm[n % 2]
                    n += 1
                    kb.dma("sp", wt, wt[:], i["mod_w"], i["mod_w"][l, :, nb * 512:(nb + 1) * 512].rearrange("(k p) n -> p k n", p=128), part=False)
                    for k in range(8):
                        kb.op("pe", lambda e: e.matmul(p[:], cT[:, k, :], wt[:, k, :], start=(k == 0), stop=(k == 7)), R=[cT, wt], P=[p])
                    kb.op("dve", lambda e: e.tensor_tensor(mv[:, nb * 512:(nb + 1) * 512], p[:], mb[:, nb * 512:(nb + 1) * 512], op=ALU.add),
                          R=[p, mb], P=[mv])
                kb.dma("sp", d["MODV"], d["MODV"][l], mv, mv[:])

    def mod_row(self, l, row, idx):
        return self.d["MODV"][l, row, idx * D:(idx + 1) * D]

    def phase_norm(self, l, sh_idx, sc_idx, tiles, dst, final=False):
        kb, d = self.kb, self.d
        with contextlib.ExitStack() as st:
            kb.barrier()
            if final:
                fw = kb.sb(st, "fw", [128, D])
                self.load_bc(fw, self.i["final_norm"], self.i["final_norm"][0, :])
                bc = {0: (None, fw)}
            else:
                bc = {}
                for row in (0, 1):
                    sh = kb.sb(st, f"sh{row}", [128, D])
                    sc = kb.sb(st, f"sc{row}", [128, D])
                    self.load_bc(sh, d["MODV"], self.mod_row(l, row, sh_idx))
                    self.load_bc(sc, d["MODV"], self.mod_row(l, row, sc_idx), plus_one=True)
                    bc[row] = (sh, sc)
            xt = [kb.sb(st, f"nx{j}", [128, D]) for j in range(2)]
            ht = [kb.sb(st, f"nh{j}", [128, D]) for j in range(2)]
            sq = kb.sb(st, "nsq", [128, D])
            ss = kb.sb(st, "nss", [128, 1])
            rs = kb.sb(st, "nrs", [128, 1])
            if tiles:
                kb.dma("sp", xt[0], xt[0][:], d["X"], d["X"][tiles[0] * 128:(tiles[0] + 1) * 128, :], part=False)
            for n, t in enumerate(tiles):
                X, Hh = xt[n % 2], ht[n % 2]
                sh, sc = bc[0 if t < self.NTL else 1]
                if n + 1 < len(tiles):
                    tn = tiles[n + 1]
                    Xn = xt[(n + 1) % 2]
                    kb.dma("sp", Xn, Xn[:], d["X"], d["X"][tn * 128:(tn + 1) * 128, :], part=False)
                kb.op("act", lambda e: e.activation(sq[:], X[:], AF.Square, accum_out=ss[:]), R=[X], W=[sq, ss])
                self.rstd(st, ss, 1, D, rs)
                kb.op("dve", lambda e: e.scalar_tensor_tensor(Hh[:], X[:], rs[:, 0:1], sc[:], op0=ALU.mult, op1=ALU.mult),
                      R=[X, rs, sc], W=[Hh])
                if sh is not None:
                    kb.op("pool", lambda e: e.tensor_tensor(Hh[:], Hh[:], sh[:], op=ALU.add), R=[Hh, sh], W=[Hh])
                kb.dma("sp", dst, dst[t * 128:(t + 1) * 128, :], Hh, Hh[:])

    def phase_linear(self, st, src, w_b, w_ap, N, tiles, post, wt=None, src_ap=None):
        kb = self.kb
        nb = (N + 511) // 512
        if wt is None:
            wt = kb.sb(st, "lw", [128, 8, N])
            kb.dma("sp", wt, wt[:], w_b, w_ap.rearrange("(k p) n -> p k n", p=128), part=False)
        xin = [kb.sb(st, f"lx{j}", [128, D]) for j in range(2)]
        xT = kb.sb(st, "lxT", [128, 8, 128])
        ys = [kb.sb(st, f"ly{j}", [128, N]) for j in range(2)]
        ptr = [kb.ps(st, f"lpt{j}", [128, 512]) for j in range(2)]
        po = [kb.ps(st, f"lpo{j}", [128, 512]) for j in range(nb)]
        sap = src_ap if src_ap is not None else src.t
        if tiles:
            kb.dma("sp", xin[0], xin[0][:], src, sap[tiles[0] * 128:(tiles[0] + 1) * 128, :], part=False)
        for n, t in enumerate(tiles):
            X, Y = xin[n % 2], ys[n % 2]
            if n + 1 < len(tiles):
                tn = tiles[n + 1]
                Xn = xin[(n + 1) % 2]
                kb.dma("sp", Xn, Xn[:], src, sap[tn * 128:(tn + 1) * 128, :], part=False)
            self.transpose8(X, xT, ptr)
            for j in range(nb):
                w = min(512, N - j * 512)
                for k in range(8):
                    kb.op("pe", lambda e: e.matmul(po[j][:, 0:w], xT[:, k, :], wt[:, k, j * 512:j * 512 + w], start=(k == 0), stop=(k == 7)),
                          R=[xT, wt], P=[po[j]])
                eng = "act" if j % 2 == 0 else "dve"
                if eng == "act":
                    kb.op("act", lambda e: e.activation(Y[:, j * 512:j * 512 + w], po[j][:, 0:w], AF.Copy), R=[po[j]], P=[Y])
                else:
                    kb.op("dve", lambda e: e.tensor_copy(Y[:, j * 512:j * 512 + w], po[j][:, 0:w]), R=[po[j]], P=[Y])
            post(n, t, Y)

    def transpose8(self, X, xT, ptr, nblk=8):
        kb = self.kb
        for k in range(nblk):
            p = ptr[k // 4]
            kb.op("pe", lambda e: e.transpose(p[:, (k % 4) * 128:(k % 4 + 1) * 128], X[:, k * 128:(k + 1) * 128], self.ident[:]),
                  R=[X, self.ident], P=[p])
        for j in range((nblk + 3) // 4):
            nn = min(4, nblk - j * 4)
            if j % 2 == 0:
                kb.op("act", lambda e: e.activation(xT[:, j * 4:j * 4 + nn, :], ptr[j][:, 0:nn * 128].rearrange("p (a b) -> p a b", a=nn), AF.Copy),
                      R=[ptr[j]], P=[xT])
            else:
                kb.op("dve", lambda e: e.tensor_copy(xT[:, j * 4:j * 4 + nn, :], ptr[j][:, 0:nn * 128].rearrange("p (a b) -> p a b", a=nn)),
                      R=[ptr[j]], P=[xT])

    def phase_qkv_A(self, l, j):
        kb, i, d = self.kb, self.i, self.d
        with contextlib.ExitStack() as st:
            kb.barrier()
            gq = kb.sb(st, "gq", [128, 128])
            gk = kb.sb(st, "gk", [128, 128])
            self.load_bc(gq, i["a_q_norm"], i["a_q_norm"][j, :])
            self.load_bc(gk, i["a_k_norm"], i["a_k_norm"][j, :])
            sq = kb.sb(st, "asq", [128, 1280])
            ss = kb.sb(st, "ass", [128, 10])
            rs = kb.sb(st, "ars", [128, 10])
            cs = [kb.sb(st, f"acs{k}", [128, 128]) for k in range(2)]
            t1 = kb.sb(st, "at1", [128, 10, 64])
            t2 = kb.sb(st, "at2", [128, 10, 64])
            yr = kb.sb(st, "ayr", [128, 1280])
            qT = kb.sb(st, "aqT", [128, 10, 128])
            ptr = [kb.ps(st, f"apt{k}", [128, 512]) for k in range(3)]

            def post(n, t, Y):
                lat = t < self.NTL
                yv = Y[:, 0:1280].rearrange("p (h e) -> p h e", h=10)
                kb.op("pool", lambda e: e.tensor_tensor(sq[:], Y[:, 0:1280], Y[:, 0:1280], op=ALU.mult), R=[Y], W=[sq])
                kb.op("dve", lambda e: e.tensor_reduce(ss[:], sq[:].rearrange("p (h e) -> p h e", h=10), axis=AX.X, op=ALU.add), R=[sq], W=[ss])
                self.rstd(st, ss, 10, 128, rs)
                kb.op("dve", lambda e: e.tensor_tensor(yv, yv, bc_last(rs[:, :], 128), op=ALU.mult), R=[Y, rs], W=[Y])
                kb.op("pool", lambda e: e.tensor_tensor(yv[:, 0:8, :], yv[:, 0:8, :], bc_mid(gq[:, :], 8), op=ALU.mult), R=[Y, gq], W=[Y])
                kb.op("pool", lambda e: e.tensor_tensor(yv[:, 8:10, :], yv[:, 8:10, :], bc_mid(gk[:, :], 2), op=ALU.mult), R=[Y, gk], W=[Y])
                if lat:
                    c_ = cs[n % 2]
                    kb.dma("sp", c_, c_[:], i["k_ropeA"], i["k_ropeA"][t * 128:(t + 1) * 128, :], part=False)
                    x1, x2 = yv[:, :, 0:64], yv[:, :, 64:128]
                    yrv = yr[:].rearrange("p (h e) -> p h e", h=10)
                    cosb, sinb = bc_mid(c_[:, 0:64], 10), bc_mid(c_[:, 64:128], 10)
                    kb.op("dve", lambda e: e.tensor_tensor(t1[:], x1, cosb, op=ALU.mult), R=[Y, c_], W=[t1])
                    kb.op("pool", lambda e: e.tensor_tensor(t2[:], x2, sinb, op=ALU.mult), R=[Y, c_], W=[t2])
                    kb.op("dve", lambda e: e.tensor_tensor(yrv[:, :, 0:64], t1[:], t2[:], op=ALU.subtract), R=[t1, t2], W=[yr])
                    kb.op("pool", lambda e: e.tensor_tensor(t1[:], x1, sinb, op=ALU.mult), R=[Y, c_], W=[t1])
                    kb.op("dve", lambda e: e.tensor_tensor(t2[:], x2, cosb, op=ALU.mult), R=[Y, c_], W=[t2])
                    kb.op("pool", lambda e: e.tensor_tensor(yrv[:, :, 64:128], t1[:], t2[:], op=ALU.add), R=[t1, t2], P=[yr])
                    src = yr
                else:
                    src = Y
                self.transpose8(src, qT, ptr, nblk=10)
                kb.dma("sp", d["QT"], d["QT"][0:10, :, t * 128:(t + 1) * 128].rearrange("h p t -> p h t"), qT, qT[:])
                kb.dma("sp", d["V"], d["V"][t * 128:(t + 1) * 128, :], Y, Y[:, 1280:1536])

            self.phase_linear(st, d["H"], i["a_w_qkv"], i["a_w_qkv"][j], 1536, list(range(self.NT)), post)

    def phase_attn_A(self, last):
        kb, d, T, S, C = self.kb, self.d, self.T, self.S, self.C
        scale = 128 ** -0.5
        with contextlib.ExitStack() as st:
            kb.barrier()
            kT = kb.sb(st, "kT", [128, T])
            vx = kb.sb(st, "vx", [128, self.NT, 129])
            qb = [kb.sb(st, f"qb{k}", [128, 512]) for k in range(2)]
            pT = [kb.sb(st, f"pT{k}", [128, 512]) for k in range(3)]
            ob = [kb.sb(st, f"ob{k}", [128, 128]) for k in range(2)]
            rc = kb.sb(st, "rc", [128, 1])
            psS = [kb.ps(st, f"psS{k}", [128, 512]) for k in range(2)]
            psO = [kb.ps(st, f"psO{k}", [128, 512]) for k in range(4)]
            nq = 0
            nch = 0
            for g in range(2):
                kb.dma("sp", kT, kT[:], d["QT"], d["QT"][8 + g], part=False)
                kb.op("pool", lambda e: e.memset(vx[:, :, 128:129], 1.0), W=[vx])
                kb.dma("sp", vx, vx[:, :, 0:128], d["V"], d["V"][:, g * 128:(g + 1) * 128].rearrange("(c p) e -> p c e", p=128))
                blocks = [(q0, min(512, S - q0), list(range(self.NT))) for q0 in range(0, S, 512)]
                if not last:
                    blocks += [(q0, min(512, T - q0), list(range(self.NTL, self.NT))) for q0 in range(S, T, 512)]
                items = [(h, q0, nqq, chunks) for h in range(4 * g, 4 * g + 4) for (q0, nqq, chunks) in blocks]
                kb.dma("sp", qb[nq % 2], qb[nq % 2][:, 0:items[0][2]], d["QT"], d["QT"][items[0][0], :, items[0][1]:items[0][1] + items[0][2]], part=False)
                for ii, (h, q0, nqq, chunks) in enumerate(items):
                    if True:
                        Q = qb[nq % 2]
                        nq += 1
                        if ii + 1 < len(items):
                            hn_, q0n, nqn, _ = items[ii + 1]
                            Qn = qb[nq % 2]
                            kb.dma("sp", Qn, Qn[:, 0:nqn], d["QT"], d["QT"][hn_, :, q0n:q0n + nqn], part=False)
                        nsub = nqq // 128
                        for ci, ch in enumerate(chunks):
                            pS = psS[nch % 2]
                            P_ = pT[nch % 3]
                            nch += 1
                            kb.op("pe", lambda e: e.matmul(pS[:, 0:nqq], kT[:, ch * 128:(ch + 1) * 128], Q[:, 0:nqq], start=True, stop=True),
                                  R=[kT, Q], P=[pS])
                            kb.op("act", lambda e: e.activation(P_[:, 0:nqq], pS[:, 0:nqq], AF.Exp, scale=scale), R=[pS], W=[P_])
                            for s in range(nsub):
                                kb.op("pe", lambda e: e.matmul(psO[s][:, 0:129], P_[:, s * 128:(s + 1) * 128], vx[:, ch, :],
                                                               start=(ci == 0), stop=(ci == len(chunks) - 1)), R=[P_, vx], P=[psO[s]])
                        for s in range(nsub):
                            O = ob[s % 2]
                            kb.op("dve", lambda e: e.reciprocal(rc[:], psO[s][:, 128:129]), R=[psO[s]], W=[rc])
                            kb.op("dve", lambda e: e.tensor_scalar(O[:], psO[s][:, 0:128], rc[:, 0:1], None, op0=ALU.mult), R=[psO[s], rc], W=[O])
                            r0 = q0 + s * 128
                            kb.dma("sp", d["O"], d["O"][r0:r0 + 128, h * 128:(h + 1) * 128], O, O[:])

    def phase_out_proj(self, l, w_b, w_ap, last, src=None):
        kb, d = self.kb, self.d
        src = src if src is not None else d["O"]
        with contextlib.ExitStack() as st:
            kb.barrier()
            gt = {}
            for row in ((0,) if last else (0, 1)):
                g = kb.sb(st, f"og{row}", [128, D])
                self.load_bc(g, d["MODV"], self.mod_row(l, row, 2))
                gt[row] = g
            xt = [kb.sb(st, f"ox{k}", [128, D]) for k in range(2)]

            def post(n, t, Y):
                Xt = xt[n % 2]
                g = gt[0 if t < self.NTL else 1]
                kb.dma("sp", Xt, Xt[:], d["X"], d["X"][t * 128:(t + 1) * 128, :], part=False)
                kb.op("pool", lambda e: e.tensor_tensor(Y[:], Y[:], g[:], op=ALU.mult), R=[Y, g], W=[Y])
                kb.op("dve", lambda e: e.tensor_tensor(Xt[:], Xt[:], Y[:], op=ALU.add), R=[Xt, Y], W=[Xt])
                kb.dma("sp", d["X"], d["X"][t * 128:(t + 1) * 128, :], Xt, Xt[:])

            tiles = list(range(self.NTL if last else self.NT))
            self.phase_linear(st, src, w_b, w_ap, D, tiles, post)


    def phase_qkv_B(self, l, j):
        kb, i, d = self.kb, self.i, self.d
        with contextlib.ExitStack() as st:
            kb.barrier()
            cs = [kb.sb(st, f"bcs{k}", [128, 64]) for k in range(2)]
            t1 = kb.sb(st, "bt1", [128, 20, 32])
            t2 = kb.sb(st, "bt2", [128, 20, 32])
            yr = kb.sb(st, "byr", [128, 1280])
            qT = kb.sb(st, "bqT", [128, 10, 128])
            ptr = [kb.ps(st, f"bpt{k}", [128, 512]) for k in range(3)]

            def post(n, t, Y):
                lat = t < self.NTL
                if lat:
                    yv = Y[:, 0:1280].rearrange("p (h e) -> p h e", h=20)
                    c_ = cs[n % 2]
                    kb.dma("sp", c_, c_[:], i["k_ropeB"], i["k_ropeB"][t * 128:(t + 1) * 128, :], part=False)
                    x1, x2 = yv[:, :, 0:32], yv[:, :, 32:64]
                    yrv = yr[:].rearrange("p (h e) -> p h e", h=20)
                    cosb, sinb = bc_mid(c_[:, 0:32], 20), bc_mid(c_[:, 32:64], 20)
                    kb.op("dve", lambda e: e.tensor_tensor(t1[:], x1, cosb, op=ALU.mult), R=[Y, c_], W=[t1])
                    kb.op("pool", lambda e: e.tensor_tensor(t2[:], x2, sinb, op=ALU.mult), R=[Y, c_], W=[t2])
                    kb.op("dve", lambda e: e.tensor_tensor(yrv[:, :, 0:32], t1[:], t2[:], op=ALU.subtract), R=[t1, t2], W=[yr])
                    kb.op("pool", lambda e: e.tensor_tensor(t1[:], x1, sinb, op=ALU.mult), R=[Y, c_], W=[t1])
                    kb.op("dve", lambda e: e.tensor_tensor(t2[:], x2, cosb, op=ALU.mult), R=[Y, c_], W=[t2])
                    kb.op("pool", lambda e: e.tensor_tensor(yrv[:, :, 32:64], t1[:], t2[:], op=ALU.add), R=[t1, t2], P=[yr])
                    src = yr
                else:
                    src = Y
                self.transpose8(src, qT, ptr, nblk=10)
                for half in range(2):
                    dst = d["QT"][:, 0:64, t * 128:(t + 1) * 128].rearrange("(b two) p t -> two p b t", two=2)[half]
                    kb.dma("sp", d["QT"], dst, qT, qT[half * 64:(half + 1) * 64, :, :])
                kb.dma("sp", d["V"], d["V"][t * 128:(t + 1) * 128, :], Y, Y[:, 1280:1536])

            self.phase_linear(st, d["H"], i["b_w_qkv"], i["b_w_qkv"][j], 1536, list(range(self.NT)), post)

    def phase_attn_B(self, j, last):
        kb, i, d, T, S, C = self.kb, self.i, self.d, self.T, self.S, self.C
        scale = 64 ** -0.5
        NT, NTL = self.NT, self.NTL
        with contextlib.ExitStack() as st:
            kb.barrier()
            kT = kb.sb(st, "bkT", [64, 4, T])
            vx = kb.sb(st, "bvx", [128, NT, 4, 65])
            es = kb.sb(st, "bes", [128, 16])
            tri = kb.sb(st, "btri", [128, 2, 128])
            kb.dma("sp", kT, kT[:], d["QT"], d["QT"][16:20, 0:64, :].rearrange("g p t -> p g t"), part=False)
            kb.op("pool", lambda e: e.memset(vx[:, :, :, 64:65], 1.0), W=[vx])
            for g in range(4):
                kb.dma("sp", vx, vx[:, :, g, 0:64], d["V"], d["V"][:, g * 64:(g + 1) * 64].rearrange("(c p) e -> p c e", p=128))
            self.load_bc(es, i["b_sink"], i["b_sink"][j, :])
            kb.op("act", lambda e: e.activation(es[:], es[:], AF.Exp), R=[es], W=[es])
            kb.dma("sp", tri, tri[:], i["k_tri"], i["k_tri"][:].rearrange("a p q -> p a q"), part=False)
            qa = [kb.sb(st, f"bqa{k}", [64, 16, 128]) for k in range(2)]
            pT = [kb.sb(st, f"bpT{k}", [128, 512]) for k in range(3)]
            ot = [kb.sb(st, f"bot{k}", [128, D]) for k in range(2)]
            den = kb.sb(st, "bden", [128, 1])
            psS = [kb.ps(st, f"bpsS{k}", [128, 512]) for k in range(2)]
            psO = [kb.ps(st, f"bpsO{k}", [128, 512]) for k in range(4)]
            nch = 0
            tiles = list(range(NTL if last else NT))
            for n, t in enumerate(tiles):
                if t < NTL:
                    chunks = []
                    if t - 1 >= 0:
                        chunks.append((t - 1, 0))
                    chunks.append((t, None))
                    if t + 1 < NTL:
                        chunks.append((t + 1, 1))
                    chunks += [(c_, None) for c_ in range(NTL, NT)]
                else:
                    chunks = [(c_, None) for c_ in range(NTL, NT)]
                Q = qa[n % 2]
                O = ot[n % 2]
                kb.dma("sp", Q, Q[:], d["QT"], d["QT"][0:16, 0:64, t * 128:(t + 1) * 128].rearrange("h p t -> p h t"), part=False)
                for g in range(4):
                    for ci, (ch, mk) in enumerate(chunks):
                        pS = psS[nch % 2]
                        P_ = pT[nch % 3]
                        nch += 1
                        kb.op("pe", lambda e: e.matmul(pS[:], kT[:, g, ch * 128:(ch + 1) * 128], Q[:, 4 * g:4 * g + 4, :].rearrange("p h t -> p (h t)"),
                                                       start=True, stop=True), R=[kT, Q], P=[pS])
                        kb.op("act", lambda e: e.activation(P_[:], pS[:], AF.Exp, scale=scale), R=[pS], W=[P_])
                        if mk is not None:
                            pv = P_[:].rearrange("p (h t) -> p h t", h=4)
                            kb.op("pool", lambda e: e.tensor_tensor(pv, pv, bc_mid(tri[:, mk, :], 4), op=ALU.mult), R=[P_, tri], W=[P_])
                        for s in range(4):
                            kb.op("pe", lambda e: e.matmul(psO[s][:, 0:65], P_[:, s * 128:(s + 1) * 128], vx[:, ch, g, :],
                                                           start=(ci == 0), stop=(ci == len(chunks) - 1)), R=[P_, vx], P=[psO[s]])
                    for s in range(4):
                        h = 4 * g + s
                        kb.op("dve", lambda e: e.tensor_tensor(den[:], psO[s][:, 64:65], es[:, h:h + 1], op=ALU.add), R=[psO[s], es], W=[den])
                        kb.op("dve", lambda e: e.reciprocal(den[:], den[:]), R=[den], W=[den])
                        kb.op("dve", lambda e: e.tensor_scalar(O[:, h * 64:(h + 1) * 64], psO[s][:, 0:64], den[:, 0:1], None, op0=ALU.mult),
                              R=[psO[s], den], P=[O])
                kb.dma("sp", d["O"], d["O"][t * 128:(t + 1) * 128, :], O, O[:])


    def phase_rwkv(self, l, j, last, dbg=99):
        kb, i, d, T, S, C = self.kb, self.i, self.d, self.T, self.S, self.C
        NT, NTL = self.NT, self.NTL
        alltiles = list(range(NT))
        with contextlib.ExitStack() as st:
            kb.barrier()
            mu = [kb.sb(st, f"mu{k}", [128, D]) for k in range(6)]
            for k in range(6):
                self.load_bc(mu[k], i["c_mu"], i["c_mu"][j, k, :])
            hh = [kb.sb(st, f"rh{k}", [128, D]) for k in range(2)]
            hp = [kb.sb(st, f"rhp{k}", [128, D]) for k in range(2)]
            hn = [kb.sb(st, f"rhn{k}", [128, D]) for k in range(2)]
            xx = kb.sb(st, "rxx", [128, D])
            xj = [kb.sb(st, f"rxj{k}", [128, D]) for k in range(3)]
            nx = 0
            for n, t in enumerate(alltiles):
                t0 = t * 128
                Hc, Hp, Hn = hh[n % 2], hp[n % 2], hn[n % 2]
                kb.dma("sp", Hc, Hc[:], d["H"], d["H"][t0:t0 + 128, :], part=False)
                if t == 0 or t == NTL:
                    kb.dma("sp", Hp, Hp[0:1, :], i["k_zero"], i["k_zero"][0:1, :], part=False)
                    kb.dma("sp", Hp, Hp[1:128, :], d["H"], d["H"][t0:t0 + 127, :])
                else:
                    kb.dma("sp", Hp, Hp[:], d["H"], d["H"][t0 - 1:t0 + 127, :], part=False)
                if t == NTL - 1 or t == NT - 1:
                    kb.dma("sp", Hn, Hn[127:128, :], i["k_zero"], i["k_zero"][0:1, :], part=False)
                    kb.dma("sp", Hn, Hn[0:127, :], d["H"], d["H"][t0 + 1:t0 + 128, :])
                else:
                    kb.dma("sp", Hn, Hn[:], d["H"], d["H"][t0 + 1:t0 + 129, :], part=False)
                kb.op("pool", lambda e: e.tensor_tensor(Hp[:], Hp[:], Hn[:], op=ALU.add), R=[Hp, Hn], W=[Hp])
                kb.op("dve", lambda e: e.scalar_tensor_tensor(xx[:], Hp[:], 0.5, Hc[:], op0=ALU.mult, op1=ALU.subtract), R=[Hp, Hc], W=[xx])
                for k in range(6):
                    X = xj[nx % 3]
                    nx += 1
                    e1, e2 = ("pool", "dve") if k % 2 == 0 else ("dve", "pool")
                    kb.op(e1, lambda e: e.tensor_tensor(X[:], xx[:], mu[k][:], op=ALU.mult), R=[xx, mu[k]], W=[X])
                    kb.op(e2, lambda e: e.tensor_tensor(X[:], X[:], Hc[:], op=ALU.add), R=[X, Hc], W=[X])
                    kb.dma("sp", d["XJ"], d["XJ"][k, t0:t0 + 128, :], X, X[:])
        for (src_k, wi, dst) in ((0, 0, "RR"), (2, 1, "KK"), (3, 2, "VV")):
            with contextlib.ExitStack() as st:
                kb.barrier()

                def post(n, t, Y, dst=dst):
                    kb.dma("sp", d[dst], d[dst][t * 128:(t + 1) * 128, :], Y, Y[:])

                self.phase_linear(st, d["XJ"], i["c_w_rkv"], i["c_w_rkv"][j, wi], D, alltiles, post, src_ap=d["XJ"][src_k])
        def lora(src_k, fill_w1, act, fill_w2, nsplit, epi):
            with contextlib.ExitStack() as st:
                kb.barrier()
                wt = kb.sb(st, "lrw1", [128, 8, 128])
                fill_w1(wt)
                w2t = kb.sb(st, "lrw2", [128, D])
                fill_w2(w2t)
                yT = kb.sb(st, "lryT", [128, 128])
                ot = [kb.sb(st, f"lro{k}", [128, D]) for k in range(2)]
                tmp = kb.sb(st, "lrtmp", [128, 512])
                pt = kb.ps(st, "lrpt", [128, 512])
                po = [kb.ps(st, f"lrpo{k}", [128, 512]) for k in range(2)]
                cnt = [0]

                def post(n, t, Y):
                    if act is not None:
                        kb.op("act", lambda e: e.activation(Y[:], Y[:], act), R=[Y], W=[Y])
                    kb.op("pe", lambda e: e.transpose(pt[:, 0:128], Y[:, 0:128], self.ident[:]), R=[Y, self.ident], W=[pt])
                    kb.op("dve", lambda e: e.tensor_copy(yT[:], pt[:, 0:128]), R=[pt], W=[yT])
                    kk = 128 // nsplit
                    for dd in range(nsplit):
                        O = ot[cnt[0] % 2]
                        cnt[0] += 1
                        for nb in range(2):
                            kb.op("pe", lambda e: e.matmul(po[nb][:], yT[dd * kk:(dd + 1) * kk, :], w2t[dd * kk:(dd + 1) * kk, nb * 512:(nb + 1) * 512],
                                                           start=True, stop=True), R=[yT, w2t], P=[po[nb]])
                            epi(dd, nb, po[nb], O, tmp)
                        self_dst = epi.dst(dd)
                        kb.dma("sp", self_dst[0], self_dst[1][t * 128:(t + 1) * 128, :], O, O[:])

                self.phase_linear(st, d["XJ"], None, None, 128, alltiles, post, wt=wt, src_ap=d["XJ"][src_k])

        def fill2(arr):
            def f(wt):
                for dd in range(2):
                    kb.dma("sp", wt, wt[:, :, dd * 64:(dd + 1) * 64], i[arr], i[arr][j, dd].rearrange("(k p) n -> p k n", p=128))
            return f

        def fill2b(arr):
            def f(w2t):
                for dd in range(2):
                    kb.dma("sp", w2t, w2t[dd * 64:(dd + 1) * 64, :], i[arr], i[arr][j, dd])
            return f

        with contextlib.ExitStack() as stb:
            kb.barrier()
            bw = [kb.sb(stb, f"bw{k}", [128, D]) for k in range(2)]
            ba = [kb.sb(stb, f"ba{k}", [128, D]) for k in range(2)]
            for dd in range(2):
                self.load_bc(bw[dd], i["c_w0"], i["c_w0"][j, dd, :])
                self.load_bc(ba[dd], i["c_a0"], i["c_a0"][j, dd, :])

            def epi_w(dd, nb, ps, O, tmp):
                sl = slice(nb * 512, (nb + 1) * 512)
                kb.op("dve", lambda e: e.tensor_tensor(tmp[:], ps[:], bw[dd][:, sl], op=ALU.add), R=[ps, bw[dd]], W=[tmp])
                kb.op("act", lambda e: e.activation(tmp[:], tmp[:], AF.Sigmoid), R=[tmp], W=[tmp])
                kb.op("act", lambda e: e.activation(O[:, sl], tmp[:], AF.Exp, scale=-float(np.exp(-0.5))), R=[tmp], P=[O])
            epi_w.dst = lambda dd: (d["DEC"], d["DEC"][dd])

            def epi_a(dd, nb, ps, O, tmp):
                sl = slice(nb * 512, (nb + 1) * 512)
                kb.op("dve", lambda e: e.tensor_tensor(tmp[:], ps[:], ba[dd][:, sl], op=ALU.add), R=[ps, ba[dd]], W=[tmp])
                kb.op("act", lambda e: e.activation(O[:, sl], tmp[:], AF.Sigmoid), R=[tmp], P=[O])
            epi_a.dst = lambda dd: (d["AA"], d["AA"][dd])

            def epi_g(dd, nb, ps, O, tmp):
                sl = slice(nb * 512, (nb + 1) * 512)
                kb.op("act", lambda e: e.activation(O[:, sl], ps[:], AF.Copy), R=[ps], P=[O])
            epi_g.dst = lambda dd: (d["GG"], d["GG"])

            lora(1, fill2("c_w1"), AF.Tanh, fill2b("c_w2"), 2, epi_w)
            lora(4, fill2("c_a1"), None, fill2b("c_a2"), 2, epi_a)
            lora(5, lambda wt: kb.dma("sp", wt, wt[:], i["c_g1"], i["c_g1"][j].rearrange("(k p) n -> p k n", p=128), part=False),
                 AF.Sigmoid, lambda w2t: kb.dma("sp", w2t, w2t[:], i["c_g2"], i["c_g2"][j], part=False), 1, epi_g)
        if dbg == 2:
            return
        with contextlib.ExitStack() as st:
            kb.barrier()
            kkb = kb.sb(st, "kkb", [128, D])
            kab = kb.sb(st, "kab", [128, D])
            rkb = [kb.sb(st, f"rkb{k}", [128, D]) for k in range(2)]
            self.load_bc(kkb, i["c_k_k"], i["c_k_k"][j, :])
            self.load_bc(kab, i["c_k_a"], i["c_k_a"][j, :])
            for dd in range(2):
                self.load_bc(rkb[dd], i["c_r_k"], i["c_r_k"][j, dd, :])
            tk = kb.sb(st, "tk", [128, D])
            tr = kb.sb(st, "tr", [128, D])
            ta = [kb.sb(st, f"ta{k}", [128, D]) for k in range(2)]
            tw = [kb.sb(st, f"tw{k}", [128, D]) for k in range(2)]
            kk = kb.sb(st, "kk", [128, D])
            t1 = kb.sb(st, "t1", [128, D])
            t2 = kb.sb(st, "t2", [128, D])
            t3 = kb.sb(st, "t3", [128, D])
            t4 = kb.sb(st, "t4", [128, D])
            ss = kb.sb(st, "ss", [128, 16])
            rs = kb.sb(st, "rs", [128, 16])
            sc = kb.sb(st, "sc", [128, 96])
            hv = lambda b: b[:].rearrange("p (h e) -> p h e", h=16)
            for n, t in enumerate(alltiles):
                rows = slice(t * 128, (t + 1) * 128)
                kb.dma("sp", tk, tk[:], d["KK"], d["KK"][rows, :], part=False)
                kb.dma("sp", tr, tr[:], d["RR"], d["RR"][rows, :], part=False)
                for dd in range(2):
                    kb.dma("sp", ta[dd], ta[dd][:], d["AA"], d["AA"][dd, rows, :], part=False)
                    kb.dma("sp", tw[dd], tw[dd][:], d["DEC"], d["DEC"][dd, rows, :], part=False)
                kb.op("pool", lambda e: e.tensor_tensor(kk[:], tk[:], kkb[:], op=ALU.mult), R=[tk, kkb], W=[kk])
                kb.op("dve", lambda e: e.tensor_tensor(t1[:], kk[:], kk[:], op=ALU.mult), R=[kk], W=[t1])
                kb.op("dve", lambda e: e.tensor_reduce(ss[:], hv(t1), axis=AX.X, op=ALU.add), R=[t1], W=[ss])
                kb.op("dve", lambda e: e.tensor_scalar(rs[:], ss[:], 1e-24, None, op0=ALU.max), R=[ss], W=[rs])
                kb.op("act", lambda e: e.activation(rs[:], rs[:], AF.Sqrt), R=[rs], W=[rs])
                kb.op("dve", lambda e: e.reciprocal(rs[:], rs[:]), R=[rs], W=[rs])
                kb.op("dve", lambda e: e.tensor_tensor(hv(kk), hv(kk), bc_last(rs[:, :], 64), op=ALU.mult), R=[kk, rs], W=[kk])
                kb.op("act", lambda e: e.activation(t1[:], kk[:], AF.Copy, scale=-1.0), R=[kk], W=[t1])
                kb.dma("sp", d["AV"], d["AV"][rows, :], t1, t1[:])
                for dd in range(2):
                    kb.op("dve", lambda e: e.scalar_tensor_tensor(t2[:], ta[dd][:], -1.0, kab[:], op0=ALU.add, op1=ALU.mult), R=[ta[dd], kab], W=[t2])
                    kb.op("dve", lambda e: e.scalar_tensor_tensor(t2[:], t2[:], 1.0, tk[:], op0=ALU.add, op1=ALU.mult), R=[t2, tk], W=[t2])
                    kb.dma("sp", d["KD"], d["KD"][dd, rows, :], t2, t2[:])
                    kb.op("pool", lambda e: e.tensor_tensor(t3[:], kk[:], ta[dd][:], op=ALU.mult), R=[kk, ta[dd]], W=[t3])
                    kb.dma("sp", d["BB"], d["BB"][dd, rows, :], t3, t3[:])
                    kb.op("pool", lambda e: e.tensor_tensor(t4[:], tr[:], tw[dd][:], op=ALU.mult), R=[tr, tw[dd]], W=[t4])
                    kb.dma("sp", d["RP"], d["RP"][dd, rows, :], t4, t4[:])
                    kb.op("pool", lambda e: e.tensor_tensor(t3[:], t3[:], tr[:], op=ALU.mult), R=[t3, tr], W=[t3])
                    kb.op("dve", lambda e: e.tensor_reduce(sc[:, dd * 16:(dd + 1) * 16], hv(t3), axis=AX.X, op=ALU.add), R=[t3], P=[sc])
                    kb.op("pool", lambda e: e.tensor_tensor(t2[:], t2[:], tr[:], op=ALU.mult), R=[t2, tr], W=[t2])
                    kb.op("dve", lambda e: e.tensor_reduce(sc[:, 32 + dd * 16:32 + (dd + 1) * 16], hv(t2), axis=AX.X, op=ALU.add), R=[t2], P=[sc])
                    kb.op("pool", lambda e: e.tensor_tensor(t2[:], t2[:], rkb[dd][:], op=ALU.mult), R=[t2, rkb[dd]], W=[t2])
                    kb.op("dve", lambda e: e.tensor_reduce(sc[:, 64 + dd * 16:64 + (dd + 1) * 16], hv(t2), axis=AX.X, op=ALU.add), R=[t2], P=[sc])
                kb.op("dve", lambda e: e.tensor_tensor(sc[:, 64:80], sc[:, 64:80], sc[:, 80:96], op=ALU.add), R=[sc], W=[sc])
                kb.dma("sp", d["SC"], d["SC"][rows, :], sc, sc[:, 0:80])
        if dbg == 3:
            return
        with contextlib.ExitStack() as st:
            kb.barrier()
            bd = kb.sb(st, "bd", [48, 512])
            kb.dma("sp", bd, bd[:], i["k_bd"], i["k_bd"][:], part=False)
            selc = kb.sb(st, "selc", [128, 64, 16])
            kb.dma("sp", selc, selc[:], i["k_sel"], i["k_sel"][:], part=False)

            def scan_dir(dd):
                svt = [kb.sb(st, f"svt{dd}{k}", [128, 512]) for k in range(2)]
                vr = kb.sb(st, f"VR{dd}", [128, 512])
                lt = kb.sb(st, f"LT{dd}", [128, 64, 32])
                wt_ = kb.sb(st, f"WT{dd}", [128, 64, 8])
                l2 = kb.sb(st, f"L2{dd}", [48, 64, 128])
                Mr = [kb.sb(st, f"Mr{dd}{k}", [48, 512]) for k in range(2)]
                stg = [kb.sb(st, f"stg{dd}{a}", [64, D]) for a in range(3)]
                po1 = kb.ps(st, f"spo1{dd}", [128, 512])
                pu = kb.ps(st, f"spu{dd}", [128, 512])
                pt = kb.ps(st, f"spt{dd}", [128, 512])
                kb.op("pool", lambda e: e.memset(lt[:], 0.0), W=[lt])
                kb.op("pool", lambda e: e.memset(l2[:], 0.0), W=[l2])
                kb.op("pool", lambda e: e.memset(svt[0][:], 0.0), W=[svt[0]])
                yield
                p = 0
                nstep = 0
                nchunk = 0
                for (lo, hi) in [(S, T), (0, S)]:
                    c0s = list(range(lo, hi, 64))
                    if dd == 1:
                        c0s = c0s[::-1]
                    for c0 in c0s:
                        if dbg == 41 and nchunk >= 1:
                            break
                        nchunk += 1
                        srcs = [(d["AV"], d["AV"][c0:c0 + 64, :]), (d["RP"], d["RP"][dd, c0:c0 + 64, :]), (d["DEC"], d["DEC"][dd, c0:c0 + 64, :])]
                        for a, (sb_, sap) in enumerate(srcs):
                            kb.dma("sp", stg[a], stg[a][:], sb_, sap, part=False)
                        for (r0, arr) in ((0, "BB"), (32, "KD")):
                            for hg in range(2):
                                kb.dma("sp", l2, l2[r0 + hg * 8:r0 + hg * 8 + 8, :, hg * 64:(hg + 1) * 64], d[arr],
                                       d[arr][dd, c0:c0 + 64, :].rearrange("t (hp hg k) -> hg hp t k", hg=2, k=64)[hg])
                        for hg in range(2):
                            kb.dma("sp", vr, vr[hg * 64:(hg + 1) * 64, :].rearrange("t (hp v) -> t hp v", v=64), d["VV"],
                                   d["VV"][c0:c0 + 64, :].rearrange("t (hp hg v) -> hg t hp v", hg=2, v=64)[hg])
                        for a in range(3):
                            G = stg[a]
                            for hp in range(8):
                                kb.op("pe", lambda e: e.transpose(pt[:, hp * 64:(hp + 1) * 64], G[:, hp * 128:(hp + 1) * 128], self.ident[0:64, 0:64]),
                                      R=[G, self.ident], P=[pt])
                            if a < 2:
                                kb.op("act", lambda e: e.activation(lt[0:64, :, a * 16:a * 16 + 8], pt[0:64, :].rearrange("p (h t) -> p t h", h=8), AF.Copy),
                                      R=[pt], P=[lt])
                                kb.op("dve", lambda e: e.tensor_copy(lt[64:128, :, a * 16 + 8:a * 16 + 16], pt[64:128, :].rearrange("p (h t) -> p t h", h=8)),
                                      R=[pt], P=[lt])
                            else:
                                kb.op("act", lambda e: e.activation(wt_[:], pt[:].rearrange("p (h t) -> p t h", h=8), AF.Copy), R=[pt], W=[wt_])
                        yield
                        order = range(64) if dd == 0 else range(63, -1, -1)
                        for tl in order:
                            cur, nxt = p, 1 - p
                            M = Mr[nstep % 2]
                            nstep += 1
                            kb.op("pe", lambda e: e.matmul(po1[0:32, :], lt[:, tl, :], svt[cur][:], start=True, stop=True), R=[lt, svt[cur]], W=[po1])
                            kb.op("pe", lambda e: e.matmul(po1[32:48, :], selc[:, tl, :], vr[:], start=True, stop=True), R=[selc, vr], P=[po1])
                            kb.op("pool", lambda e: e.tensor_tensor(svt[nxt][:].rearrange("p (h e) -> p h e", h=8),
                                                                     svt[cur][:].rearrange("p (h e) -> p h e", h=8),
                                                                     bc_last(wt_[:, tl, :], 64), op=ALU.mult), R=[svt[cur], wt_], W=[svt[nxt]])
                            yield
                            kb.op("dve", lambda e: e.tensor_tensor(M[:], po1[0:48, :], bd[:], op=ALU.mult), R=[po1, bd], W=[M])
                            yield
                            kb.op("pe", lambda e: e.matmul(pu[:], l2[:, tl, :], M[:], start=True, stop=True), R=[l2, M], W=[pu])
                            yield
                            kb.op("dve", lambda e: e.tensor_tensor(svt[nxt][:], svt[nxt][:], pu[:], op=ALU.add), R=[svt[nxt], pu], W=[svt[nxt]])
                            tok = c0 + tl
                            qsb = d["QS"][dd][tok // 2048]
                            kb.dma("sp", qsb, qsb[tok % 2048, :].rearrange("(p n) -> p n", p=32), M, M[0:32, :])
                            p = nxt
                            yield

            gens = [scan_dir(0), scan_dir(1)]
            while gens:
                for g in list(gens):
                    try:
                        next(g)
                    except StopIteration:
                        gens.remove(g)
        if dbg in (4, 41):
            return
        tiles = list(range(NTL if last else NT))
        with contextlib.ExitStack() as st:
            kb.barrier()
            lw = kb.sb(st, "lnw", [128, D])
            lb = kb.sb(st, "lnb", [128, D])
            self.load_bc(lw, i["c_ln_w"], i["c_ln_w"][j, :])
            self.load_bc(lb, i["c_ln_b"], i["c_ln_b"][j, :])
            sa = [kb.sb(st, f"osa{k}", [128, D]) for k in range(2)]
            qq = [kb.sb(st, f"oqq{k}", [128, D]) for k in range(2)]
            tv = kb.sb(st, "otv", [128, D])
            tg = kb.sb(st, "otg", [128, D])
            sc = kb.sb(st, "osc", [128, 80])
            o = kb.sb(st, "oo", [128, D])
            t1 = kb.sb(st, "ot1", [128, D])
            m1 = kb.sb(st, "om1", [128, 16])
            m2 = kb.sb(st, "om2", [128, 16])
            hv = lambda b: b[:].rearrange("p (h e) -> p h e", h=16)
            for n, t in enumerate(tiles):
                rows = slice(t * 128, (t + 1) * 128)
                for dd in range(2):
                    qsb = d["QS"][dd][(t * 128) // 2048]
                    r0 = (t * 128) % 2048
                    qv = qsb[r0:r0 + 128, :].rearrange("t (ty g q v) -> t ty g q v", ty=2, g=2, q=64, v=64)
                    for ty, dstb in ((0, sa[dd]), (1, qq[dd])):
                        dv = dstb[:].rearrange("p (hp hg v) -> p hg hp v", hg=2, v=64)
                        for hg in range(2):
                            kb.dma("sp", dstb, dv[:, hg], qsb, qv[:, ty, hg, ::9, :])
                kb.dma("sp", tv, tv[:], d["VV"], d["VV"][rows, :], part=False)
                kb.dma("sp", tg, tg[:], d["GG"], d["GG"][rows, :], part=False)
                kb.dma("sp", sc, sc[:], d["SC"], d["SC"][rows, :], part=False)
                kb.op("pool", lambda e: e.tensor_tensor(o[:], qq[0][:], qq[1][:], op=ALU.add), R=[qq[0], qq[1]], W=[o])
                for dd in range(2):
                    kb.op("dve", lambda e: e.tensor_tensor(hv(t1), hv(sa[dd]), bc_last(sc[:, dd * 16:(dd + 1) * 16], 64), op=ALU.mult), R=[sa[dd], sc], W=[t1])
                    kb.op("pool", lambda e: e.tensor_tensor(o[:], o[:], t1[:], op=ALU.add), R=[o, t1], W=[o])
                kb.op("dve", lambda e: e.tensor_tensor(m1[:], sc[:, 32:48], sc[:, 48:64], op=ALU.add), R=[sc], W=[m1])
                kb.op("dve", lambda e: e.tensor_tensor(hv(t1), hv(tv), bc_last(m1[:, :], 64), op=ALU.mult), R=[tv, m1], W=[t1])
                kb.op("pool", lambda e: e.tensor_tensor(o[:], o[:], t1[:], op=ALU.add), R=[o, t1], W=[o])
                kb.op("dve", lambda e: e.tensor_reduce(m1[:], hv(o), axis=AX.X, op=ALU.add), R=[o], W=[m1])
                kb.op("dve", lambda e: e.tensor_scalar(m1[:], m1[:], -1.0 / 64, None, op0=ALU.mult), R=[m1], W=[m1])
                kb.op("dve", lambda e: e.tensor_tensor(hv(o), hv(o), bc_last(m1[:, :], 64), op=ALU.add), R=[o, m1], W=[o])
                kb.op("pool", lambda e: e.tensor_tensor(t1[:], o[:], o[:], op=ALU.mult), R=[o], W=[t1])
                kb.op("dve", lambda e: e.tensor_reduce(m2[:], hv(t1), axis=AX.X, op=ALU.add), R=[t1], W=[m2])
                kb.op("dve", lambda e: e.tensor_scalar(m2[:], m2[:], 1.0 / 64, 64 * 1e-5, op0=ALU.mult, op1=ALU.add), R=[m2], W=[m2])
                kb.op("act", lambda e: e.activation(m2[:], m2[:], AF.Sqrt), R=[m2], W=[m2])
                kb.op("dve", lambda e: e.reciprocal(m2[:], m2[:]), R=[m2], W=[m2])
                kb.op("dve", lambda e: e.tensor_tensor(hv(o), hv(o), bc_last(m2[:, :], 64), op=ALU.mult), R=[o, m2], W=[o])
                kb.op("pool", lambda e: e.tensor_tensor(o[:], o[:], lw[:], op=ALU.mult), R=[o, lw], W=[o])
                kb.op("dve", lambda e: e.tensor_tensor(o[:], o[:], lb[:], op=ALU.add), R=[o, lb], W=[o])
                kb.op("dve", lambda e: e.tensor_tensor(hv(t1), hv(tv), bc_last(sc[:, 64:80], 64), op=ALU.mult), R=[tv, sc], W=[t1])
                kb.op("pool", lambda e: e.tensor_tensor(o[:], o[:], t1[:], op=ALU.add), R=[o, t1], W=[o])
                kb.op("dve", lambda e: e.tensor_tensor(o[:], o[:], tg[:], op=ALU.mult), R=[o, tg], W=[o])
                kb.dma("sp", d["O"], d["O"][rows, :], o, o[:])
        self.phase_out_proj(l, i["c_w_o"], i["c_w_o"][j], last)

    def phase_moe(self, l, last):
        kb, i, d, S, C, T, FF = self.kb, self.i, self.d, self.S, self.C, self.T, self.FF
        capl, capc = 2 * S // NE, 2 * C // NE
        tiles = list(range(self.NTL if last else self.NT))
        self.phase_norm(l, 3, 4, tiles, d["H"])
        chunks = [(0, s0, min(128, capl - s0)) for s0 in range(0, capl, 128)]
        if not last:
            chunks += [(1, s0, min(128, capc - s0)) for s0 in range(0, capc, 128)]
        NCH = len(chunks)
        NSL = NCH * 128
        offs = [sum(c[2] for c in chunks[:k]) for k in range(NCH)]
        NSC = sum(c[2] for c in chunks)
        with contextlib.ExitStack() as sto:
            kb.barrier()
            idxT = kb.sb(sto, "idxT", [128, NCH, NE], I32)
            gateT = kb.sb(sto, "gateT", [128, NCH, NE])
            with contextlib.ExitStack() as st:
                kb.barrier()
                affT = kb.sb(st, "affT", [NE, T])
                mx = kb.sb(st, "rmx", [128, 1])
                sm = kb.sb(st, "rsm", [128, 1])
                ex = kb.sb(st, "rex", [128, NE])
                pa = kb.ps(st, "rpa", [NE, 128])

                def post(n, t, Y):
                    kb.op("dve", lambda e: e.tensor_reduce(mx[:], Y[:, 0:NE], axis=AX.X, op=ALU.max), R=[Y], W=[mx])
                    kb.op("dve", lambda e: e.tensor_scalar(mx[:], mx[:], -1.0, None, op0=ALU.mult), R=[mx], W=[mx])
                    kb.op("act", lambda e: e.activation(ex[:], Y[:, 0:NE], AF.Exp, bias=mx[:, 0:1], scale=1.0, accum_out=sm[:]), R=[Y, mx], W=[ex, sm])
                    kb.op("dve", lambda e: e.reciprocal(sm[:], sm[:]), R=[sm], W=[sm])
                    kb.op("dve", lambda e: e.tensor_scalar(ex[:], ex[:], sm[:, 0:1], None, op0=ALU.mult), R=[ex, sm], W=[ex])
                    kb.op("pe", lambda e: e.transpose(pa[:], ex[:], self.ident[:]), R=[ex, self.ident], W=[pa])
                    kb.op("dve", lambda e: e.tensor_copy(affT[:, t * 128:(t + 1) * 128], pa[:]), R=[pa], P=[affT])

                with contextlib.ExitStack() as st2:
                    kb.barrier()
                    self.phase_linear(st2, d["H"], i["router_w"], i["router_w"][l], NE, tiles, post)
                kb.barrier()
                vals = kb.sb(st, "tvals", [NE, NSL])
                idxf = kb.sb(st, "tidxf", [NE, NSL])
                idxu = kb.sb(st, "tidxu", [NE, 8], U32)
                kb.op("dve", lambda e: e.memset(vals[:], 0.0), W=[vals])
                kb.op("dve", lambda e: e.memset(idxf[:], 0.0), W=[idxf])
                for (isc, s0, cnt), ci in zip(chunks, range(NCH)):
                    lo, hi = (S, T) if isc else (0, S)
                    for it in range(cnt // 8):
                        col = ci * 128 + it * 8
                        work = affT[:, lo:hi]
                        kb.op("dve", lambda e: e.max(out=vals[:, col:col + 8], in_=work), R=[affT], P=[vals])
                        kb.op("dve", lambda e: e.max_index(out=idxu[:], in_max=vals[:, col:col + 8], in_values=work), R=[affT, vals], W=[idxu])
                        kb.op("dve", lambda e: e.tensor_copy(idxf[:, col:col + 8], idxu[:]), R=[idxu], P=[idxf])
                        kb.op("dve", lambda e: e.match_replace(out=work, in_to_replace=vals[:, col:col + 8], in_values=work, imm_value=-1.0),
                              R=[vals], W=[affT])
                    if isc:
                        kb.op("dve", lambda e: e.tensor_scalar(idxf[:, ci * 128:ci * 128 + cnt], idxf[:, ci * 128:ci * 128 + cnt], float(S), None, op0=ALU.add),
                              R=[idxf], W=[idxf])
                padf = kb.sb(st, "padf", [128, 1])
                kb.dma("sp", padf, padf[:], i["k_pad"], i["k_pad"][:], part=False)
                idxTf = kb.sb(st, "idxTf", [128, NCH, NE])
                kb.op("dve", lambda e: e.tensor_copy(idxTf[:].rearrange("p a b -> p (a b)"), padf[:, 0:1].to_broadcast([128, NCH * NE])), R=[padf], W=[idxTf])
                kb.op("dve", lambda e: e.memset(gateT[:], 0.0), W=[gateT])
                pt = kb.ps(st, "tpt", [128, 2 * NE])
                for (isc, s0, cnt), ci in zip(chunks, range(NCH)):
                    kb.op("pe", lambda e: e.transpose(pt[:, 0:NE], idxf[:, ci * 128:(ci + 1) * 128], self.ident[0:NE, 0:NE]), R=[idxf, self.ident], P=[pt])
                    kb.op("pe", lambda e: e.transpose(pt[:, NE:2 * NE], vals[:, ci * 128:(ci + 1) * 128], self.ident[0:NE, 0:NE]), R=[vals, self.ident], P=[pt])
                    kb.op("dve", lambda e: e.tensor_copy(idxTf[0:cnt, ci, :], pt[0:cnt, 0:NE]), R=[pt], W=[idxTf])
                    kb.op("dve", lambda e: e.tensor_copy(gateT[0:cnt, ci, :], pt[0:cnt, NE:2 * NE]), R=[pt], W=[gateT])
                kb.op("dve", lambda e: e.tensor_copy(idxT[:], idxTf[:]), R=[idxTf], W=[idxT])
            if self.cfg.get('dbg', 99) == 5:
                return
            with contextlib.ExitStack() as st:
                kb.barrier()
                NF = FF // 128
                FB = min(256, FF)
                NFB = FF // FB
                gm = {}
                for row in ((0,) if last else (0, 1)):
                    g = kb.sb(st, f"mg{row}", [128, D])
                    self.load_bc(g, d["MODV"], self.mod_row(l, row, 5))
                    gm[row] = g
                xg = [kb.sb(st, f"xg{k}", [128, D]) for k in range(2)]
                xsT = kb.sb(st, "xsT", [128, 8, NSC])
                hT = kb.sb(st, "hT", [128, NF, NSC])
                w1 = [kb.sb(st, f"w1_{k}", [128, 8, FB]) for k in range(2)]
                w3 = [kb.sb(st, f"w3_{k}", [128, 8, FB]) for k in range(2)]
                w2 = [kb.sb(st, f"w2_{k}", [128, NF, 256]) for k in range(2)]
                sg = kb.sb(st, "sg", [128, 512])
                yo = [kb.sb(st, f"yo{k}", [128, D]) for k in range(NCH)]
                xr = [kb.sb(st, f"xr{k}", [128, D]) for k in range(2)]
                ptr = [kb.ps(st, f"mpt{k}", [128, 512]) for k in range(2)]
                p1 = [kb.ps(st, f"mp1{k}", [128, 512]) for k in range(2)]
                p3 = [kb.ps(st, f"mp3{k}", [128, 512]) for k in range(2)]
                py = [kb.ps(st, f"mpy{k}", [128, 512]) for k in range(2)]
                nw = 0
                nw2 = 0
                ng = 0
                npp = 0
                cgs = [(c0, min(512, NSC - c0)) for c0 in range(0, NSC, 512)]
                for Yo in yo:
                    kb.op("pool", lambda e: e.memset(Yo[:], 0.0), W=[Yo])
                ngc = [0]

                def gather_stage(ex):
                        for ci in range(NCH):
                            G = xg[ngc[0] % 2]
                            ngc[0] += 1
                            kb.op("pool", lambda e: e.indirect_dma_start(out=G[:, :], out_offset=None, in_=d["H"][:, :],
                                                                          in_offset=bass.IndirectOffsetOnAxis(ap=idxT[:, ci, ex:ex + 1], axis=0)),
                                  R=[d["H"], idxT], W=[G], dma=True)
                            for k in range(8):
                                p = ptr[k // 4]
                                kb.op("pe", lambda e: e.transpose(p[:, (k % 4) * 128:(k % 4 + 1) * 128], G[:, k * 128:(k + 1) * 128], self.ident[:]),
                                      R=[G, self.ident], P=[p])
                            for jj in range(2):
                                if jj == 0:
                                    kb.op("act", lambda e: e.activation(xsT[:, 0:4, offs[ci]:offs[ci] + chunks[ci][2]], ptr[0][:].rearrange("p (a b) -> p a b", a=4)[:, :, 0:chunks[ci][2]], AF.Copy),
                                          R=[ptr[0]], P=[xsT])
                                else:
                                    kb.op("dve", lambda e: e.tensor_copy(xsT[:, 4:8, offs[ci]:offs[ci] + chunks[ci][2]], ptr[1][:].rearrange("p (a b) -> p a b", a=4)[:, :, 0:chunks[ci][2]]),
                                          R=[ptr[1]], P=[xsT])

                gather_stage(0)
                for ex in range(NE):
                    for fb in range(NFB):
                        W1, W3 = w1[nw % 2], w3[nw % 2]
                        nw += 1
                        kb.dma("sp", W1, W1[:], i["ffn_w1"], i["ffn_w1"][l, ex, :, fb * FB:(fb + 1) * FB].rearrange("(k p) f -> p k f", p=128), part=False)
                        kb.dma("sp", W3, W3[:], i["ffn_w3"], i["ffn_w3"][l, ex, :, fb * FB:(fb + 1) * FB].rearrange("(k p) f -> p k f", p=128), part=False)
                        for fc in range(FB // 128):
                            f = fb * (FB // 128) + fc
                            for (c0, cw) in cgs:
                                P1, P3 = p1[npp % 2], p3[npp % 2]
                                npp += 1
                                for k in range(8):
                                    kb.op("pe", lambda e: e.matmul(P1[:, 0:cw], W1[:, k, fc * 128:(fc + 1) * 128], xsT[:, k, c0:c0 + cw], start=(k == 0), stop=(k == 7)),
                                          R=[W1, xsT], P=[P1])
                                for k in range(8):
                                    kb.op("pe", lambda e: e.matmul(P3[:, 0:cw], W3[:, k, fc * 128:(fc + 1) * 128], xsT[:, k, c0:c0 + cw], start=(k == 0), stop=(k == 7)),
                                          R=[W3, xsT], P=[P3])
                                kb.op("act", lambda e: e.activation(sg[:, 0:cw], P1[:, 0:cw], AF.Silu), R=[P1], W=[sg])
                                kb.op("dve", lambda e: e.tensor_tensor(hT[:, f, c0:c0 + cw], sg[:, 0:cw], P3[:, 0:cw], op=ALU.mult), R=[sg, P3], P=[hT])
                    if ex + 1 < NE:
                        gather_stage(ex + 1)
                    for db in range(4):
                        W2 = w2[nw2 % 2]
                        nw2 += 1
                        kb.dma("sp", W2, W2[:], i["ffn_w2"], i["ffn_w2"][l, ex, :, db * 256:(db + 1) * 256].rearrange("(f p) n -> p f n", p=128), part=False)
                        for ci in range(NCH):
                            PY = py[(db * NCH + ci) % 2]
                            Yo = yo[ci]
                            cn = chunks[ci][2]
                            for f in range(NF):
                                kb.op("pe", lambda e: e.matmul(PY[0:cn, 0:256], hT[:, f, offs[ci]:offs[ci] + cn], W2[:, f, :], start=(f == 0), stop=(f == NF - 1)),
                                      R=[hT, W2], P=[PY])
                            g = gm[chunks[ci][0]]
                            kb.op("dve", lambda e: e.scalar_tensor_tensor(Yo[0:cn, db * 256:(db + 1) * 256], PY[0:cn, 0:256], gateT[0:cn, ci, ex:ex + 1], g[0:cn, db * 256:(db + 1) * 256],
                                                                          op0=ALU.mult, op1=ALU.mult), R=[PY, gateT, g], P=[Yo])
                            if db == 3:
                                Xr = xr[ci % 2]
                                kb.op("pool", lambda e: e.indirect_dma_start(out=Xr[:, :], out_offset=None, in_=d["X"][:, :],
                                                                              in_offset=bass.IndirectOffsetOnAxis(ap=idxT[:, ci, ex:ex + 1], axis=0)),
                                      R=[d["X"], idxT], W=[Xr], dma=True)
                                kb.op("dve", lambda e: e.tensor_tensor(Xr[:], Xr[:], Yo[:], op=ALU.add), R=[Xr, Yo], W=[Xr])
                                kb.op("pool", lambda e: e.indirect_dma_start(out=d["X"][:, :], out_offset=bass.IndirectOffsetOnAxis(ap=idxT[:, ci, ex:ex + 1], axis=0),
                                                                              in_=Xr[:, :], in_offset=None),
                                      R=[Xr, idxT], W=[d["X"]], dma=True)

    def build(self):
        kb = self.kb
        self.declare()
        with contextlib.ExitStack() as gst:
            self.gst = gst
            self.phase_init()
            cnt = {0: 0, 1: 0, 2: 0}
            dbg = self.cfg.get("dbg", 99)
            for l, kind in enumerate(self.kinds):
                last = l == self.L - 1
                j = cnt[kind]
                cnt[kind] += 1
                if dbg >= 1:
                    self.phase_norm(l, 0, 1, list(range(self.NT)), self.d["H"])
                if kind == 0:
                    if dbg >= 2:
                        self.phase_qkv_A(l, j)
                    if dbg >= 3:
                        self.phase_attn_A(last)
                    if dbg >= 4:
                        self.phase_out_proj(l, self.i["a_w_o"], self.i["a_w_o"][j], last)
                elif kind == 1:
                    if dbg >= 2:
                        self.phase_qkv_B(l, j)
                    if dbg >= 3:
                        self.phase_attn_B(j, last)
                    if dbg >= 4:
                        self.phase_out_proj(l, self.i["b_w_o"], self.i["b_w_o"][j], last)
                else:
                    if dbg >= 2:
                        self.phase_rwkv(l, j, last, dbg)
                if dbg >= 5:
                    self.phase_moe(l, last)
            self.phase_norm(self.L - 1, 0, 0, list(range(self.NTL)), self.out, final=True)
            kb.finish([self.out])
        return kb.nc


def _constants(cfg):
    S, C = cfg["S"], cfg["C"]
    T = S + C
    k = {}
    k["k_ident"] = np.eye(128, dtype=np.float32)
    rows = np.repeat(np.arange(S // 64), 64).astype(np.float32)
    cols = np.tile(np.arange(64), S // 64).astype(np.float32)

    def tab(hd):
        nf = hd // 4
        inv = (np.float32(10000.0) ** (-np.arange(nf, dtype=np.float32) / np.float32(nf))).astype(np.float32)
        ang = np.concatenate([rows[:, None] * inv, cols[:, None] * inv], axis=-1).astype(np.float32)
        return np.concatenate([np.cos(ang), np.sin(ang)], axis=-1).astype(np.float32)

    k["k_ropeA"] = tab(128)
    k["k_ropeB"] = tab(64)
    jj, ii = np.meshgrid(np.arange(128), np.arange(128), indexing="ij")
    k["k_tri"] = np.stack([(jj >= ii), (jj <= ii)]).astype(np.float32)
    bd = np.zeros((48, 8, 64), np.float32)
    for r in range(48):
        bd[r, r % 8, :] = 1.0
    k["k_bd"] = bd.reshape(48, 512)
    sel = np.zeros((2, 64, 64, 2, 8), np.float32)
    for hg in range(2):
        for t in range(64):
            sel[hg, t, t, hg, :] = 1.0
    k["k_sel"] = sel.reshape(128, 64, 16)
    k["k_pad"] = (T + np.arange(128, dtype=np.float32)).reshape(128, 1)
    k["k_zero"] = np.zeros((128, D), np.float32)
    return k


_NC_CACHE = {}


def kernel(**inputs):
    cfg = CFG
    n = cfg["ncores"]
    key = repr(cfg)
    if key not in _NC_CACHE:
        _NC_CACHE[key] = Prog(cfg).build()
    nc = _NC_CACHE[key]
    consts = _constants(cfg)
    f = lambda a: np.ascontiguousarray(np.asarray(a, dtype=np.float32))
    shared = {}
    for name, a in inputs.items():
        if name in ("x", "c", "ctx"):
            continue
        a = f(a)
        if name in ("c_ctx", "final_norm"):
            a = a.reshape(1, D)
        if name == "c_r_k":
            a = a.reshape(a.shape[0], 2, D)
        shared[name] = a
    shared.update(consts)
    x, c, ctx = f(inputs["x"]), f(inputs["c"]), f(inputs["ctx"])
    in_maps = []
    for b in range(n):
        m = dict(shared)
        m["x"] = x[b]
        m["c"] = c[b:b + 1]
        m["ctx"] = ctx[b]
        in_maps.append(m)
    res = run_bass_kernel_spmd(nc, in_maps, core_ids=list(range(n)))
    return np.stack([np.asarray(r["out"]) for r in res.results], axis=0).astype(np.float32)
```

```python
import contextlib
import numpy as np
import concourse.bass as bass
import concourse.mybir as mybir
from concourse.bass_utils import run_bass_kernel_spmd

F32 = mybir.dt.float32
U32 = mybir.dt.uint32
I32 = mybir.dt.int32
ALU = mybir.AluOpType
AF = mybir.ActivationFunctionType
AX = mybir.AxisListType

D = 1024
NE = 16
EPS = 1e-6
CFG = dict(S=4096, C=256, FF=2048, kinds=[0, 1, 2, 0], ncores=8)


class Buf:
    __slots__ = ("t", "w", "r", "pr", "name")

    def __init__(self, t, name):
        self.t = t
        self.name = name
        self.w = []
        self.r = []
        self.pr = []

    def __getitem__(self, k):
        return self.t[k]


class KB:
    SEM_LIMIT = 30000
    N_DMA_SLOTS = 48

    def __init__(self):
        self.nc = bass.Bass("TRN2", target_bir_lowering=False)
        nc = self.nc
        self.es = contextlib.ExitStack()
        self.engs = {"pe": nc.tensor, "dve": nc.vector, "act": nc.scalar, "pool": nc.gpsimd, "sp": nc.sync}
        self.sem = {}
        self.cnt = {}
        self.nsem = 0
        for e in self.engs:
            self._new_sem(e)
        self.seen = {e: {} for e in self.engs}
        self.slots = []
        for i in range(self.N_DMA_SLOTS):
            s = self.es.enter_context(nc.semaphore(f"dq{i}"))
            self.slots.append([s, 0])
        self.slot_i = 0
        self.uid = 0
        self.ninst = 0

    def _new_sem(self, e):
        self.nsem += 1
        self.sem[e] = self.es.enter_context(self.nc.semaphore(f"s_{e}_{self.nsem}"))
        self.cnt[e] = 0

    def inp(self, name, shape, dtype=F32):
        return Buf(self.nc.dram_tensor(name, list(shape), dtype, kind="ExternalInput").ap(), name)

    def outp(self, name, shape, dtype=F32):
        return Buf(self.nc.dram_tensor(name, list(shape), dtype, kind="ExternalOutput").ap(), name)

    def dram(self, name, shape, dtype=F32):
        return Buf(self.nc.dram_tensor(name, list(shape), dtype, kind="Internal").ap(), name)

    def sb(self, stack, name, shape, dtype=F32):
        self.uid += 1
        t = stack.enter_context(self.nc.sbuf_tensor(f"{name}_{self.uid}", list(shape), dtype))
        return Buf(t, name)

    def ps(self, stack, name, shape, dtype=F32):
        self.uid += 1
        t = stack.enter_context(self.nc.psum_tensor(f"{name}_{self.uid}", list(shape), dtype))
        return Buf(t, name)

    def _wait(self, eng, sem, val):
        d = self.seen[eng]
        k = id(sem)
        if d.get(k, (None, 0))[1] >= val:
            return
        self.engs[eng].wait_ge(sem, val)
        d[k] = (sem, val)
        self.ninst += 1

    @staticmethod
    def _add(lst, ev):
        out = [x for x in lst if not (x[0] is ev[0] and x[1] <= ev[1])]
        out.append(ev)
        return out

    def op(self, eng, fn, R=(), W=(), P=(), dma=False):
        for b in R:
            for ev in b.w:
                self._wait(eng, ev[0], ev[1])
        for b in W:
            for ev in b.w + b.r + b.pr:
                self._wait(eng, ev[0], ev[1])
        for b in P:
            for ev in b.r + b.pr:
                self._wait(eng, ev[0], ev[1])
            for ev in b.w:
                if ev[2]:
                    self._wait(eng, ev[0], ev[1])
        if dma:
            slot = self.slots[self.slot_i]
            self.slot_i = (self.slot_i + 1) % len(self.slots)
            if slot[1] > 0:
                self._wait(eng, slot[0], slot[1])
            inst = fn(self.engs[eng])
            slot[1] += 16
            inst.then_inc(slot[0], 16)
            sem, val = slot[0], slot[1]
        else:
            if self.cnt[eng] >= self.SEM_LIMIT:
                self._new_sem(eng)
            inst = fn(self.engs[eng])
            self.cnt[eng] += 1
            sem, val = self.sem[eng], self.cnt[eng]
            inst.then_inc(sem, 1)
        self.ninst += 1
        for b in R:
            b.r = self._add(b.r, (sem, val))
        for b in W:
            b.w = [(sem, val, True)]
            b.r = []
            b.pr = []
        for b in P:
            if b.r:
                b.pr = b.r
                b.r = []
                b.w = []
            b.w = self._add(b.w, (sem, val, False))
        return (sem, val)

    def dma(self, eng, out_b, out_ap, in_b, in_ap, part=True, **kw):
        def f(e):
            return e.dma_start(out=out_ap, in_=in_ap, **kw)
        if part:
            return self.op(eng, f, R=[in_b], P=[out_b], dma=True)
        return self.op(eng, f, R=[in_b], W=[out_b], dma=True)

    def barrier(self):
        for e in self.engs:
            for e2 in self.engs:
                if e2 != e and self.cnt[e2] > 0:
                    self._wait(e, self.sem[e2], self.cnt[e2])
            for slot in self.slots:
                if slot[1] > 0:
                    self._wait(e, slot[0], slot[1])

    def finish(self, bufs):
        for b in bufs:
            for ev in b.w:
                self._wait("sp", ev[0], ev[1])
        self.es.close()
        return self.nc


def bc_last(ap, n):
    return ap.unsqueeze(2).to_broadcast([ap.shape[0], ap.shape[1], n])


def bc_mid(ap, n):
    return ap.unsqueeze(1).to_broadcast([ap.shape[0], n, ap.shape[1]])


class Prog:
    def __init__(self, cfg):
        self.cfg = cfg
        self.S, self.C, self.FF = cfg["S"], cfg["C"], cfg["FF"]
        self.T = self.S + self.C
        self.kinds = cfg["kinds"]
        self.L = len(self.kinds)
        self.NT = self.T // 128
        self.NTL = self.S // 128
        self.kb = KB()

    def rstd(self, st, ss, n, width, tmp):
        kb = self.kb
        kb.op("dve", lambda e: e.tensor_scalar(tmp[:, 0:n], ss[:, 0:n], 1.0 / width, EPS, op0=ALU.mult, op1=ALU.add),
              R=[ss], W=[tmp])
        kb.op("act", lambda e: e.activation(tmp[:, 0:n], tmp[:, 0:n], AF.Sqrt), R=[tmp], W=[tmp])
        kb.op("dve", lambda e: e.reciprocal(tmp[:, 0:n], tmp[:, 0:n]), R=[tmp], W=[tmp])

    def load_bc(self, dst, src_b, row_ap, plus_one=False):
        kb = self.kb
        kb.dma("sp", dst, dst[:], src_b, row_ap.partition_broadcast(128), part=False)
        if plus_one:
            kb.op("pool", lambda e: e.tensor_scalar(dst[:], dst[:], 1.0, None, op0=ALU.add), R=[dst], W=[dst])

    def declare(self):
        kb, S, C, T, L, FF = self.kb, self.S, self.C, self.T, self.L, self.FF
        nA, nB, nC = max(1, self.kinds.count(0)), max(1, self.kinds.count(1)), max(1, self.kinds.count(2))
        i = {}
        i["x"] = kb.inp("x", [S, D])
        i["c"] = kb.inp("c", [1, D])
        i["ctx"] = kb.inp("ctx", [C, D])
        i["c_ctx"] = kb.inp("c_ctx", [1, D])
        i["mod_w"] = kb.inp("mod_w", [L, D, 6 * D])
        i["mod_b"] = kb.inp("mod_b", [L, 6 * D])
        i["a_w_qkv"] = kb.inp("a_w_qkv", [nA, D, 1536])
        i["a_w_o"] = kb.inp("a_w_o", [nA, D, D])
        i["a_q_norm"] = kb.inp("a_q_norm", [nA, 128])
        i["a_k_norm"] = kb.inp("a_k_norm", [nA, 128])
        i["b_w_qkv"] = kb.inp("b_w_qkv", [nB, D, 1536])
        i["b_w_o"] = kb.inp("b_w_o", [nB, D, D])
        i["b_sink"] = kb.inp("b_sink", [nB, 16])
        i["c_mu"] = kb.inp("c_mu", [nC, 6, D])
        i["c_w_rkv"] = kb.inp("c_w_rkv", [nC, 3, D, D])
        i["c_w_o"] = kb.inp("c_w_o", [nC, D, D])
        i["c_w0"] = kb.inp("c_w0", [nC, 2, D])
        i["c_w1"] = kb.inp("c_w1", [nC, 2, D, 64])
        i["c_w2"] = kb.inp("c_w2", [nC, 2, 64, D])
        i["c_a0"] = kb.inp("c_a0", [nC, 2, D])
        i["c_a1"] = kb.inp("c_a1", [nC, 2, D, 64])
        i["c_a2"] = kb.inp("c_a2", [nC, 2, 64, D])
        i["c_g1"] = kb.inp("c_g1", [nC, D, 128])
        i["c_g2"] = kb.inp("c_g2", [nC, 128, D])
        i["c_k_k"] = kb.inp("c_k_k", [nC, D])
        i["c_k_a"] = kb.inp("c_k_a", [nC, D])
        i["c_r_k"] = kb.inp("c_r_k", [nC, 2, D])
        i["c_ln_w"] = kb.inp("c_ln_w", [nC, D])
        i["c_ln_b"] = kb.inp("c_ln_b", [nC, D])
        i["router_w"] = kb.inp("router_w", [L, D, NE])
        i["ffn_w1"] = kb.inp("ffn_w1", [L, NE, D, FF])
        i["ffn_w3"] = kb.inp("ffn_w3", [L, NE, D, FF])
        i["ffn_w2"] = kb.inp("ffn_w2", [L, NE, FF, D])
        i["final_norm"] = kb.inp("final_norm", [1, D])
        i["k_ident"] = kb.inp("k_ident", [128, 128])
        i["k_ropeA"] = kb.inp("k_ropeA", [S, 128])
        i["k_ropeB"] = kb.inp("k_ropeB", [S, 64])
        i["k_tri"] = kb.inp("k_tri", [2, 128, 128])
        i["k_bd"] = kb.inp("k_bd", [48, 512])
        i["k_sel"] = kb.inp("k_sel", [128, 64, 16])
        i["k_pad"] = kb.inp("k_pad", [128, 1])
        i["k_zero"] = kb.inp("k_zero", [128, D])
        self.i = i
        self.out = kb.outp("out", [S, D])
        d = {}
        d["X"] = kb.dram("X", [T + 128, D])
        d["H"] = kb.dram("H", [T + 128, D])
        d["MODV"] = kb.dram("MODV", [L, 2, 6 * D])
        d["QT"] = kb.dram("QT", [20, 128, T])
        d["V"] = kb.dram("V", [T, 256])
        d["O"] = kb.dram("O", [T, D])
        if 2 in self.kinds:
            d["XJ"] = kb.dram("XJ", [6, T, D])
            for nm in ("RR", "KK", "VV", "GG", "AV"):
                d[nm] = kb.dram(nm, [T, D])
            for nm in ("DEC", "AA", "BB", "KD", "RP"):
                d[nm] = kb.dram(nm, [2, T, D])
            d["SC"] = kb.dram("SC", [T, 80])
            self.QSB = 1024
            d["QS"] = [[kb.dram(f"QS{a}_{b}", [min(2048, T - b * 2048), 32 * 512]) for b in range((T + 2047) // 2048)] for a in range(2)]
        self.d = d

    def phase_init(self):
        kb, i, d, S, C, T = self.kb, self.i, self.d, self.S, self.C, self.T
        with contextlib.ExitStack() as st:
            kb.barrier()
            self.ident = kb.sb(self.gst, "ident", [128, 128])
            kb.dma("sp", self.ident, self.ident[:], i["k_ident"], i["k_ident"][:], part=False)
            tl = [kb.sb(st, f"cp{j}", [128, D]) for j in range(2)]
            for t in range(self.NT):
                b = tl[t % 2]
                src = i["x"][t * 128:(t + 1) * 128, :] if t < self.NTL else i["ctx"][(t - self.NTL) * 128:(t - self.NTL + 1) * 128, :]
                kb.dma("sp", b, b[:], i["x"] if t < self.NTL else i["ctx"], src, part=False)
                kb.dma("sp", d["X"], d["X"][t * 128:(t + 1) * 128, :], b, b[:])
            z = kb.sb(st, "z", [128, D])
            kb.dma("sp", z, z[:], i["k_zero"], i["k_zero"][:], part=False)
            kb.dma("sp", d["X"], d["X"][T:T + 128, :], z, z[:])
            kb.dma("sp", d["H"], d["H"][T:T + 128, :], z, z[:])
            cT = kb.sb(st, "cT", [128, 8, 2])
            kb.dma("sp", cT, cT[:, :, 0], i["c"], i["c"][0, :].rearrange("(k p) -> p k", p=128), allow_slow_non_contiguous=True)
            kb.dma("sp", cT, cT[:, :, 1], i["c_ctx"], i["c_ctx"][0, :].rearrange("(k p) -> p k", p=128), allow_slow_non_contiguous=True)
            kb.op("act", lambda e: e.activation(cT[:], cT[:], AF.Silu), R=[cT], W=[cT])
            wts = [kb.sb(st, f"mw{j}", [128, 8, 512]) for j in range(2)]
            mb = kb.sb(st, "mb", [2, 6 * D])
            mv = kb.sb(st, "mv", [2, 6 * D])
            pm = [kb.ps(st, f"pm{j}", [2, 512]) for j in range(2)]
            n = 0
            for l in range(self.L):
                kb.dma("sp", mb, mb[:], i["mod_b"], i["mod_b"][l, :].partition_broadcast(2), part=False)
                for nb in range(12):
                    wt = wts[n % 2]
                    p = pm[n % 2]
                    n += 1
                    kb.dma("sp", wt, wt[:], i["mod_w"], i["mod_w"][l, :, nb * 512:(nb + 1) * 512].rearrange("(k p) n -> p k n", p=128), part=False)
                    for k in range(8):
                        kb.op("pe", lambda e: e.matmul(p[:], cT[:, k, :], wt[:, k, :], start=(k == 0), stop=(k == 7)), R=[cT, wt], P=[p])
                    kb.op("dve", lambda e: e.tensor_tensor(mv[:, nb * 512:(nb + 1) * 512], p[:], mb[:, nb * 512:(nb + 1) * 512], op=ALU.add),
                          R=[p, mb], P=[mv])
                kb.dma("sp", d["MODV"], d["MODV"][l], mv, mv[:])

    def mod_row(self, l, row, idx):
        return self.d["MODV"][l, row, idx * D:(idx + 1) * D]

    def phase_norm(self, l, sh_idx, sc_idx, tiles, dst, final=False):
        kb, d = self.kb, self.d
        with contextlib.ExitStack() as st:
            kb.barrier()
            if final:
                fw = kb.sb(st, "fw", [128, D])
                self.load_bc(fw, self.i["final_norm"], self.i["final_norm"][0, :])
                bc = {0: (None, fw)}
            else:
                bc = {}
                for row in (0, 1):
                    sh = kb.sb(st, f"sh{row}", [128, D])
                    sc = kb.sb(st, f"sc{row}", [128, D])
                    self.load_bc(sh, d["MODV"], self.mod_row(l, row, sh_idx))
                    self.load_bc(sc, d["MODV"], self.mod_row(l, row, sc_idx), plus_one=True)
                    bc[row] = (sh, sc)
            xt = [kb.sb(st, f"nx{j}", [128, D]) for j in range(2)]
            ht = [kb.sb(st, f"nh{j}", [128, D]) for j in range(2)]
            sq = kb.sb(st, "nsq", [128, D])
            ss = kb.sb(st, "nss", [128, 1])
            rs = kb.sb(st, "nrs", [128, 1])
            if tiles:
                kb.dma("sp", xt[0], xt[0][:], d["X"], d["X"][tiles[0] * 128:(tiles[0] + 1) * 128, :], part=False)
            for n, t in enumerate(tiles):
                X, Hh = xt[n % 2], ht[n % 2]
                sh, sc = bc[0 if t < self.NTL else 1]
                if n + 1 < len(tiles):
                    tn = tiles[n + 1]
                    Xn = xt[(n + 1) % 2]
                    kb.dma("sp", Xn, Xn[:], d["X"], d["X"][tn * 128:(tn + 1) * 128, :], part=False)
                kb.op("act", lambda e: e.activation(sq[:], X[:], AF.Square, accum_out=ss[:]), R=[X], W=[sq, ss])
                self.rstd(st, ss, 1, D, rs)
                kb.op("dve", lambda e: e.scalar_tensor_tensor(Hh[:], X[:], rs[:, 0:1], sc[:], op0=ALU.mult, op1=ALU.mult),
                      R=[X, rs, sc], W=[Hh])
                if sh is not None:
                    kb.op("pool", lambda e: e.tensor_tensor(Hh[:], Hh[:], sh[:], op=ALU.add), R=[Hh, sh], W=[Hh])
                kb.dma("sp", dst, dst[t * 128:(t + 1) * 128, :], Hh, Hh[:])

    def phase_linear(self, st, src, w_b, w_ap, N, tiles, post, wt=None, src_ap=None):
        kb = self.kb
        nb = (N + 511) // 512
        if wt is None:
            wt = kb.sb(st, "lw", [128, 8, N])
            kb.dma("sp", wt, wt[:], w_b, w_ap.rearrange("(k p) n -> p k n", p=128), part=False)
        xin = [kb.sb(st, f"lx{j}", [128, D]) for j in range(2)]
        xT = kb.sb(st, "lxT", [128, 8, 128])
        ys = [kb.sb(st, f"ly{j}", [128, N]) for j in range(2)]
        ptr = [kb.ps(st, f"lpt{j}", [128, 512]) for j in range(2)]
        po = [kb.ps(st, f"lpo{j}", [128, 512]) for j in range(nb)]
        sap = src_ap if src_ap is not None else src.t
        if tiles:
            kb.dma("sp", xin[0], xin[0][:], src, sap[tiles[0] * 128:(tiles[0] + 1) * 128, :], part=False)
        for n, t in enumerate(tiles):
            X, Y = xin[n % 2], ys[n % 2]
            if n + 1 < len(tiles):
                tn = tiles[n + 1]
                Xn = xin[(n + 1) % 2]
                kb.dma("sp", Xn, Xn[:], src, sap[tn * 128:(tn + 1) * 128, :], part=False)
            self.transpose8(X, xT, ptr)
            for j in range(nb):
                w = min(512, N - j * 512)
                for k in range(8):
                    kb.op("pe", lambda e: e.matmul(po[j][:, 0:w], xT[:, k, :], wt[:, k, j * 512:j * 512 + w], start=(k == 0), stop=(k == 7)),
                          R=[xT, wt], P=[po[j]])
                eng = "act" if j % 2 == 0 else "dve"
                if eng == "act":
                    kb.op("act", lambda e: e.activation(Y[:, j * 512:j * 512 + w], po[j][:, 0:w], AF.Copy), R=[po[j]], P=[Y])
                else:
                    kb.op("dve", lambda e: e.tensor_copy(Y[:, j * 512:j * 512 + w], po[j][:, 0:w]), R=[po[j]], P=[Y])
            post(n, t, Y)

    def transpose8(self, X, xT, ptr, nblk=8):
        kb = self.kb
        for k in range(nblk):
            p = ptr[k // 4]
            kb.op("pe", lambda e: e.transpose(p[:, (k % 4) * 128:(k % 4 + 1) * 128], X[:, k * 128:(k + 1) * 128], self.ident[:]),
                  R=[X, self.ident], P=[p])
        for j in range((nblk + 3) // 4):
            nn = min(4, nblk - j * 4)
            if j % 2 == 0:
                kb.op("act", lambda e: e.activation(xT[:, j * 4:j * 4 + nn, :], ptr[j][:, 0:nn * 128].rearrange("p (a b) -> p a b", a=nn), AF.Copy),
                      R=[ptr[j]], P=[xT])
            else:
                kb.op("dve", lambda e: e.tensor_copy(xT[:, j * 4:j * 4 + nn, :], ptr[j][:, 0:nn * 128].rearrange("p (a b) -> p a b", a=nn)),
                      R=[ptr[j]], P=[xT])

    def phase_qkv_A(self, l, j):
        kb, i, d = self.kb, self.i, self.d
        with contextlib.ExitStack() as st:
            kb.barrier()
            gq = kb.sb(st, "gq", [128, 128])
            gk = kb.sb(st, "gk", [128, 128])
            self.load_bc(gq, i["a_q_norm"], i["a_q_norm"][j, :])
            self.load_bc(gk, i["a_k_norm"], i["a_k_norm"][j, :])
            sq = kb.sb(st, "asq", [128, 1280])
            ss = kb.sb(st, "ass", [128, 10])
            rs = kb.sb(st, "ars", [128, 10])
            cs = [kb.sb(st, f"acs{k}", [128, 128]) for k in range(2)]
            t1 = kb.sb(st, "at1", [128, 10, 64])
            t2 = kb.sb(st, "at2", [128, 10, 64])
            yr = kb.sb(st, "ayr", [128, 1280])
            qT = kb.sb(st, "aqT", [128, 10, 128])
            ptr = [kb.ps(st, f"apt{k}", [128, 512]) for k in range(3)]

            def post(n, t, Y):
                lat = t < self.NTL
                yv = Y[:, 0:1280].rearrange("p (h e) -> p h e", h=10)
                kb.op("pool", lambda e: e.tensor_tensor(sq[:], Y[:, 0:1280], Y[:, 0:1280], op=ALU.mult), R=[Y], W=[sq])
                kb.op("dve", lambda e: e.tensor_reduce(ss[:], sq[:].rearrange("p (h e) -> p h e", h=10), axis=AX.X, op=ALU.add), R=[sq], W=[ss])
                self.rstd(st, ss, 10, 128, rs)
                kb.op("dve", lambda e: e.tensor_tensor(yv, yv, bc_last(rs[:, :], 128), op=ALU.mult), R=[Y, rs], W=[Y])
                kb.op("pool", lambda e: e.tensor_tensor(yv[:, 0:8, :], yv[:, 0:8, :], bc_mid(gq[:, :], 8), op=ALU.mult), R=[Y, gq], W=[Y])
                kb.op("pool", lambda e: e.tensor_tensor(yv[:, 8:10, :], yv[:, 8:10, :], bc_mid(gk[:, :], 2), op=ALU.mult), R=[Y, gk], W=[Y])
                if lat:
                    c_ = cs[n % 2]
                    kb.dma("sp", c_, c_[:], i["k_ropeA"], i["k_ropeA"][t * 128:(t + 1) * 128, :], part=False)
                    x1, x2 = yv[:, :, 0:64], yv[:, :, 64:128]
                    yrv = yr[:].rearrange("p (h e) -> p h e", h=10)
                    cosb, sinb = bc_mid(c_[:, 0:64], 10), bc_mid(c_[:, 64:128], 10)
                    kb.op("dve", lambda e: e.tensor_tensor(t1[:], x1, cosb, op=ALU.mult), R=[Y, c_], W=[t1])
                    kb.op("pool", lambda e: e.tensor_tensor(t2[:], x2, sinb, op=ALU.mult), R=[Y, c_], W=[t2])
                    kb.op("dve", lambda e: e.tensor_tensor(yrv[:, :, 0:64], t1[:], t2[:], op=ALU.subtract), R=[t1, t2], W=[yr])
                    kb.op("pool", lambda e: e.tensor_tensor(t1[:], x1, sinb, op=ALU.mult), R=[Y, c_], W=[t1])
                    kb.op("dve", lambda e: e.tensor_tensor(t2[:], x2, cosb, op=ALU.mult), R=[Y, c_], W=[t2])
                    kb.op("pool", lambda e: e.tensor_tensor(yrv[:, :, 64:128], t1[:], t2[:], op=ALU.add), R=[t1, t2], P=[yr])
                    src = yr
                else:
                    src = Y
                self.transpose8(src, qT, ptr, nblk=10)
                kb.dma("sp", d["QT"], d["QT"][0:10, :, t * 128:(t + 1) * 128].rearrange("h p t -> p h t"), qT, qT[:])
                kb.dma("sp", d["V"], d["V"][t * 128:(t + 1) * 128, :], Y, Y[:, 1280:1536])

            self.phase_linear(st, d["H"], i["a_w_qkv"], i["a_w_qkv"][j], 1536, list(range(self.NT)), post)

    def phase_attn_A(self, last):
        kb, d, T, S, C = self.kb, self.d, self.T, self.S, self.C
        scale = 128 ** -0.5
        with contextlib.ExitStack() as st:
            kb.barrier()
            kT = kb.sb(st, "kT", [128, T])
            vx = kb.sb(st, "vx", [128, self.NT, 129])
            qb = [kb.sb(st, f"qb{k}", [128, 512]) for k in range(2)]
            pT = [kb.sb(st, f"pT{k}", [128, 512]) for k in range(3)]
            ob = [kb.sb(st, f"ob{k}", [128, 128]) for k in range(2)]
            rc = kb.sb(st, "rc", [128, 1])
            psS = [kb.ps(st, f"psS{k}", [128, 512]) for k in range(2)]
            psO = [kb.ps(st, f"psO{k}", [128, 512]) for k in range(4)]
            nq = 0
            nch = 0
            for g in range(2):
                kb.dma("sp", kT, kT[:], d["QT"], d["QT"][8 + g], part=False)
                kb.op("pool", lambda e: e.memset(vx[:, :, 128:129], 1.0), W=[vx])
                kb.dma("sp", vx, vx[:, :, 0:128], d["V"], d["V"][:, g * 128:(g + 1) * 128].rearrange("(c p) e -> p c e", p=128))
                blocks = [(q0, min(512, S - q0), list(range(self.NT))) for q0 in range(0, S, 512)]
                if not last:
                    blocks += [(q0, min(512, T - q0), list(range(self.NTL, self.NT))) for q0 in range(S, T, 512)]
                items = [(h, q0, nqq, chunks) for h in range(4 * g, 4 * g + 4) for (q0, nqq, chunks) in blocks]
                kb.dma("sp", qb[nq % 2], qb[nq % 2][:, 0:items[0][2]], d["QT"], d["QT"][items[0][0], :, items[0][1]:items[0][1] + items[0][2]], part=False)
                for ii, (h, q0, nqq, chunks) in enumerate(items):
                    if True:
                        Q = qb[nq % 2]
                        nq += 1
                        if ii + 1 < len(items):
                            hn_, q0n, nqn, _ = items[ii + 1]
                            Qn = qb[nq % 2]
                            kb.dma("sp", Qn, Qn[:, 0:nqn], d["QT"], d["QT"][hn_, :, q0n:q0n + nqn], part=False)
                        nsub = nqq // 128
                        def emit_s(ci):
                            pS = psS[(nch + ci) % 2]
                            ch = chunks[ci]
                            kb.op("pe", lambda e: e.matmul(pS[:, 0:nqq], kT[:, ch * 128:(ch + 1) * 128], Q[:, 0:nqq], start=True, stop=True),
                                  R=[kT, Q], P=[pS])
                        emit_s(0)
                        for ci, ch in enumerate(chunks):
                            pS = psS[(nch + ci) % 2]
                            P_ = pT[(nch + ci) % 3]
                            if ci + 1 < len(chunks):
                                emit_s(ci + 1)
                            kb.op("act", lambda e: e.activation(P_[:, 0:nqq], pS[:, 0:nqq], AF.Exp, scale=scale), R=[pS], W=[P_])
                            for s in range(nsub):
                                kb.op("pe", lambda e: e.matmul(psO[s][:, 0:129], P_[:, s * 128:(s + 1) * 128], vx[:, ch, :],
                                                               start=(ci == 0), stop=(ci == len(chunks) - 1)), R=[P_, vx], P=[psO[s]])
                        nch += len(chunks)
                        for s in range(nsub):
                            O = ob[s % 2]
                            kb.op("dve", lambda e: e.reciprocal(rc[:], psO[s][:, 128:129]), R=[psO[s]], W=[rc])
                            kb.op("dve", lambda e: e.tensor_scalar(O[:], psO[s][:, 0:128], rc[:, 0:1], None, op0=ALU.mult), R=[psO[s], rc], W=[O])
                            r0 = q0 + s * 128
                            kb.dma("sp", d["O"], d["O"][r0:r0 + 128, h * 128:(h + 1) * 128], O, O[:])

    def phase_out_proj(self, l, w_b, w_ap, last, src=None):
        kb, d = self.kb, self.d
        src = src if src is not None else d["O"]
        with contextlib.ExitStack() as st:
            kb.barrier()
            gt = {}
            for row in ((0,) if last else (0, 1)):
                g = kb.sb(st, f"og{row}", [128, D])
                self.load_bc(g, d["MODV"], self.mod_row(l, row, 2))
                gt[row] = g
            xt = [kb.sb(st, f"ox{k}", [128, D]) for k in range(2)]

            def post(n, t, Y):
                Xt = xt[n % 2]
                g = gt[0 if t < self.NTL else 1]
                kb.dma("sp", Xt, Xt[:], d["X"], d["X"][t * 128:(t + 1) * 128, :], part=False)
                kb.op("pool", lambda e: e.tensor_tensor(Y[:], Y[:], g[:], op=ALU.mult), R=[Y, g], W=[Y])
                kb.op("dve", lambda e: e.tensor_tensor(Xt[:], Xt[:], Y[:], op=ALU.add), R=[Xt, Y], W=[Xt])
                kb.dma("sp", d["X"], d["X"][t * 128:(t + 1) * 128, :], Xt, Xt[:])

            tiles = list(range(self.NTL if last else self.NT))
            self.phase_linear(st, src, w_b, w_ap, D, tiles, post)


    def phase_qkv_B(self, l, j):
        kb, i, d = self.kb, self.i, self.d
        with contextlib.ExitStack() as st:
            kb.barrier()
            cs = [kb.sb(st, f"bcs{k}", [128, 64]) for k in range(2)]
            t1 = kb.sb(st, "bt1", [128, 20, 32])
            t2 = kb.sb(st, "bt2", [128, 20, 32])
            yr = kb.sb(st, "byr", [128, 1280])
            qT = kb.sb(st, "bqT", [128, 10, 128])
            ptr = [kb.ps(st, f"bpt{k}", [128, 512]) for k in range(3)]

            def post(n, t, Y):
                lat = t < self.NTL
                if lat:
                    yv = Y[:, 0:1280].rearrange("p (h e) -> p h e", h=20)
                    c_ = cs[n % 2]
                    kb.dma("sp", c_, c_[:], i["k_ropeB"], i["k_ropeB"][t * 128:(t + 1) * 128, :], part=False)
                    x1, x2 = yv[:, :, 0:32], yv[:, :, 32:64]
                    yrv = yr[:].rearrange("p (h e) -> p h e", h=20)
                    cosb, sinb = bc_mid(c_[:, 0:32], 20), bc_mid(c_[:, 32:64], 20)
                    kb.op("dve", lambda e: e.tensor_tensor(t1[:], x1, cosb, op=ALU.mult), R=[Y, c_], W=[t1])
                    kb.op("pool", lambda e: e.tensor_tensor(t2[:], x2, sinb, op=ALU.mult), R=[Y, c_], W=[t2])
                    kb.op("dve", lambda e: e.tensor_tensor(yrv[:, :, 0:32], t1[:], t2[:], op=ALU.subtract), R=[t1, t2], W=[yr])
                    kb.op("pool", lambda e: e.tensor_tensor(t1[:], x1, sinb, op=ALU.mult), R=[Y, c_], W=[t1])
                    kb.op("dve", lambda e: e.tensor_tensor(t2[:], x2, cosb, op=ALU.mult), R=[Y, c_], W=[t2])
                    kb.op("pool", lambda e: e.tensor_tensor(yrv[:, :, 32:64], t1[:], t2[:], op=ALU.add), R=[t1, t2], P=[yr])
                    src = yr
                else:
                    src = Y
                self.transpose8(src, qT, ptr, nblk=10)
                for half in range(2):
                    dst = d["QT"][:, 0:64, t * 128:(t + 1) * 128].rearrange("(b two) p t -> two p b t", two=2)[half]
                    kb.dma("sp", d["QT"], dst, qT, qT[half * 64:(half + 1) * 64, :, :])
                kb.dma("sp", d["V"], d["V"][t * 128:(t + 1) * 128, :], Y, Y[:, 1280:1536])

            self.phase_linear(st, d["H"], i["b_w_qkv"], i["b_w_qkv"][j], 1536, list(range(self.NT)), post)

    def phase_attn_B(self, j, last):
        kb, i, d, T, S, C = self.kb, self.i, self.d, self.T, self.S, self.C
        scale = 64 ** -0.5
        NT, NTL = self.NT, self.NTL
        with contextlib.ExitStack() as st:
            kb.barrier()
            kT = kb.sb(st, "bkT", [64, 4, T])
            vx = kb.sb(st, "bvx", [128, NT, 4, 65])
            es = kb.sb(st, "bes", [128, 16])
            tri = kb.sb(st, "btri", [128, 2, 128])
            kb.dma("sp", kT, kT[:], d["QT"], d["QT"][16:20, 0:64, :].rearrange("g p t -> p g t"), part=False)
            kb.op("pool", lambda e: e.memset(vx[:, :, :, 64:65], 1.0), W=[vx])
            for g in range(4):
                kb.dma("sp", vx, vx[:, :, g, 0:64], d["V"], d["V"][:, g * 64:(g + 1) * 64].rearrange("(c p) e -> p c e", p=128))
            self.load_bc(es, i["b_sink"], i["b_sink"][j, :])
            kb.op("act", lambda e: e.activation(es[:], es[:], AF.Exp), R=[es], W=[es])
            kb.dma("sp", tri, tri[:], i["k_tri"], i["k_tri"][:].rearrange("a p q -> p a q"), part=False)
            qa = [kb.sb(st, f"bqa{k}", [64, 16, 128]) for k in range(2)]
            pT = [kb.sb(st, f"bpT{k}", [128, 512]) for k in range(3)]
            ot = [kb.sb(st, f"bot{k}", [128, D]) for k in range(2)]
            den = kb.sb(st, "bden", [128, 1])
            psS = [kb.ps(st, f"bpsS{k}", [128, 512]) for k in range(2)]
            psO = [kb.ps(st, f"bpsO{k}", [128, 512]) for k in range(4)]
            nch = 0
            tiles = list(range(NTL if last else NT))
            for n, t in enumerate(tiles):
                if t < NTL:
                    chunks = []
                    if t - 1 >= 0:
                        chunks.append((t - 1, 0))
                    chunks.append((t, None))
                    if t + 1 < NTL:
                        chunks.append((t + 1, 1))
                    chunks += [(c_, None) for c_ in range(NTL, NT)]
                else:
                    chunks = [(c_, None) for c_ in range(NTL, NT)]
                Q = qa[n % 2]
                O = ot[n % 2]
                kb.dma("sp", Q, Q[:], d["QT"], d["QT"][0:16, 0:64, t * 128:(t + 1) * 128].rearrange("h p t -> p h t"), part=False)
                for g in range(4):
                    def emit_s(ci):
                        pS = psS[(nch + ci) % 2]
                        ch = chunks[ci][0]
                        kb.op("pe", lambda e: e.matmul(pS[:], kT[:, g, ch * 128:(ch + 1) * 128], Q[:, 4 * g:4 * g + 4, :].rearrange("p h t -> p (h t)"),
                                                       start=True, stop=True), R=[kT, Q], P=[pS])
                    emit_s(0)
                    for ci, (ch, mk) in enumerate(chunks):
                        pS = psS[(nch + ci) % 2]
                        P_ = pT[(nch + ci) % 3]
                        if ci + 1 < len(chunks):
                            emit_s(ci + 1)
                        kb.op("act", lambda e: e.activation(P_[:], pS[:], AF.Exp, scale=scale), R=[pS], W=[P_])
                        if mk is not None:
                            pv = P_[:].rearrange("p (h t) -> p h t", h=4)
                            kb.op("pool", lambda e: e.tensor_tensor(pv, pv, bc_mid(tri[:, mk, :], 4), op=ALU.mult), R=[P_, tri], W=[P_])
                        for s in range(4):
                            kb.op("pe", lambda e: e.matmul(psO[s][:, 0:65], P_[:, s * 128:(s + 1) * 128], vx[:, ch, g, :],
                                                           start=(ci == 0), stop=(ci == len(chunks) - 1)), R=[P_, vx], P=[psO[s]])
                    nch += len(chunks)
                    for s in range(4):
                        h = 4 * g + s
                        kb.op("dve", lambda e: e.tensor_tensor(den[:], psO[s][:, 64:65], es[:, h:h + 1], op=ALU.add), R=[psO[s], es], W=[den])
                        kb.op("dve", lambda e: e.reciprocal(den[:], den[:]), R=[den], W=[den])
                        kb.op("dve", lambda e: e.tensor_scalar(O[:, h * 64:(h + 1) * 64], psO[s][:, 0:64], den[:, 0:1], None, op0=ALU.mult),
                              R=[psO[s], den], P=[O])
                kb.dma("sp", d["O"], d["O"][t * 128:(t + 1) * 128, :], O, O[:])


    def phase_rwkv(self, l, j, last, dbg=99):
        kb, i, d, T, S, C = self.kb, self.i, self.d, self.T, self.S, self.C
        NT, NTL = self.NT, self.NTL
        alltiles = list(range(NT))
        with contextlib.ExitStack() as st:
            kb.barrier()
            mu = [kb.sb(st, f"mu{k}", [128, D]) for k in range(6)]
            for k in range(6):
                self.load_bc(mu[k], i["c_mu"], i["c_mu"][j, k, :])
            hh = [kb.sb(st, f"rh{k}", [128, D]) for k in range(2)]
            hp = [kb.sb(st, f"rhp{k}", [128, D]) for k in range(2)]
            hn = [kb.sb(st, f"rhn{k}", [128, D]) for k in range(2)]
            xx = kb.sb(st, "rxx", [128, D])
            xj = [kb.sb(st, f"rxj{k}", [128, D]) for k in range(3)]
            nx = 0
            for n, t in enumerate(alltiles):
                t0 = t * 128
                Hc, Hp, Hn = hh[n % 2], hp[n % 2], hn[n % 2]
                kb.dma("sp", Hc, Hc[:], d["H"], d["H"][t0:t0 + 128, :], part=False)
                if t == 0 or t == NTL:
                    kb.dma("sp", Hp, Hp[0:1, :], i["k_zero"], i["k_zero"][0:1, :], part=False)
                    kb.dma("sp", Hp, Hp[1:128, :], d["H"], d["H"][t0:t0 + 127, :])
                else:
                    kb.dma("sp", Hp, Hp[:], d["H"], d["H"][t0 - 1:t0 + 127, :], part=False)
                if t == NTL - 1 or t == NT - 1:
                    kb.dma("sp", Hn, Hn[127:128, :], i["k_zero"], i["k_zero"][0:1, :], part=False)
                    kb.dma("sp", Hn, Hn[0:127, :], d["H"], d["H"][t0 + 1:t0 + 128, :])
                else:
                    kb.dma("sp", Hn, Hn[:], d["H"], d["H"][t0 + 1:t0 + 129, :], part=False)
                kb.op("pool", lambda e: e.tensor_tensor(Hp[:], Hp[:], Hn[:], op=ALU.add), R=[Hp, Hn], W=[Hp])
                kb.op("dve", lambda e: e.scalar_tensor_tensor(xx[:], Hp[:], 0.5, Hc[:], op0=ALU.mult, op1=ALU.subtract), R=[Hp, Hc], W=[xx])
                for k in range(6):
                    X = xj[nx % 3]
                    nx += 1
                    e1, e2 = ("pool", "dve") if k % 2 == 0 else ("dve", "pool")
                    kb.op(e1, lambda e: e.tensor_tensor(X[:], xx[:], mu[k][:], op=ALU.mult), R=[xx, mu[k]], W=[X])
                    kb.op(e2, lambda e: e.tensor_tensor(X[:], X[:], Hc[:], op=ALU.add), R=[X, Hc], W=[X])
                    kb.dma("sp", d["XJ"], d["XJ"][k, t0:t0 + 128, :], X, X[:])
        for (src_k, wi, dst) in ((0, 0, "RR"), (2, 1, "KK"), (3, 2, "VV")):
            with contextlib.ExitStack() as st:
                kb.barrier()

                def post(n, t, Y, dst=dst):
                    kb.dma("sp", d[dst], d[dst][t * 128:(t + 1) * 128, :], Y, Y[:])

                self.phase_linear(st, d["XJ"], i["c_w_rkv"], i["c_w_rkv"][j, wi], D, alltiles, post, src_ap=d["XJ"][src_k])
        def lora(src_k, fill_w1, act, fill_w2, nsplit, epi):
            with contextlib.ExitStack() as st:
                kb.barrier()
                wt = kb.sb(st, "lrw1", [128, 8, 128])
                fill_w1(wt)
                w2t = kb.sb(st, "lrw2", [128, D])
                fill_w2(w2t)
                yT = kb.sb(st, "lryT", [128, 128])
                ot = [kb.sb(st, f"lro{k}", [128, D]) for k in range(2)]
                tmp = kb.sb(st, "lrtmp", [128, 512])
                pt = kb.ps(st, "lrpt", [128, 512])
                po = [kb.ps(st, f"lrpo{k}", [128, 512]) for k in range(2)]
                cnt = [0]

                def post(n, t, Y):
                    if act is not None:
                        kb.op("act", lambda e: e.activation(Y[:], Y[:], act), R=[Y], W=[Y])
                    kb.op("pe", lambda e: e.transpose(pt[:, 0:128], Y[:, 0:128], self.ident[:]), R=[Y, self.ident], W=[pt])
                    kb.op("dve", lambda e: e.tensor_copy(yT[:], pt[:, 0:128]), R=[pt], W=[yT])
                    kk = 128 // nsplit
                    for dd in range(nsplit):
                        O = ot[cnt[0] % 2]
                        cnt[0] += 1
                        for nb in range(2):
                            kb.op("pe", lambda e: e.matmul(po[nb][:], yT[dd * kk:(dd + 1) * kk, :], w2t[dd * kk:(dd + 1) * kk, nb * 512:(nb + 1) * 512],
                                                           start=True, stop=True), R=[yT, w2t], P=[po[nb]])
                            epi(dd, nb, po[nb], O, tmp)
                        self_dst = epi.dst(dd)
                        kb.dma("sp", self_dst[0], self_dst[1][t * 128:(t + 1) * 128, :], O, O[:])

                self.phase_linear(st, d["XJ"], None, None, 128, alltiles, post, wt=wt, src_ap=d["XJ"][src_k])

        def fill2(arr):
            def f(wt):
                for dd in range(2):
                    kb.dma("sp", wt, wt[:, :, dd * 64:(dd + 1) * 64], i[arr], i[arr][j, dd].rearrange("(k p) n -> p k n", p=128))
            return f

        def fill2b(arr):
            def f(w2t):
                for dd in range(2):
                    kb.dma("sp", w2t, w2t[dd * 64:(dd + 1) * 64, :], i[arr], i[arr][j, dd])
            return f

        with contextlib.ExitStack() as stb:
            kb.barrier()
            bw = [kb.sb(stb, f"bw{k}", [128, D]) for k in range(2)]
            ba = [kb.sb(stb, f"ba{k}", [128, D]) for k in range(2)]
            for dd in range(2):
                self.load_bc(bw[dd], i["c_w0"], i["c_w0"][j, dd, :])
                self.load_bc(ba[dd], i["c_a0"], i["c_a0"][j, dd, :])

            def epi_w(dd, nb, ps, O, tmp):
                sl = slice(nb * 512, (nb + 1) * 512)
                kb.op("dve", lambda e: e.tensor_tensor(tmp[:], ps[:], bw[dd][:, sl], op=ALU.add), R=[ps, bw[dd]], W=[tmp])
                kb.op("act", lambda e: e.activation(tmp[:], tmp[:], AF.Sigmoid), R=[tmp], W=[tmp])
                kb.op("act", lambda e: e.activation(O[:, sl], tmp[:], AF.Exp, scale=-float(np.exp(-0.5))), R=[tmp], P=[O])
            epi_w.dst = lambda dd: (d["DEC"], d["DEC"][dd])

            def epi_a(dd, nb, ps, O, tmp):
                sl = slice(nb * 512, (nb + 1) * 512)
                kb.op("dve", lambda e: e.tensor_tensor(tmp[:], ps[:], ba[dd][:, sl], op=ALU.add), R=[ps, ba[dd]], W=[tmp])
                kb.op("act", lambda e: e.activation(O[:, sl], tmp[:], AF.Sigmoid), R=[tmp], P=[O])
            epi_a.dst = lambda dd: (d["AA"], d["AA"][dd])

            def epi_g(dd, nb, ps, O, tmp):
                sl = slice(nb * 512, (nb + 1) * 512)
                kb.op("act", lambda e: e.activation(O[:, sl], ps[:], AF.Copy), R=[ps], P=[O])
            epi_g.dst = lambda dd: (d["GG"], d["GG"])

            lora(1, fill2("c_w1"), AF.Tanh, fill2b("c_w2"), 2, epi_w)
            lora(4, fill2("c_a1"), None, fill2b("c_a2"), 2, epi_a)
            lora(5, lambda wt: kb.dma("sp", wt, wt[:], i["c_g1"], i["c_g1"][j].rearrange("(k p) n -> p k n", p=128), part=False),
                 AF.Sigmoid, lambda w2t: kb.dma("sp", w2t, w2t[:], i["c_g2"], i["c_g2"][j], part=False), 1, epi_g)
        if dbg == 2:
            return
        with contextlib.ExitStack() as st:
            kb.barrier()
            kkb = kb.sb(st, "kkb", [128, D])
            kab = kb.sb(st, "kab", [128, D])
            rkb = [kb.sb(st, f"rkb{k}", [128, D]) for k in range(2)]
            self.load_bc(kkb, i["c_k_k"], i["c_k_k"][j, :])
            self.load_bc(kab, i["c_k_a"], i["c_k_a"][j, :])
            for dd in range(2):
                self.load_bc(rkb[dd], i["c_r_k"], i["c_r_k"][j, dd, :])
            tk = kb.sb(st, "tk", [128, D])
            tr = kb.sb(st, "tr", [128, D])
            ta = [kb.sb(st, f"ta{k}", [128, D]) for k in range(2)]
            tw = [kb.sb(st, f"tw{k}", [128, D]) for k in range(2)]
            kk = kb.sb(st, "kk", [128, D])
            t1 = kb.sb(st, "t1", [128, D])
            t2 = kb.sb(st, "t2", [128, D])
            t3 = kb.sb(st, "t3", [128, D])
            t4 = kb.sb(st, "t4", [128, D])
            ss = kb.sb(st, "ss", [128, 16])
            rs = kb.sb(st, "rs", [128, 16])
            sc = kb.sb(st, "sc", [128, 96])
            hv = lambda b: b[:].rearrange("p (h e) -> p h e", h=16)
            for n, t in enumerate(alltiles):
                rows = slice(t * 128, (t + 1) * 128)
                kb.dma("sp", tk, tk[:], d["KK"], d["KK"][rows, :], part=False)
                kb.dma("sp", tr, tr[:], d["RR"], d["RR"][rows, :], part=False)
                for dd in range(2):
                    kb.dma("sp", ta[dd], ta[dd][:], d["AA"], d["AA"][dd, rows, :], part=False)
                    kb.dma("sp", tw[dd], tw[dd][:], d["DEC"], d["DEC"][dd, rows, :], part=False)
                kb.op("pool", lambda e: e.tensor_tensor(kk[:], tk[:], kkb[:], op=ALU.mult), R=[tk, kkb], W=[kk])
                kb.op("dve", lambda e: e.tensor_tensor(t1[:], kk[:], kk[:], op=ALU.mult), R=[kk], W=[t1])
                kb.op("dve", lambda e: e.tensor_reduce(ss[:], hv(t1), axis=AX.X, op=ALU.add), R=[t1], W=[ss])
                kb.op("dve", lambda e: e.tensor_scalar(rs[:], ss[:], 1e-24, None, op0=ALU.max), R=[ss], W=[rs])
                kb.op("act", lambda e: e.activation(rs[:], rs[:], AF.Sqrt), R=[rs], W=[rs])
                kb.op("dve", lambda e: e.reciprocal(rs[:], rs[:]), R=[rs], W=[rs])
                kb.op("dve", lambda e: e.tensor_tensor(hv(kk), hv(kk), bc_last(rs[:, :], 64), op=ALU.mult), R=[kk, rs], W=[kk])
                kb.op("act", lambda e: e.activation(t1[:], kk[:], AF.Copy, scale=-1.0), R=[kk], W=[t1])
                kb.dma("sp", d["AV"], d["AV"][rows, :], t1, t1[:])
                for dd in range(2):
                    kb.op("dve", lambda e: e.scalar_tensor_tensor(t2[:], ta[dd][:], -1.0, kab[:], op0=ALU.add, op1=ALU.mult), R=[ta[dd], kab], W=[t2])
                    kb.op("dve", lambda e: e.scalar_tensor_tensor(t2[:], t2[:], 1.0, tk[:], op0=ALU.add, op1=ALU.mult), R=[t2, tk], W=[t2])
                    kb.dma("sp", d["KD"], d["KD"][dd, rows, :], t2, t2[:])
                    kb.op("pool", lambda e: e.tensor_tensor(t3[:], kk[:], ta[dd][:], op=ALU.mult), R=[kk, ta[dd]], W=[t3])
                    kb.dma("sp", d["BB"], d["BB"][dd, rows, :], t3, t3[:])
                    kb.op("pool", lambda e: e.tensor_tensor(t4[:], tr[:], tw[dd][:], op=ALU.mult), R=[tr, tw[dd]], W=[t4])
                    kb.dma("sp", d["RP"], d["RP"][dd, rows, :], t4, t4[:])
                    kb.op("pool", lambda e: e.tensor_tensor(t3[:], t3[:], tr[:], op=ALU.mult), R=[t3, tr], W=[t3])
                    kb.op("dve", lambda e: e.tensor_reduce(sc[:, dd * 16:(dd + 1) * 16], hv(t3), axis=AX.X, op=ALU.add), R=[t3], P=[sc])
                    kb.op("pool", lambda e: e.tensor_tensor(t2[:], t2[:], tr[:], op=ALU.mult), R=[t2, tr], W=[t2])
                    kb.op("dve", lambda e: e.tensor_reduce(sc[:, 32 + dd * 16:32 + (dd + 1) * 16], hv(t2), axis=AX.X, op=ALU.add), R=[t2], P=[sc])
                    kb.op("pool", lambda e: e.tensor_tensor(t2[:], t2[:], rkb[dd][:], op=ALU.mult), R=[t2, rkb[dd]], W=[t2])
                    kb.op("dve", lambda e: e.tensor_reduce(sc[:, 64 + dd * 16:64 + (dd + 1) * 16], hv(t2), axis=AX.X, op=ALU.add), R=[t2], P=[sc])
                kb.op("dve", lambda e: e.tensor_tensor(sc[:, 64:80], sc[:, 64:80], sc[:, 80:96], op=ALU.add), R=[sc], W=[sc])
                kb.dma("sp", d["SC"], d["SC"][rows, :], sc, sc[:, 0:80])
        if dbg == 3:
            return
        with contextlib.ExitStack() as st:
            kb.barrier()
            bd = kb.sb(st, "bd", [48, 512])
            kb.dma("sp", bd, bd[:], i["k_bd"], i["k_bd"][:], part=False)
            selc = kb.sb(st, "selc", [128, 64, 16])
            kb.dma("sp", selc, selc[:], i["k_sel"], i["k_sel"][:], part=False)

            def scan_dir(dd):
                svt = [kb.sb(st, f"svt{dd}{k}", [128, 512]) for k in range(2)]
                vr = kb.sb(st, f"VR{dd}", [128, 512])
                lt = kb.sb(st, f"LT{dd}", [128, 64, 32])
                wt_ = kb.sb(st, f"WT{dd}", [128, 64, 8])
                l2 = kb.sb(st, f"L2{dd}", [48, 64, 128])
                Mr = [kb.sb(st, f"Mr{dd}{k}", [48, 512]) for k in range(2)]
                stg = [kb.sb(st, f"stg{dd}{a}", [64, D]) for a in range(3)]
                po1 = kb.ps(st, f"spo1{dd}", [128, 512])
                pu = kb.ps(st, f"spu{dd}", [128, 512])
                pt = kb.ps(st, f"spt{dd}", [128, 512])
                kb.op("pool", lambda e: e.memset(lt[:], 0.0), W=[lt])
                kb.op("pool", lambda e: e.memset(l2[:], 0.0), W=[l2])
                kb.op("pool", lambda e: e.memset(svt[0][:], 0.0), W=[svt[0]])
                yield
                p = 0
                nstep = 0
                nchunk = 0
                for (lo, hi) in [(S, T), (0, S)]:
                    c0s = list(range(lo, hi, 64))
                    if dd == 1:
                        c0s = c0s[::-1]
                    for c0 in c0s:
                        if dbg == 41 and nchunk >= 1:
                            break
                        nchunk += 1
                        srcs = [(d["AV"], d["AV"][c0:c0 + 64, :]), (d["RP"], d["RP"][dd, c0:c0 + 64, :]), (d["DEC"], d["DEC"][dd, c0:c0 + 64, :])]
                        for a, (sb_, sap) in enumerate(srcs):
                            kb.dma("sp", stg[a], stg[a][:], sb_, sap, part=False)
                        for (r0, arr) in ((0, "BB"), (32, "KD")):
                            for hg in range(2):
                                kb.dma("sp", l2, l2[r0 + hg * 8:r0 + hg * 8 + 8, :, hg * 64:(hg + 1) * 64], d[arr],
                                       d[arr][dd, c0:c0 + 64, :].rearrange("t (hp hg k) -> hg hp t k", hg=2, k=64)[hg])
                        for hg in range(2):
                            kb.dma("sp", vr, vr[hg * 64:(hg + 1) * 64, :].rearrange("t (hp v) -> t hp v", v=64), d["VV"],
                                   d["VV"][c0:c0 + 64, :].rearrange("t (hp hg v) -> hg t hp v", hg=2, v=64)[hg])
                        for a in range(3):
                            G = stg[a]
                            for hp in range(8):
                                kb.op("pe", lambda e: e.transpose(pt[:, hp * 64:(hp + 1) * 64], G[:, hp * 128:(hp + 1) * 128], self.ident[0:64, 0:64]),
                                      R=[G, self.ident], P=[pt])
                            if a < 2:
                                kb.op("act", lambda e: e.activation(lt[0:64, :, a * 16:a * 16 + 8], pt[0:64, :].rearrange("p (h t) -> p t h", h=8), AF.Copy),
                                      R=[pt], P=[lt])
                                kb.op("dve", lambda e: e.tensor_copy(lt[64:128, :, a * 16 + 8:a * 16 + 16], pt[64:128, :].rearrange("p (h t) -> p t h", h=8)),
                                      R=[pt], P=[lt])
                            else:
                                kb.op("act", lambda e: e.activation(wt_[:], pt[:].rearrange("p (h t) -> p t h", h=8), AF.Copy), R=[pt], W=[wt_])
                        yield
                        order = range(64) if dd == 0 else range(63, -1, -1)
                        for tl in order:
                            cur, nxt = p, 1 - p
                            M = Mr[nstep % 2]
                            nstep += 1
                            kb.op("pe", lambda e: e.matmul(po1[0:32, :], lt[:, tl, :], svt[cur][:], start=True, stop=True), R=[lt, svt[cur]], W=[po1])
                            kb.op("pe", lambda e: e.matmul(po1[32:48, :], selc[:, tl, :], vr[:], start=True, stop=True), R=[selc, vr], P=[po1])
                            kb.op("pool", lambda e: e.tensor_tensor(svt[nxt][:].rearrange("p (h e) -> p h e", h=8),
                                                                     svt[cur][:].rearrange("p (h e) -> p h e", h=8),
                                                                     bc_last(wt_[:, tl, :], 64), op=ALU.mult), R=[svt[cur], wt_], W=[svt[nxt]])
                            yield
                            kb.op("dve", lambda e: e.tensor_tensor(M[:], po1[0:48, :], bd[:], op=ALU.mult), R=[po1, bd], W=[M])
                            yield
                            kb.op("pe", lambda e: e.matmul(pu[:], l2[:, tl, :], M[:], start=True, stop=True), R=[l2, M], W=[pu])
                            yield
                            kb.op("dve", lambda e: e.tensor_tensor(svt[nxt][:], svt[nxt][:], pu[:], op=ALU.add), R=[svt[nxt], pu], W=[svt[nxt]])
                            tok = c0 + tl
                            qsb = d["QS"][dd][tok // 2048]
                            kb.dma("sp", qsb, qsb[tok % 2048, :].rearrange("(p n) -> p n", p=32), M, M[0:32, :])
                            p = nxt
                            yield

            gens = [scan_dir(0), scan_dir(1)]
            while gens:
                for g in list(gens):
                    try:
                        next(g)
                    except StopIteration:
                        gens.remove(g)
        if dbg in (4, 41):
            return
        tiles = list(range(NTL if last else NT))
        with contextlib.ExitStack() as st:
            kb.barrier()
            lw = kb.sb(st, "lnw", [128, D])
            lb = kb.sb(st, "lnb", [128, D])
            self.load_bc(lw, i["c_ln_w"], i["c_ln_w"][j, :])
            self.load_bc(lb, i["c_ln_b"], i["c_ln_b"][j, :])
            sa = [kb.sb(st, f"osa{k}", [128, D]) for k in range(2)]
            qq = [kb.sb(st, f"oqq{k}", [128, D]) for k in range(2)]
            tv = kb.sb(st, "otv", [128, D])
            tg = kb.sb(st, "otg", [128, D])
            sc = kb.sb(st, "osc", [128, 80])
            o = kb.sb(st, "oo", [128, D])
            t1 = kb.sb(st, "ot1", [128, D])
            m1 = kb.sb(st, "om1", [128, 16])
            m2 = kb.sb(st, "om2", [128, 16])
            hv = lambda b: b[:].rearrange("p (h e) -> p h e", h=16)
            for n, t in enumerate(tiles):
                rows = slice(t * 128, (t + 1) * 128)
                for dd in range(2):
                    qsb = d["QS"][dd][(t * 128) // 2048]
                    r0 = (t * 128) % 2048
                    qv = qsb[r0:r0 + 128, :].rearrange("t (ty g q v) -> t ty g q v", ty=2, g=2, q=64, v=64)
                    for ty, dstb in ((0, sa[dd]), (1, qq[dd])):
                        dv = dstb[:].rearrange("p (hp hg v) -> p hg hp v", hg=2, v=64)
                        for hg in range(2):
                            kb.dma("sp", dstb, dv[:, hg], qsb, qv[:, ty, hg, ::9, :])
                kb.dma("sp", tv, tv[:], d["VV"], d["VV"][rows, :], part=False)
                kb.dma("sp", tg, tg[:], d["GG"], d["GG"][rows, :], part=False)
                kb.dma("sp", sc, sc[:], d["SC"], d["SC"][rows, :], part=False)
                kb.op("pool", lambda e: e.tensor_tensor(o[:], qq[0][:], qq[1][:], op=ALU.add), R=[qq[0], qq[1]], W=[o])
                for dd in range(2):
                    kb.op("dve", lambda e: e.tensor_tensor(hv(t1), hv(sa[dd]), bc_last(sc[:, dd * 16:(dd + 1) * 16], 64), op=ALU.mult), R=[sa[dd], sc], W=[t1])
                    kb.op("pool", lambda e: e.tensor_tensor(o[:], o[:], t1[:], op=ALU.add), R=[o, t1], W=[o])
                kb.op("dve", lambda e: e.tensor_tensor(m1[:], sc[:, 32:48], sc[:, 48:64], op=ALU.add), R=[sc], W=[m1])
                kb.op("dve", lambda e: e.tensor_tensor(hv(t1), hv(tv), bc_last(m1[:, :], 64), op=ALU.mult), R=[tv, m1], W=[t1])
                kb.op("pool", lambda e: e.tensor_tensor(o[:], o[:], t1[:], op=ALU.add), R=[o, t1], W=[o])
                kb.op("dve", lambda e: e.tensor_reduce(m1[:], hv(o), axis=AX.X, op=ALU.add), R=[o], W=[m1])
                kb.op("dve", lambda e: e.tensor_scalar(m1[:], m1[:], -1.0 / 64, None, op0=ALU.mult), R=[m1], W=[m1])
                kb.op("dve", lambda e: e.tensor_tensor(hv(o), hv(o), bc_last(m1[:, :], 64), op=ALU.add), R=[o, m1], W=[o])
                kb.op("pool", lambda e: e.tensor_tensor(t1[:], o[:], o[:], op=ALU.mult), R=[o], W=[t1])
                kb.op("dve", lambda e: e.tensor_reduce(m2[:], hv(t1), axis=AX.X, op=ALU.add), R=[t1], W=[m2])
                kb.op("dve", lambda e: e.tensor_scalar(m2[:], m2[:], 1.0 / 64, 64 * 1e-5, op0=ALU.mult, op1=ALU.add), R=[m2], W=[m2])
                kb.op("act", lambda e: e.activation(m2[:], m2[:], AF.Sqrt), R=[m2], W=[m2])
                kb.op("dve", lambda e: e.reciprocal(m2[:], m2[:]), R=[m2], W=[m2])
                kb.op("dve", lambda e: e.tensor_tensor(hv(o), hv(o), bc_last(m2[:, :], 64), op=ALU.mult), R=[o, m2], W=[o])
                kb.op("pool", lambda e: e.tensor_tensor(o[:], o[:], lw[:], op=ALU.mult), R=[o, lw], W=[o])
                kb.op("dve", lambda e: e.tensor_tensor(o[:], o[:], lb[:], op=ALU.add), R=[o, lb], W=[o])
                kb.op("dve", lambda e: e.tensor_tensor(hv(t1), hv(tv), bc_last(sc[:, 64:80], 64), op=ALU.mult), R=[tv, sc], W=[t1])
                kb.op("pool", lambda e: e.tensor_tensor(o[:], o[:], t1[:], op=ALU.add), R=[o, t1], W=[o])
                kb.op("dve", lambda e: e.tensor_tensor(o[:], o[:], tg[:], op=ALU.mult), R=[o, tg], W=[o])
                kb.dma("sp", d["O"], d["O"][rows, :], o, o[:])
        self.phase_out_proj(l, i["c_w_o"], i["c_w_o"][j], last)

    def phase_moe(self, l, last):
        kb, i, d, S, C, T, FF = self.kb, self.i, self.d, self.S, self.C, self.T, self.FF
        capl, capc = 2 * S // NE, 2 * C // NE
        tiles = list(range(self.NTL if last else self.NT))
        self.phase_norm(l, 3, 4, tiles, d["H"])
        chunks = [(0, s0, min(128, capl - s0)) for s0 in range(0, capl, 128)]
        if not last:
            chunks += [(1, s0, min(128, capc - s0)) for s0 in range(0, capc, 128)]
        NCH = len(chunks)
        NSL = NCH * 128
        offs = [sum(c[2] for c in chunks[:k]) for k in range(NCH)]
        NSC = sum(c[2] for c in chunks)
        with contextlib.ExitStack() as sto:
            kb.barrier()
            idxT = kb.sb(sto, "idxT", [128, NCH, NE], I32)
            gateT = kb.sb(sto, "gateT", [128, NCH, NE])
            with contextlib.ExitStack() as st:
                kb.barrier()
                affT = kb.sb(st, "affT", [NE, T])
                mx = kb.sb(st, "rmx", [128, 1])
                sm = kb.sb(st, "rsm", [128, 1])
                ex = kb.sb(st, "rex", [128, NE])
                pa = kb.ps(st, "rpa", [NE, 128])

                def post(n, t, Y):
                    kb.op("dve", lambda e: e.tensor_reduce(mx[:], Y[:, 0:NE], axis=AX.X, op=ALU.max), R=[Y], W=[mx])
                    kb.op("dve", lambda e: e.tensor_scalar(mx[:], mx[:], -1.0, None, op0=ALU.mult), R=[mx], W=[mx])
                    kb.op("act", lambda e: e.activation(ex[:], Y[:, 0:NE], AF.Exp, bias=mx[:, 0:1], scale=1.0, accum_out=sm[:]), R=[Y, mx], W=[ex, sm])
                    kb.op("dve", lambda e: e.reciprocal(sm[:], sm[:]), R=[sm], W=[sm])
                    kb.op("dve", lambda e: e.tensor_scalar(ex[:], ex[:], sm[:, 0:1], None, op0=ALU.mult), R=[ex, sm], W=[ex])
                    kb.op("pe", lambda e: e.transpose(pa[:], ex[:], self.ident[:]), R=[ex, self.ident], W=[pa])
                    kb.op("dve", lambda e: e.tensor_copy(affT[:, t * 128:(t + 1) * 128], pa[:]), R=[pa], P=[affT])

                with contextlib.ExitStack() as st2:
                    kb.barrier()
                    self.phase_linear(st2, d["H"], i["router_w"], i["router_w"][l], NE, tiles, post)
                kb.barrier()
                vals = kb.sb(st, "tvals", [NE, NSL])
                idxf = kb.sb(st, "tidxf", [NE, NSL])
                idxu = kb.sb(st, "tidxu", [NE, 8], U32)
                kb.op("dve", lambda e: e.memset(vals[:], 0.0), W=[vals])
                kb.op("dve", lambda e: e.memset(idxf[:], 0.0), W=[idxf])
                for (isc, s0, cnt), ci in zip(chunks, range(NCH)):
                    lo, hi = (S, T) if isc else (0, S)
                    for it in range(cnt // 8):
                        col = ci * 128 + it * 8
                        work = affT[:, lo:hi]
                        kb.op("dve", lambda e: e.max(out=vals[:, col:col + 8], in_=work), R=[affT], P=[vals])
                        kb.op("dve", lambda e: e.max_index(out=idxu[:], in_max=vals[:, col:col + 8], in_values=work), R=[affT, vals], W=[idxu])
                        kb.op("dve", lambda e: e.tensor_copy(idxf[:, col:col + 8], idxu[:]), R=[idxu], P=[idxf])
                        kb.op("dve", lambda e: e.match_replace(out=work, in_to_replace=vals[:, col:col + 8], in_values=work, imm_value=-1.0),
                              R=[vals], W=[affT])
                    if isc:
                        kb.op("dve", lambda e: e.tensor_scalar(idxf[:, ci * 128:ci * 128 + cnt], idxf[:, ci * 128:ci * 128 + cnt], float(S), None, op0=ALU.add),
                              R=[idxf], W=[idxf])
                padf = kb.sb(st, "padf", [128, 1])
                kb.dma("sp", padf, padf[:], i["k_pad"], i["k_pad"][:], part=False)
                idxTf = kb.sb(st, "idxTf", [128, NCH, NE])
                kb.op("dve", lambda e: e.tensor_copy(idxTf[:].rearrange("p a b -> p (a b)"), padf[:, 0:1].to_broadcast([128, NCH * NE])), R=[padf], W=[idxTf])
                kb.op("dve", lambda e: e.memset(gateT[:], 0.0), W=[gateT])
                pt = kb.ps(st, "tpt", [128, 2 * NE])
                for (isc, s0, cnt), ci in zip(chunks, range(NCH)):
                    kb.op("pe", lambda e: e.transpose(pt[:, 0:NE], idxf[:, ci * 128:(ci + 1) * 128], self.ident[0:NE, 0:NE]), R=[idxf, self.ident], P=[pt])
                    kb.op("pe", lambda e: e.transpose(pt[:, NE:2 * NE], vals[:, ci * 128:(ci + 1) * 128], self.ident[0:NE, 0:NE]), R=[vals, self.ident], P=[pt])
                    kb.op("dve", lambda e: e.tensor_copy(idxTf[0:cnt, ci, :], pt[0:cnt, 0:NE]), R=[pt], W=[idxTf])
                    kb.op("dve", lambda e: e.tensor_copy(gateT[0:cnt, ci, :], pt[0:cnt, NE:2 * NE]), R=[pt], W=[gateT])
                kb.op("dve", lambda e: e.tensor_copy(idxT[:], idxTf[:]), R=[idxTf], W=[idxT])
            if self.cfg.get('dbg', 99) == 5:
                return
            with contextlib.ExitStack() as st:
                kb.barrier()
                NF = FF // 128
                FB = min(256, FF)
                NFB = FF // FB
                gm = {}
                for row in ((0,) if last else (0, 1)):
                    g = kb.sb(st, f"mg{row}", [128, D])
                    self.load_bc(g, d["MODV"], self.mod_row(l, row, 5))
                    gm[row] = g
                xg = [kb.sb(st, f"xg{k}", [128, D]) for k in range(2)]
                xsT = kb.sb(st, "xsT", [128, 8, NSC])
                hT = kb.sb(st, "hT", [128, NF, NSC])
                w1 = [kb.sb(st, f"w1_{k}", [128, 8, FB]) for k in range(2)]
                w3 = [kb.sb(st, f"w3_{k}", [128, 8, FB]) for k in range(2)]
                w2 = [kb.sb(st, f"w2_{k}", [128, NF, 256]) for k in range(2)]
                sg = kb.sb(st, "sg", [128, 512])
                yo = [kb.sb(st, f"yo{k}", [128, D]) for k in range(NCH)]
                xr = [kb.sb(st, f"xr{k}", [128, D]) for k in range(2)]
                ptr = [kb.ps(st, f"mpt{k}", [128, 512]) for k in range(2)]
                p1 = [kb.ps(st, f"mp1{k}", [128, 512]) for k in range(2)]
                p3 = [kb.ps(st, f"mp3{k}", [128, 512]) for k in range(2)]
                py = [kb.ps(st, f"mpy{k}", [128, 512]) for k in range(2)]
                nw = 0
                nw2 = 0
                ng = 0
                npp = 0
                cgs = [(c0, min(512, NSC - c0)) for c0 in range(0, NSC, 512)]
                for Yo in yo:
                    kb.op("pool", lambda e: e.memset(Yo[:], 0.0), W=[Yo])
                for ex in range(NE):
                    for ci in range(NCH):
                        G = xg[ng % 2]
                        ng += 1
                        kb.op("pool", lambda e: e.indirect_dma_start(out=G[:, :], out_offset=None, in_=d["H"][:, :],
                                                                      in_offset=bass.IndirectOffsetOnAxis(ap=idxT[:, ci, ex:ex + 1], axis=0)),
                              R=[d["H"], idxT], W=[G], dma=True)
                        for k in range(8):
                            p = ptr[k // 4]
                            kb.op("pe", lambda e: e.transpose(p[:, (k % 4) * 128:(k % 4 + 1) * 128], G[:, k * 128:(k + 1) * 128], self.ident[:]),
                                  R=[G, self.ident], P=[p])
                        for jj in range(2):
                            if jj == 0:
                                kb.op("act", lambda e: e.activation(xsT[:, 0:4, offs[ci]:offs[ci] + chunks[ci][2]], ptr[0][:].rearrange("p (a b) -> p a b", a=4)[:, :, 0:chunks[ci][2]], AF.Copy),
                                      R=[ptr[0]], P=[xsT])
                            else:
                                kb.op("dve", lambda e: e.tensor_copy(xsT[:, 4:8, offs[ci]:offs[ci] + chunks[ci][2]], ptr[1][:].rearrange("p (a b) -> p a b", a=4)[:, :, 0:chunks[ci][2]]),
                                      R=[ptr[1]], P=[xsT])
                    for fb in range(NFB):
                        W1, W3 = w1[nw % 2], w3[nw % 2]
                        nw += 1
                        kb.dma("sp", W1, W1[:], i["ffn_w1"], i["ffn_w1"][l, ex, :, fb * FB:(fb + 1) * FB].rearrange("(k p) f -> p k f", p=128), part=False)
                        kb.dma("sp", W3, W3[:], i["ffn_w3"], i["ffn_w3"][l, ex, :, fb * FB:(fb + 1) * FB].rearrange("(k p) f -> p k f", p=128), part=False)
                        for fc in range(FB // 128):
                            f = fb * (FB // 128) + fc
                            for (c0, cw) in cgs:
                                P1, P3 = p1[npp % 2], p3[npp % 2]
                                npp += 1
                                for k in range(8):
                                    kb.op("pe", lambda e: e.matmul(P1[:, 0:cw], W1[:, k, fc * 128:(fc + 1) * 128], xsT[:, k, c0:c0 + cw], start=(k == 0), stop=(k == 7)),
                                          R=[W1, xsT], P=[P1])
                                for k in range(8):
                                    kb.op("pe", lambda e: e.matmul(P3[:, 0:cw], W3[:, k, fc * 128:(fc + 1) * 128], xsT[:, k, c0:c0 + cw], start=(k == 0), stop=(k == 7)),
                                          R=[W3, xsT], P=[P3])
                                kb.op("act", lambda e: e.activation(sg[:, 0:cw], P1[:, 0:cw], AF.Silu), R=[P1], W=[sg])
                                kb.op("dve", lambda e: e.tensor_tensor(hT[:, f, c0:c0 + cw], sg[:, 0:cw], P3[:, 0:cw], op=ALU.mult), R=[sg, P3], P=[hT])
                    for db in range(4):
                        W2 = w2[nw2 % 2]
                        nw2 += 1
                        kb.dma("sp", W2, W2[:], i["ffn_w2"], i["ffn_w2"][l, ex, :, db * 256:(db + 1) * 256].rearrange("(f p) n -> p f n", p=128), part=False)
                        for ci in range(NCH):
                            PY = py[(db * NCH + ci) % 2]
                            Yo = yo[ci]
                            cn = chunks[ci][2]
                            for f in range(NF):
                                kb.op("pe", lambda e: e.matmul(PY[0:cn, 0:256], hT[:, f, offs[ci]:offs[ci] + cn], W2[:, f, :], start=(f == 0), stop=(f == NF - 1)),
                                      R=[hT, W2], P=[PY])
                            g = gm[chunks[ci][0]]
                            kb.op("dve", lambda e: e.scalar_tensor_tensor(Yo[0:cn, db * 256:(db + 1) * 256], PY[0:cn, 0:256], gateT[0:cn, ci, ex:ex + 1], g[0:cn, db * 256:(db + 1) * 256],
                                                                          op0=ALU.mult, op1=ALU.mult), R=[PY, gateT, g], P=[Yo])
                            if db == 3:
                                Xr = xr[ci % 2]
                                kb.op("pool", lambda e: e.indirect_dma_start(out=Xr[:, :], out_offset=None, in_=d["X"][:, :],
                                                                              in_offset=bass.IndirectOffsetOnAxis(ap=idxT[:, ci, ex:ex + 1], axis=0)),
                                      R=[d["X"], idxT], W=[Xr], dma=True)
                                kb.op("dve", lambda e: e.tensor_tensor(Xr[:], Xr[:], Yo[:], op=ALU.add), R=[Xr, Yo], W=[Xr])
                                kb.op("pool", lambda e: e.indirect_dma_start(out=d["X"][:, :], out_offset=bass.IndirectOffsetOnAxis(ap=idxT[:, ci, ex:ex + 1], axis=0),
                                                                              in_=Xr[:, :], in_offset=None),
                                      R=[Xr, idxT], W=[d["X"]], dma=True)

    def build(self):
        kb = self.kb
        self.declare()
        with contextlib.ExitStack() as gst:
            self.gst = gst
            self.phase_init()
            cnt = {0: 0, 1: 0, 2: 0}
            dbg = self.cfg.get("dbg", 99)
            for l, kind in enumerate(self.kinds):
                last = l == self.L - 1
                j = cnt[kind]
                cnt[kind] += 1
                if dbg >= 1:
                    self.phase_norm(l, 0, 1, list(range(self.NT)), self.d["H"])
                if kind == 0:
                    if dbg >= 2:
                        self.phase_qkv_A(l, j)
                    if dbg >= 3:
                        self.phase_attn_A(last)
                    if dbg >= 4:
                        self.phase_out_proj(l, self.i["a_w_o"], self.i["a_w_o"][j], last)
                elif kind == 1:
                    if dbg >= 2:
                        self.phase_qkv_B(l, j)
                    if dbg >= 3:
                        self.phase_attn_B(j, last)
                    if dbg >= 4:
                        self.phase_out_proj(l, self.i["b_w_o"], self.i["b_w_o"][j], last)
                else:
                    if dbg >= 2:
                        self.phase_rwkv(l, j, last, dbg)
                if dbg >= 5:
                    self.phase_moe(l, last)
            self.phase_norm(self.L - 1, 0, 0, list(range(self.NTL)), self.out, final=True)
            kb.finish([self.out])
        return kb.nc


def _constants(cfg):
    S, C = cfg["S"], cfg["C"]
    T = S + C
    k = {}
    k["k_ident"] = np.eye(128, dtype=np.float32)
    rows = np.repeat(np.arange(S // 64), 64).astype(np.float32)
    cols = np.tile(np.arange(64), S // 64).astype(np.float32)

    def tab(hd):
        nf = hd // 4
        inv = (np.float32(10000.0) ** (-np.arange(nf, dtype=np.float32) / np.float32(nf))).astype(np.float32)
        ang = np.concatenate([rows[:, None] * inv, cols[:, None] * inv], axis=-1).astype(np.float32)
        return np.concatenate([np.cos(ang), np.sin(ang)], axis=-1).astype(np.float32)

    k["k_ropeA"] = tab(128)
    k["k_ropeB"] = tab(64)
    jj, ii = np.meshgrid(np.arange(128), np.arange(128), indexing="ij")
    k["k_tri"] = np.stack([(jj >= ii), (jj <= ii)]).astype(np.float32)
    bd = np.zeros((48, 8, 64), np.float32)
    for r in range(48):
        bd[r, r % 8, :] = 1.0
    k["k_bd"] = bd.reshape(48, 512)
    sel = np.zeros((2, 64, 64, 2, 8), np.float32)
    for hg in range(2):
        for t in range(64):
            sel[hg, t, t, hg, :] = 1.0
    k["k_sel"] = sel.reshape(128, 64, 16)
    k["k_pad"] = (T + np.arange(128, dtype=np.float32)).reshape(128, 1)
    k["k_zero"] = np.zeros((128, D), np.float32)
    return k


_NC_CACHE = {}


def kernel(**inputs):
    cfg = CFG
    n = cfg["ncores"]
    key = repr(cfg)
    if key not in _NC_CACHE:
        _NC_CACHE[key] = Prog(cfg).build()
    nc = _NC_CACHE[key]
    consts = _constants(cfg)
    f = lambda a: np.ascontiguousarray(np.asarray(a, dtype=np.float32))
    shared = {}
    for name, a in inputs.items():
        if name in ("x", "c", "ctx"):
            continue
        a = f(a)
        if name in ("c_ctx", "final_norm"):
            a = a.reshape(1, D)
        if name == "c_r_k":
            a = a.reshape(a.shape[0], 2, D)
        shared[name] = a
    shared.update(consts)
    x, c, ctx = f(inputs["x"]), f(inputs["c"]), f(inputs["ctx"])
    in_maps = []
    for b in range(n):
        m = dict(shared)
        m["x"] = x[b]
        m["c"] = c[b:b + 1]
        m["ctx"] = ctx[b]
        in_maps.append(m)
    res = run_bass_kernel_spmd(nc, in_maps, core_ids=list(range(n)))
    return np.stack([np.asarray(r["out"]) for r in res.results], axis=0).astype(np.float32)
```

```python
import contextlib
import numpy as np
import concourse.bass as bass
import concourse.mybir as mybir
from concourse.bass_utils import run_bass_kernel_spmd

F32 = mybir.dt.float32
U32 = mybir.dt.uint32
I32 = mybir.dt.int32
ALU = mybir.AluOpType
AF = mybir.ActivationFunctionType
AX = mybir.AxisListType

D = 1024
NE = 16
EPS = 1e-6
CFG = dict(S=4096, C=256, FF=2048, kinds=[0, 1, 2, 0], ncores=8)


class Buf:
    __slots__ = ("t", "w", "r", "pr", "name")

    def __init__(self, t, name):
        self.t = t
        self.name = name
        self.w = []
        self.r = []
        self.pr = []

    def __getitem__(self, k):
        return self.t[k]


class KB:
    SEM_LIMIT = 30000
    N_DMA_SLOTS = 48

    def __init__(self):
        self.nc = bass.Bass("TRN2", target_bir_lowering=False)
        nc = self.nc
        self.es = contextlib.ExitStack()
        self.engs = {"pe": nc.tensor, "dve": nc.vector, "act": nc.scalar, "pool": nc.gpsimd, "sp": nc.sync}
        self.sem = {}
        self.cnt = {}
        self.nsem = 0
        for e in self.engs:
            self._new_sem(e)
        self.seen = {e: {} for e in self.engs}
        self.slots = []
        for i in range(self.N_DMA_SLOTS):
            s = self.es.enter_context(nc.semaphore(f"dq{i}"))
            self.slots.append([s, 0])
        self.slot_i = 0
        self.uid = 0
        self.ninst = 0

    def _new_sem(self, e):
        self.nsem += 1
        self.sem[e] = self.es.enter_context(self.nc.semaphore(f"s_{e}_{self.nsem}"))
        self.cnt[e] = 0

    def inp(self, name, shape, dtype=F32):
        return Buf(self.nc.dram_tensor(name, list(shape), dtype, kind="ExternalInput").ap(), name)

    def outp(self, name, shape, dtype=F32):
        return Buf(self.nc.dram_tensor(name, list(shape), dtype, kind="ExternalOutput").ap(), name)

    def dram(self, name, shape, dtype=F32):
        return Buf(self.nc.dram_tensor(name, list(shape), dtype, kind="Internal").ap(), name)

    def sb(self, stack, name, shape, dtype=F32):
        self.uid += 1
        t = stack.enter_context(self.nc.sbuf_tensor(f"{name}_{self.uid}", list(shape), dtype))
        return Buf(t, name)

    def ps(self, stack, name, shape, dtype=F32):
        self.uid += 1
        t = stack.enter_context(self.nc.psum_tensor(f"{name}_{self.uid}", list(shape), dtype))
        return Buf(t, name)

    def _wait(self, eng, sem, val):
        d = self.seen[eng]
        k = id(sem)
        if d.get(k, (None, 0))[1] >= val:
            return
        self.engs[eng].wait_ge(sem, val)
        d[k] = (sem, val)
        self.ninst += 1

    @staticmethod
    def _add(lst, ev):
        out = [x for x in lst if not (x[0] is ev[0] and x[1] <= ev[1])]
        out.append(ev)
        return out

    def op(self, eng, fn, R=(), W=(), P=(), dma=False):
        for b in R:
            for ev in b.w:
                self._wait(eng, ev[0], ev[1])
        for b in W:
            for ev in b.w + b.r + b.pr:
                self._wait(eng, ev[0], ev[1])
        for b in P:
            for ev in b.r + b.pr:
                self._wait(eng, ev[0], ev[1])
            for ev in b.w:
                if ev[2]:
                    self._wait(eng, ev[0], ev[1])
        if dma:
            slot = self.slots[self.slot_i]
            self.slot_i = (self.slot_i + 1) % len(self.slots)
            if slot[1] > 0:
                self._wait(eng, slot[0], slot[1])
            inst = fn(self.engs[eng])
            slot[1] += 16
            inst.then_inc(slot[0], 16)
            sem, val = slot[0], slot[1]
        else:
            if self.cnt[eng] >= self.SEM_LIMIT:
                self._new_sem(eng)
            inst = fn(self.engs[eng])
            self.cnt[eng] += 1
            sem, val = self.sem[eng], self.cnt[eng]
            inst.then_inc(sem, 1)
        self.ninst += 1
        for b in R:
            b.r = self._add(b.r, (sem, val))
        for b in W:
            b.w = [(sem, val, True)]
            b.r = []
            b.pr = []
        for b in P:
            if b.r:
                b.pr = b.r
                b.r = []
                b.w = []
            b.w = self._add(b.w, (sem, val, False))
        return (sem, val)

    def dma(self, eng, out_b, out_ap, in_b, in_ap, part=True, **kw):
        def f(e):
            return e.dma_start(out=out_ap, in_=in_ap, **kw)
        if part:
            return self.op(eng, f, R=[in_b], P=[out_b], dma=True)
        return self.op(eng, f, R=[in_b], W=[out_b], dma=True)

    def barrier(self):
        for e in self.engs:
            for e2 in self.engs:
                if e2 != e and self.cnt[e2] > 0:
                    self._wait(e, self.sem[e2], self.cnt[e2])
            for slot in self.slots:
                if slot[1] > 0:
                    self._wait(e, slot[0], slot[1])

    def finish(self, bufs):
        for b in bufs:
            for ev in b.w:
                self._wait("sp", ev[0], ev[1])
        self.es.close()
        return self.nc


def bc_last(ap, n):
    return ap.unsqueeze(2).to_broadcast([ap.shape[0], ap.shape[1], n])


def bc_mid(ap, n):
    return ap.unsqueeze(1).to_broadcast([ap.shape[0], n, ap.shape[1]])


class Prog:
    def __init__(self, cfg):
        self.cfg = cfg
        self.S, self.C, self.FF = cfg["S"], cfg["C"], cfg["FF"]
        self.T = self.S + self.C
        self.kinds = cfg["kinds"]
        self.L = len(self.kinds)
        self.NT = self.T // 128
        self.NTL = self.S // 128
        self.kb = KB()

    def rstd(self, st, ss, n, width, tmp):
        kb = self.kb
        kb.op("dve", lambda e: e.tensor_scalar(tmp[:, 0:n], ss[:, 0:n], 1.0 / width, EPS, op0=ALU.mult, op1=ALU.add),
              R=[ss], W=[tmp])
        kb.op("act", lambda e: e.activation(tmp[:, 0:n], tmp[:, 0:n], AF.Sqrt), R=[tmp], W=[tmp])
        kb.op("dve", lambda e: e.reciprocal(tmp[:, 0:n], tmp[:, 0:n]), R=[tmp], W=[tmp])

    def load_bc(self, dst, src_b, row_ap, plus_one=False):
        kb = self.kb
        kb.dma("sp", dst, dst[:], src_b, row_ap.partition_broadcast(128), part=False)
        if plus_one:
            kb.op("pool", lambda e: e.tensor_scalar(dst[:], dst[:], 1.0, None, op0=ALU.add), R=[dst], W=[dst])

    def declare(self):
        kb, S, C, T, L, FF = self.kb, self.S, self.C, self.T, self.L, self.FF
        nA, nB, nC = max(1, self.kinds.count(0)), max(1, self.kinds.count(1)), max(1, self.kinds.count(2))
        i = {}
        i["x"] = kb.inp("x", [S, D])
        i["c"] = kb.inp("c", [1, D])
        i["ctx"] = kb.inp("ctx", [C, D])
        i["c_ctx"] = kb.inp("c_ctx", [1, D])
        i["mod_w"] = kb.inp("mod_w", [L, D, 6 * D])
        i["mod_b"] = kb.inp("mod_b", [L, 6 * D])
        i["a_w_qkv"] = kb.inp("a_w_qkv", [nA, D, 1536])
        i["a_w_o"] = kb.inp("a_w_o", [nA, D, D])
        i["a_q_norm"] = kb.inp("a_q_norm", [nA, 128])
        i["a_k_norm"] = kb.inp("a_k_norm", [nA, 128])
        i["b_w_qkv"] = kb.inp("b_w_qkv", [nB, D, 1536])
        i["b_w_o"] = kb.inp("b_w_o", [nB, D, D])
        i["b_sink"] = kb.inp("b_sink", [nB, 16])
        i["c_mu"] = kb.inp("c_mu", [nC, 6, D])
        i["c_w_rkv"] = kb.inp("c_w_rkv", [nC, 3, D, D])
        i["c_w_o"] = kb.inp("c_w_o", [nC, D, D])
        i["c_w0"] = kb.inp("c_w0", [nC, 2, D])
        i["c_w1"] = kb.inp("c_w1", [nC, 2, D, 64])
        i["c_w2"] = kb.inp("c_w2", [nC, 2, 64, D])
        i["c_a0"] = kb.inp("c_a0", [nC, 2, D])
        i["c_a1"] = kb.inp("c_a1", [nC, 2, D, 64])
        i["c_a2"] = kb.inp("c_a2", [nC, 2, 64, D])
        i["c_g1"] = kb.inp("c_g1", [nC, D, 128])
        i["c_g2"] = kb.inp("c_g2", [nC, 128, D])
        i["c_k_k"] = kb.inp("c_k_k", [nC, D])
        i["c_k_a"] = kb.inp("c_k_a", [nC, D])
        i["c_r_k"] = kb.inp("c_r_k", [nC, 2, D])
        i["c_ln_w"] = kb.inp("c_ln_w", [nC, D])
        i["c_ln_b"] = kb.inp("c_ln_b", [nC, D])
        i["router_w"] = kb.inp("router_w", [L, D, NE])
        i["ffn_w1"] = kb.inp("ffn_w1", [L, NE, D, FF])
        i["ffn_w3"] = kb.inp("ffn_w3", [L, NE, D, FF])
        i["ffn_w2"] = kb.inp("ffn_w2", [L, NE, FF, D])
        i["final_norm"] = kb.inp("final_norm", [1, D])
        i["k_ident"] = kb.inp("k_ident", [128, 128])
        i["k_ropeA"] = kb.inp("k_ropeA", [S, 128])
        i["k_ropeB"] = kb.inp("k_ropeB", [S, 64])
        i["k_tri"] = kb.inp("k_tri", [2, 128, 128])
        i["k_bd"] = kb.inp("k_bd", [48, 512])
        i["k_sel"] = kb.inp("k_sel", [128, 64, 16])
        i["k_pad"] = kb.inp("k_pad", [128, 1])
        i["k_zero"] = kb.inp("k_zero", [128, D])
        self.i = i
        self.out = kb.outp("out", [S, D])
        d = {}
        d["X"] = kb.dram("X", [T + 128, D])
        d["H"] = kb.dram("H", [T + 128, D])
        d["MODV"] = kb.dram("MODV", [L, 2, 6 * D])
        d["QT"] = kb.dram("QT", [20, 128, T])
        d["V"] = kb.dram("V", [T, 256])
        d["O"] = kb.dram("O", [T, D])
        if 2 in self.kinds:
            d["XJ"] = kb.dram("XJ", [6, T, D])
            for nm in ("RR", "KK", "VV", "GG", "AV"):
                d[nm] = kb.dram(nm, [T, D])
            for nm in ("DEC", "AA", "BB", "KD", "RP"):
                d[nm] = kb.dram(nm, [2, T, D])
            d["SC"] = kb.dram("SC", [T, 80])
            self.QSB = 1024
            d["QS"] = [[kb.dram(f"QS{a}_{b}", [min(2048, T - b * 2048), 32 * 512]) for b in range((T + 2047) // 2048)] for a in range(2)]
        self.d = d

    def phase_init(self):
        kb, i, d, S, C, T = self.kb, self.i, self.d, self.S, self.C, self.T
        with contextlib.ExitStack() as st:
            kb.barrier()
            self.ident = kb.sb(self.gst, "ident", [128, 128])
            kb.dma("sp", self.ident, self.ident[:], i["k_ident"], i["k_ident"][:], part=False)
            tl = [kb.sb(st, f"cp{j}", [128, D]) for j in range(2)]
            for t in range(self.NT):
                b = tl[t % 2]
                src = i["x"][t * 128:(t + 1) * 128, :] if t < self.NTL else i["ctx"][(t - self.NTL) * 128:(t - self.NTL + 1) * 128, :]
                kb.dma("sp", b, b[:], i["x"] if t < self.NTL else i["ctx"], src, part=False)
                kb.dma("sp", d["X"], d["X"][t * 128:(t + 1) * 128, :], b, b[:])
            z = kb.sb(st, "z", [128, D])
            kb.dma("sp", z, z[:], i["k_zero"], i["k_zero"][:], part=False)
            kb.dma("sp", d["X"], d["X"][T:T + 128, :], z, z[:])
            kb.dma("sp", d["H"], d["H"][T:T + 128, :], z, z[:])
            cT = kb.sb(st, "cT", [128, 8, 2])
            kb.dma("sp", cT, cT[:, :, 0], i["c"], i["c"][0, :].rearrange("(k p) -> p k", p=128), allow_slow_non_contiguous=True)
            kb.dma("sp", cT, cT[:, :, 1], i["c_ctx"], i["c_ctx"][0, :].rearrange("(k p) -> p k", p=128), allow_slow_non_contiguous=True)
            kb.op("act", lambda e: e.activation(cT[:], cT[:], AF.Silu), R=[cT], W=[cT])
            wts = [kb.sb(st, f"mw{j}", [128, 8, 512]) for j in range(2)]
            mb = kb.sb(st, "mb", [2, 6 * D])
            mv = kb.sb(st, "mv", [2, 6 * D])
            pm = [kb.ps(st, f"pm{j}", [2, 512]) for j in range(2)]
            n = 0
            for l in range(self.L):
                kb.dma("sp", mb, mb[:], i["mod_b"], i["mod_b"][l, :].partition_broadcast(2), part=False)
                for nb in range(12):
                    wt = wts[n % 2]
                    p = pm[n % 2]
                    n += 1
                    kb.dma("sp", wt, wt[:], i["mod_w"], i["mod_w"][l, :, nb * 512:(nb + 1) * 512].rearrange("(k p) n -> p k n", p=128), part=False)
                    for k in range(8):
                        kb.op("pe", lambda e: e.matmul(p[:], cT[:, k, :], wt[:, k, :], start=(k == 0), stop=(k == 7)), R=[cT, wt], P=[p])
                    kb.op("dve", lambda e: e.tensor_tensor(mv[:, nb * 512:(nb + 1) * 512], p[:], mb[:, nb * 512:(nb + 1) * 512], op=ALU.add),
                          R=[p, mb], P=[mv])
                kb.dma("sp", d["MODV"], d["MODV"][l], mv, mv[:])

    def mod_row(self, l, row, idx):
        return self.d["MODV"][l, row, idx * D:(idx + 1) * D]

    def phase_norm(self, l, sh_idx, sc_idx, tiles, dst, final=False):
        kb, d = self.kb, self.d
        with contextlib.ExitStack() as st:
            kb.barrier()
            if final:
                fw = kb.sb(st, "fw", [128, D])
                self.load_bc(fw, self.i["final_norm"], self.i["final_norm"][0, :])
                bc = {0: (None, fw)}
            else:
                bc = {}
                for row in (0, 1):
                    sh = kb.sb(st, f"sh{row}", [128, D])
                    sc = kb.sb(st, f"sc{row}", [128, D])
                    self.load_bc(sh, d["MODV"], self.mod_row(l, row, sh_idx))
                    self.load_bc(sc, d["MODV"], self.mod_row(l, row, sc_idx), plus_one=True)
                    bc[row] = (sh, sc)
            xt = [kb.sb(st, f"nx{j}", [128, D]) for j in range(2)]
            ht = [kb.sb(st, f"nh{j}", [128, D]) for j in range(2)]
            sq = kb.sb(st, "nsq", [128, D])
            ss = kb.sb(st, "nss", [128, 1])
            rs = kb.sb(st, "nrs", [128, 1])
            if tiles:
                kb.dma("sp", xt[0], xt[0][:], d["X"], d["X"][tiles[0] * 128:(tiles[0] + 1) * 128, :], part=False)
            for n, t in enumerate(tiles):
                X, Hh = xt[n % 2], ht[n % 2]
                sh, sc = bc[0 if t < self.NTL else 1]
                if n + 1 < len(tiles):
                    tn = tiles[n + 1]
                    Xn = xt[(n + 1) % 2]
                    kb.dma("sp", Xn, Xn[:], d["X"], d["X"][tn * 128:(tn + 1) * 128, :], part=False)
                kb.op("act", lambda e: e.activation(sq[:], X[:], AF.Square, accum_out=ss[:]), R=[X], W=[sq, ss])
                self.rstd(st, ss, 1, D, rs)
                kb.op("dve", lambda e: e.scalar_tensor_tensor(Hh[:], X[:], rs[:, 0:1], sc[:], op0=ALU.mult, op1=ALU.mult),
                      R=[X, rs, sc], W=[Hh])
                if sh is not None:
                    kb.op("pool", lambda e: e.tensor_tensor(Hh[:], Hh[:], sh[:], op=ALU.add), R=[Hh, sh], W=[Hh])
                kb.dma("sp", dst, dst[t * 128:(t + 1) * 128, :], Hh, Hh[:])

    def phase_linear(self, st, src, w_b, w_ap, N, tiles, post, wt=None, src_ap=None):
        kb = self.kb
        nb = (N + 511) // 512
        if wt is None:
            wt = kb.sb(st, "lw", [128, 8, N])
            kb.dma("sp", wt, wt[:], w_b, w_ap.rearrange("(k p) n -> p k n", p=128), part=False)
        xin = [kb.sb(st, f"lx{j}", [128, D]) for j in range(3)]
        xTs = [kb.sb(st, f"lxT{j}", [128, 8, 128]) for j in range(2)]
        ys = [kb.sb(st, f"ly{j}", [128, N]) for j in range(2)]
        ptr = [kb.ps(st, f"lpt{j}", [128, 512]) for j in range(2)]
        po = [kb.ps(st, f"lpo{j}", [128, 512]) for j in range(nb)]
        sap = src_ap if src_ap is not None else src.t
        for n0 in range(min(2, len(tiles))):
            kb.dma("sp", xin[n0], xin[n0][:], src, sap[tiles[n0] * 128:(tiles[n0] + 1) * 128, :], part=False)
        if tiles:
            self.transpose8(xin[0], xTs[0], ptr)
        for n, t in enumerate(tiles):
            Y = ys[n % 2]
            xT = xTs[n % 2]
            if n + 2 < len(tiles):
                tn = tiles[n + 2]
                Xn = xin[(n + 2) % 3]
                kb.dma("sp", Xn, Xn[:], src, sap[tn * 128:(tn + 1) * 128, :], part=False)
            if n + 1 < len(tiles):
                self.transpose8(xin[(n + 1) % 3], xTs[(n + 1) % 2], ptr)
            for j in range(nb):
                w = min(512, N - j * 512)
                for k in range(8):
                    kb.op("pe", lambda e: e.matmul(po[j][:, 0:w], xT[:, k, :], wt[:, k, j * 512:j * 512 + w], start=(k == 0), stop=(k == 7)),
                          R=[xT, wt], P=[po[j]])
                eng = "act" if j % 2 == 0 else "dve"
                if eng == "act":
                    kb.op("act", lambda e: e.activation(Y[:, j * 512:j * 512 + w], po[j][:, 0:w], AF.Copy), R=[po[j]], P=[Y])
                else:
                    kb.op("dve", lambda e: e.tensor_copy(Y[:, j * 512:j * 512 + w], po[j][:, 0:w]), R=[po[j]], P=[Y])
            post(n, t, Y)

    def transpose8(self, X, xT, ptr, nblk=8):
        kb = self.kb
        for k in range(nblk):
            p = ptr[k // 4]
            kb.op("pe", lambda e: e.transpose(p[:, (k % 4) * 128:(k % 4 + 1) * 128], X[:, k * 128:(k + 1) * 128], self.ident[:]),
                  R=[X, self.ident], P=[p])
        for j in range((nblk + 3) // 4):
            nn = min(4, nblk - j * 4)
            if j % 2 == 0:
                kb.op("act", lambda e: e.activation(xT[:, j * 4:j * 4 + nn, :], ptr[j][:, 0:nn * 128].rearrange("p (a b) -> p a b", a=nn), AF.Copy),
                      R=[ptr[j]], P=[xT])
            else:
                kb.op("dve", lambda e: e.tensor_copy(xT[:, j * 4:j * 4 + nn, :], ptr[j][:, 0:nn * 128].rearrange("p (a b) -> p a b", a=nn)),
                      R=[ptr[j]], P=[xT])

    def phase_qkv_A(self, l, j):
        kb, i, d = self.kb, self.i, self.d
        with contextlib.ExitStack() as st:
            kb.barrier()
            gq = kb.sb(st, "gq", [128, 128])
            gk = kb.sb(st, "gk", [128, 128])
            self.load_bc(gq, i["a_q_norm"], i["a_q_norm"][j, :])
            self.load_bc(gk, i["a_k_norm"], i["a_k_norm"][j, :])
            sq = kb.sb(st, "asq", [128, 1280])
            ss = kb.sb(st, "ass", [128, 10])
            rs = kb.sb(st, "ars", [128, 10])
            cs = [kb.sb(st, f"acs{k}", [128, 128]) for k in range(2)]
            t1 = kb.sb(st, "at1", [128, 10, 64])
            t2 = kb.sb(st, "at2", [128, 10, 64])
            yr = kb.sb(st, "ayr", [128, 1280])
            qT = kb.sb(st, "aqT", [128, 10, 128])
            ptr = [kb.ps(st, f"apt{k}", [128, 512]) for k in range(3)]

            def post(n, t, Y):
                lat = t < self.NTL
                yv = Y[:, 0:1280].rearrange("p (h e) -> p h e", h=10)
                kb.op("pool", lambda e: e.tensor_tensor(sq[:], Y[:, 0:1280], Y[:, 0:1280], op=ALU.mult), R=[Y], W=[sq])
                kb.op("dve", lambda e: e.tensor_reduce(ss[:], sq[:].rearrange("p (h e) -> p h e", h=10), axis=AX.X, op=ALU.add), R=[sq], W=[ss])
                self.rstd(st, ss, 10, 128, rs)
                kb.op("dve", lambda e: e.tensor_tensor(yv, yv, bc_last(rs[:, :], 128), op=ALU.mult), R=[Y, rs], W=[Y])
                kb.op("pool", lambda e: e.tensor_tensor(yv[:, 0:8, :], yv[:, 0:8, :], bc_mid(gq[:, :], 8), op=ALU.mult), R=[Y, gq], W=[Y])
                kb.op("pool", lambda e: e.tensor_tensor(yv[:, 8:10, :], yv[:, 8:10, :], bc_mid(gk[:, :], 2), op=ALU.mult), R=[Y, gk], W=[Y])
                if lat:
                    c_ = cs[n % 2]
                    kb.dma("sp", c_, c_[:], i["k_ropeA"], i["k_ropeA"][t * 128:(t + 1) * 128, :], part=False)
                    x1, x2 = yv[:, :, 0:64], yv[:, :, 64:128]
                    yrv = yr[:].rearrange("p (h e) -> p h e", h=10)
                    cosb, sinb = bc_mid(c_[:, 0:64], 10), bc_mid(c_[:, 64:128], 10)
                    kb.op("dve", lambda e: e.tensor_tensor(t1[:], x1, cosb, op=ALU.mult), R=[Y, c_], W=[t1])
                    kb.op("pool", lambda e: e.tensor_tensor(t2[:], x2, sinb, op=ALU.mult), R=[Y, c_], W=[t2])
                    kb.op("dve", lambda e: e.tensor_tensor(yrv[:, :, 0:64], t1[:], t2[:], op=ALU.subtract), R=[t1, t2], W=[yr])
                    kb.op("pool", lambda e: e.tensor_tensor(t1[:], x1, sinb, op=ALU.mult), R=[Y, c_], W=[t1])
                    kb.op("dve", lambda e: e.tensor_tensor(t2[:], x2, cosb, op=ALU.mult), R=[Y, c_], W=[t2])
                    kb.op("pool", lambda e: e.tensor_tensor(yrv[:, :, 64:128], t1[:], t2[:], op=ALU.add), R=[t1, t2], P=[yr])
                    src = yr
                else:
                    src = Y
                self.transpose8(src, qT, ptr, nblk=10)
                kb.dma("sp", d["QT"], d["QT"][0:10, :, t * 128:(t + 1) * 128].rearrange("h p t -> p h t"), qT, qT[:])
                kb.dma("sp", d["V"], d["V"][t * 128:(t + 1) * 128, :], Y, Y[:, 1280:1536])

            self.phase_linear(st, d["H"], i["a_w_qkv"], i["a_w_qkv"][j], 1536, list(range(self.NT)), post)

    def phase_attn_A(self, last):
        kb, d, T, S, C = self.kb, self.d, self.T, self.S, self.C
        scale = 128 ** -0.5
        with contextlib.ExitStack() as st:
            kb.barrier()
            kT = kb.sb(st, "kT", [128, T])
            vx = kb.sb(st, "vx", [128, self.NT, 129])
            qb = [kb.sb(st, f"qb{k}", [128, 512]) for k in range(2)]
            pT = [kb.sb(st, f"pT{k}", [128, 512]) for k in range(3)]
            ob = [kb.sb(st, f"ob{k}", [128, 128]) for k in range(2)]
            rc = kb.sb(st, "rc", [128, 1])
            psS = [kb.ps(st, f"psS{k}", [128, 512]) for k in range(2)]
            psO = [kb.ps(st, f"psO{k}", [128, 512]) for k in range(4)]
            nq = 0
            nch = 0
            for g in range(2):
                kb.dma("sp", kT, kT[:], d["QT"], d["QT"][8 + g], part=False)
                kb.op("pool", lambda e: e.memset(vx[:, :, 128:129], 1.0), W=[vx])
                kb.dma("sp", vx, vx[:, :, 0:128], d["V"], d["V"][:, g * 128:(g + 1) * 128].rearrange("(c p) e -> p c e", p=128))
                blocks = [(q0, min(512, S - q0), list(range(self.NT))) for q0 in range(0, S, 512)]
                if not last:
                    blocks += [(q0, min(512, T - q0), list(range(self.NTL, self.NT))) for q0 in range(S, T, 512)]
                items = [(h, q0, nqq, chunks) for h in range(4 * g, 4 * g + 4) for (q0, nqq, chunks) in blocks]
                kb.dma("sp", qb[nq % 2], qb[nq % 2][:, 0:items[0][2]], d["QT"], d["QT"][items[0][0], :, items[0][1]:items[0][1] + items[0][2]], part=False)
                for ii, (h, q0, nqq, chunks) in enumerate(items):
                    if True:
                        Q = qb[nq % 2]
                        nq += 1
                        if ii + 1 < len(items):
                            hn_, q0n, nqn, _ = items[ii + 1]
                            Qn = qb[nq % 2]
                            kb.dma("sp", Qn, Qn[:, 0:nqn], d["QT"], d["QT"][hn_, :, q0n:q0n + nqn], part=False)
                        nsub = nqq // 128
                        def emit_s(ci):
                            pS = psS[(nch + ci) % 2]
                            ch = chunks[ci]
                            kb.op("pe", lambda e: e.matmul(pS[:, 0:nqq], kT[:, ch * 128:(ch + 1) * 128], Q[:, 0:nqq], start=True, stop=True),
                                  R=[kT, Q], P=[pS])
                        emit_s(0)
                        for ci, ch in enumerate(chunks):
                            pS = psS[(nch + ci) % 2]
                            P_ = pT[(nch + ci) % 3]
                            if ci + 1 < len(chunks):
                                emit_s(ci + 1)
                            kb.op("act", lambda e: e.activation(P_[:, 0:nqq], pS[:, 0:nqq], AF.Exp, scale=scale), R=[pS], W=[P_])
                            for s in range(nsub):
                                kb.op("pe", lambda e: e.matmul(psO[s][:, 0:129], P_[:, s * 128:(s + 1) * 128], vx[:, ch, :],
                                                               start=(ci == 0), stop=(ci == len(chunks) - 1)), R=[P_, vx], P=[psO[s]])
                        nch += len(chunks)
                        for s in range(nsub):
                            O = ob[s % 2]
                            kb.op("dve", lambda e: e.reciprocal(rc[:], psO[s][:, 128:129]), R=[psO[s]], W=[rc])
                            kb.op("dve", lambda e: e.tensor_scalar(O[:], psO[s][:, 0:128], rc[:, 0:1], None, op0=ALU.mult), R=[psO[s], rc], W=[O])
                            r0 = q0 + s * 128
                            kb.dma("sp", d["O"], d["O"][r0:r0 + 128, h * 128:(h + 1) * 128], O, O[:])

    def phase_out_proj(self, l, w_b, w_ap, last, src=None):
        kb, d = self.kb, self.d
        src = src if src is not None else d["O"]
        with contextlib.ExitStack() as st:
            kb.barrier()
            gt = {}
            for row in ((0,) if last else (0, 1)):
                g = kb.sb(st, f"og{row}", [128, D])
                self.load_bc(g, d["MODV"], self.mod_row(l, row, 2))
                gt[row] = g
            xt = [kb.sb(st, f"ox{k}", [128, D]) for k in range(2)]

            def post(n, t, Y):
                Xt = xt[n % 2]
                g = gt[0 if t < self.NTL else 1]
                kb.dma("sp", Xt, Xt[:], d["X"], d["X"][t * 128:(t + 1) * 128, :], part=False)
                kb.op("pool", lambda e: e.tensor_tensor(Y[:], Y[:], g[:], op=ALU.mult), R=[Y, g], W=[Y])
                kb.op("dve", lambda e: e.tensor_tensor(Xt[:], Xt[:], Y[:], op=ALU.add), R=[Xt, Y], W=[Xt])
                kb.dma("sp", d["X"], d["X"][t * 128:(t + 1) * 128, :], Xt, Xt[:])

            tiles = list(range(self.NTL if last else self.NT))
            self.phase_linear(st, src, w_b, w_ap, D, tiles, post)


    def phase_qkv_B(self, l, j):
        kb, i, d = self.kb, self.i, self.d
        with contextlib.ExitStack() as st:
            kb.barrier()
            cs = [kb.sb(st, f"bcs{k}", [128, 64]) for k in range(2)]
            t1 = kb.sb(st, "bt1", [128, 20, 32])
            t2 = kb.sb(st, "bt2", [128, 20, 32])
            yr = kb.sb(st, "byr", [128, 1280])
            qT = kb.sb(st, "bqT", [128, 10, 128])
            ptr = [kb.ps(st, f"bpt{k}", [128, 512]) for k in range(3)]

            def post(n, t, Y):
                lat = t < self.NTL
                if lat:
                    yv = Y[:, 0:1280].rearrange("p (h e) -> p h e", h=20)
                    c_ = cs[n % 2]
                    kb.dma("sp", c_, c_[:], i["k_ropeB"], i["k_ropeB"][t * 128:(t + 1) * 128, :], part=False)
                    x1, x2 = yv[:, :, 0:32], yv[:, :, 32:64]
                    yrv = yr[:].rearrange("p (h e) -> p h e", h=20)
                    cosb, sinb = bc_mid(c_[:, 0:32], 20), bc_mid(c_[:, 32:64], 20)
                    kb.op("dve", lambda e: e.tensor_tensor(t1[:], x1, cosb, op=ALU.mult), R=[Y, c_], W=[t1])
                    kb.op("pool", lambda e: e.tensor_tensor(t2[:], x2, sinb, op=ALU.mult), R=[Y, c_], W=[t2])
                    kb.op("dve", lambda e: e.tensor_tensor(yrv[:, :, 0:32], t1[:], t2[:], op=ALU.subtract), R=[t1, t2], W=[yr])
                    kb.op("pool", lambda e: e.tensor_tensor(t1[:], x1, sinb, op=ALU.mult), R=[Y, c_], W=[t1])
                    kb.op("dve", lambda e: e.tensor_tensor(t2[:], x2, cosb, op=ALU.mult), R=[Y, c_], W=[t2])
                    kb.op("pool", lambda e: e.tensor_tensor(yrv[:, :, 32:64], t1[:], t2[:], op=ALU.add), R=[t1, t2], P=[yr])
                    src = yr
                else:
                    src = Y
                self.transpose8(src, qT, ptr, nblk=10)
                for half in range(2):
                    dst = d["QT"][:, 0:64, t * 128:(t + 1) * 128].rearrange("(b two) p t -> two p b t", two=2)[half]
                    kb.dma("sp", d["QT"], dst, qT, qT[half * 64:(half + 1) * 64, :, :])
                kb.dma("sp", d["V"], d["V"][t * 128:(t + 1) * 128, :], Y, Y[:, 1280:1536])

            self.phase_linear(st, d["H"], i["b_w_qkv"], i["b_w_qkv"][j], 1536, list(range(self.NT)), post)

    def phase_attn_B(self, j, last):
        kb, i, d, T, S, C = self.kb, self.i, self.d, self.T, self.S, self.C
        scale = 64 ** -0.5
        NT, NTL = self.NT, self.NTL
        with contextlib.ExitStack() as st:
            kb.barrier()
            kT = kb.sb(st, "bkT", [64, 4, T])
            vx = kb.sb(st, "bvx", [128, NT, 4, 65])
            es = kb.sb(st, "bes", [128, 16])
            tri = kb.sb(st, "btri", [128, 2, 128])
            kb.dma("sp", kT, kT[:], d["QT"], d["QT"][16:20, 0:64, :].rearrange("g p t -> p g t"), part=False)
            kb.op("pool", lambda e: e.memset(vx[:, :, :, 64:65], 1.0), W=[vx])
            for g in range(4):
                kb.dma("sp", vx, vx[:, :, g, 0:64], d["V"], d["V"][:, g * 64:(g + 1) * 64].rearrange("(c p) e -> p c e", p=128))
            self.load_bc(es, i["b_sink"], i["b_sink"][j, :])
            kb.op("act", lambda e: e.activation(es[:], es[:], AF.Exp), R=[es], W=[es])
            kb.dma("sp", tri, tri[:], i["k_tri"], i["k_tri"][:].rearrange("a p q -> p a q"), part=False)
            qa = [kb.sb(st, f"bqa{k}", [64, 16, 128]) for k in range(2)]
            pT = [kb.sb(st, f"bpT{k}", [128, 512]) for k in range(3)]
            ot = [kb.sb(st, f"bot{k}", [128, D]) for k in range(2)]
            den = kb.sb(st, "bden", [128, 1])
            psS = [kb.ps(st, f"bpsS{k}", [128, 512]) for k in range(2)]
            psO = [kb.ps(st, f"bpsO{k}", [128, 512]) for k in range(4)]
            nch = 0
            tiles = list(range(NTL if last else NT))
            for n, t in enumerate(tiles):
                if t < NTL:
                    chunks = []
                    if t - 1 >= 0:
                        chunks.append((t - 1, 0))
                    chunks.append((t, None))
                    if t + 1 < NTL:
                        chunks.append((t + 1, 1))
                    chunks += [(c_, None) for c_ in range(NTL, NT)]
                else:
                    chunks = [(c_, None) for c_ in range(NTL, NT)]
                Q = qa[n % 2]
                O = ot[n % 2]
                kb.dma("sp", Q, Q[:], d["QT"], d["QT"][0:16, 0:64, t * 128:(t + 1) * 128].rearrange("h p t -> p h t"), part=False)
                for g in range(4):
                    def emit_s(ci):
                        pS = psS[(nch + ci) % 2]
                        ch = chunks[ci][0]
                        kb.op("pe", lambda e: e.matmul(pS[:], kT[:, g, ch * 128:(ch + 1) * 128], Q[:, 4 * g:4 * g + 4, :].rearrange("p h t -> p (h t)"),
                                                       start=True, stop=True), R=[kT, Q], P=[pS])
                    emit_s(0)
                    for ci, (ch, mk) in enumerate(chunks):
                        pS = psS[(nch + ci) % 2]
                        P_ = pT[(nch + ci) % 3]
                        if ci + 1 < len(chunks):
                            emit_s(ci + 1)
                        kb.op("act", lambda e: e.activation(P_[:], pS[:], AF.Exp, scale=scale), R=[pS], W=[P_])
                        if mk is not None:
                            pv = P_[:].rearrange("p (h t) -> p h t", h=4)
                            kb.op("pool", lambda e: e.tensor_tensor(pv, pv, bc_mid(tri[:, mk, :], 4), op=ALU.mult), R=[P_, tri], W=[P_])
                        for s in range(4):
                            kb.op("pe", lambda e: e.matmul(psO[s][:, 0:65], P_[:, s * 128:(s + 1) * 128], vx[:, ch, g, :],
                                                           start=(ci == 0), stop=(ci == len(chunks) - 1)), R=[P_, vx], P=[psO[s]])
                    nch += len(chunks)
                    for s in range(4):
                        h = 4 * g + s
                        kb.op("dve", lambda e: e.tensor_tensor(den[:], psO[s][:, 64:65], es[:, h:h + 1], op=ALU.add), R=[psO[s], es], W=[den])
                        kb.op("dve", lambda e: e.reciprocal(den[:], den[:]), R=[den], W=[den])
                        kb.op("dve", lambda e: e.tensor_scalar(O[:, h * 64:(h + 1) * 64], psO[s][:, 0:64], den[:, 0:1], None, op0=ALU.mult),
                              R=[psO[s], den], P=[O])
                kb.dma("sp", d["O"], d["O"][t * 128:(t + 1) * 128, :], O, O[:])


    def phase_rwkv(self, l, j, last, dbg=99):
        kb, i, d, T, S, C = self.kb, self.i, self.d, self.T, self.S, self.C
        NT, NTL = self.NT, self.NTL
        alltiles = list(range(NT))
        with contextlib.ExitStack() as st:
            kb.barrier()
            mu = [kb.sb(st, f"mu{k}", [128, D]) for k in range(6)]
            for k in range(6):
                self.load_bc(mu[k], i["c_mu"], i["c_mu"][j, k, :])
            hh = [kb.sb(st, f"rh{k}", [128, D]) for k in range(2)]
            hp = [kb.sb(st, f"rhp{k}", [128, D]) for k in range(2)]
            hn = [kb.sb(st, f"rhn{k}", [128, D]) for k in range(2)]
            xx = kb.sb(st, "rxx", [128, D])
            xj = [kb.sb(st, f"rxj{k}", [128, D]) for k in range(3)]
            nx = 0
            for n, t in enumerate(alltiles):
                t0 = t * 128
                Hc, Hp, Hn = hh[n % 2], hp[n % 2], hn[n % 2]
                kb.dma("sp", Hc, Hc[:], d["H"], d["H"][t0:t0 + 128, :], part=False)
                if t == 0 or t == NTL:
                    kb.dma("sp", Hp, Hp[0:1, :], i["k_zero"], i["k_zero"][0:1, :], part=False)
                    kb.dma("sp", Hp, Hp[1:128, :], d["H"], d["H"][t0:t0 + 127, :])
                else:
                    kb.dma("sp", Hp, Hp[:], d["H"], d["H"][t0 - 1:t0 + 127, :], part=False)
                if t == NTL - 1 or t == NT - 1:
                    kb.dma("sp", Hn, Hn[127:128, :], i["k_zero"], i["k_zero"][0:1, :], part=False)
                    kb.dma("sp", Hn, Hn[0:127, :], d["H"], d["H"][t0 + 1:t0 + 128, :])
                else:
                    kb.dma("sp", Hn, Hn[:], d["H"], d["H"][t0 + 1:t0 + 129, :], part=False)
                kb.op("pool", lambda e: e.tensor_tensor(Hp[:], Hp[:], Hn[:], op=ALU.add), R=[Hp, Hn], W=[Hp])
                kb.op("dve", lambda e: e.scalar_tensor_tensor(xx[:], Hp[:], 0.5, Hc[:], op0=ALU.mult, op1=ALU.subtract), R=[Hp, Hc], W=[xx])
                for k in range(6):
                    X = xj[nx % 3]
                    nx += 1
                    e1, e2 = ("pool", "dve") if k % 2 == 0 else ("dve", "pool")
                    kb.op(e1, lambda e: e.tensor_tensor(X[:], xx[:], mu[k][:], op=ALU.mult), R=[xx, mu[k]], W=[X])
                    kb.op(e2, lambda e: e.tensor_tensor(X[:], X[:], Hc[:], op=ALU.add), R=[X, Hc], W=[X])
                    kb.dma("sp", d["XJ"], d["XJ"][k, t0:t0 + 128, :], X, X[:])
        for (src_k, wi, dst) in ((0, 0, "RR"), (2, 1, "KK"), (3, 2, "VV")):
            with contextlib.ExitStack() as st:
                kb.barrier()

                def post(n, t, Y, dst=dst):
                    kb.dma("sp", d[dst], d[dst][t * 128:(t + 1) * 128, :], Y, Y[:])

                self.phase_linear(st, d["XJ"], i["c_w_rkv"], i["c_w_rkv"][j, wi], D, alltiles, post, src_ap=d["XJ"][src_k])
        def lora(src_k, fill_w1, act, fill_w2, nsplit, epi):
            with contextlib.ExitStack() as st:
                kb.barrier()
                wt = kb.sb(st, "lrw1", [128, 8, 128])
                fill_w1(wt)
                w2t = kb.sb(st, "lrw2", [128, D])
                fill_w2(w2t)
                yT = kb.sb(st, "lryT", [128, 128])
                ot = [kb.sb(st, f"lro{k}", [128, D]) for k in range(2)]
                tmp = kb.sb(st, "lrtmp", [128, 512])
                pt = kb.ps(st, "lrpt", [128, 512])
                po = [kb.ps(st, f"lrpo{k}", [128, 512]) for k in range(2)]
                cnt = [0]

                def post(n, t, Y):
                    if act is not None:
                        kb.op("act", lambda e: e.activation(Y[:], Y[:], act), R=[Y], W=[Y])
                    kb.op("pe", lambda e: e.transpose(pt[:, 0:128], Y[:, 0:128], self.ident[:]), R=[Y, self.ident], W=[pt])
                    kb.op("dve", lambda e: e.tensor_copy(yT[:], pt[:, 0:128]), R=[pt], W=[yT])
                    kk = 128 // nsplit
                    for dd in range(nsplit):
                        O = ot[cnt[0] % 2]
                        cnt[0] += 1
                        for nb in range(2):
                            kb.op("pe", lambda e: e.matmul(po[nb][:], yT[dd * kk:(dd + 1) * kk, :], w2t[dd * kk:(dd + 1) * kk, nb * 512:(nb + 1) * 512],
                                                           start=True, stop=True), R=[yT, w2t], P=[po[nb]])
                            epi(dd, nb, po[nb], O, tmp)
                        self_dst = epi.dst(dd)
                        kb.dma("sp", self_dst[0], self_dst[1][t * 128:(t + 1) * 128, :], O, O[:])

                self.phase_linear(st, d["XJ"], None, None, 128, alltiles, post, wt=wt, src_ap=d["XJ"][src_k])

        def fill2(arr):
            def f(wt):
                for dd in range(2):
                    kb.dma("sp", wt, wt[:, :, dd * 64:(dd + 1) * 64], i[arr], i[arr][j, dd].rearrange("(k p) n -> p k n", p=128))
            return f

        def fill2b(arr):
            def f(w2t):
                for dd in range(2):
                    kb.dma("sp", w2t, w2t[dd * 64:(dd + 1) * 64, :], i[arr], i[arr][j, dd])
            return f

        with contextlib.ExitStack() as stb:
            kb.barrier()
            bw = [kb.sb(stb, f"bw{k}", [128, D]) for k in range(2)]
            ba = [kb.sb(stb, f"ba{k}", [128, D]) for k in range(2)]
            for dd in range(2):
                self.load_bc(bw[dd], i["c_w0"], i["c_w0"][j, dd, :])
                self.load_bc(ba[dd], i["c_a0"], i["c_a0"][j, dd, :])

            def epi_w(dd, nb, ps, O, tmp):
                sl = slice(nb * 512, (nb + 1) * 512)
                kb.op("dve", lambda e: e.tensor_tensor(tmp[:], ps[:], bw[dd][:, sl], op=ALU.add), R=[ps, bw[dd]], W=[tmp])
                kb.op("act", lambda e: e.activation(tmp[:], tmp[:], AF.Sigmoid), R=[tmp], W=[tmp])
                kb.op("act", lambda e: e.activation(O[:, sl], tmp[:], AF.Exp, scale=-float(np.exp(-0.5))), R=[tmp], P=[O])
            epi_w.dst = lambda dd: (d["DEC"], d["DEC"][dd])

            def epi_a(dd, nb, ps, O, tmp):
                sl = slice(nb * 512, (nb + 1) * 512)
                kb.op("dve", lambda e: e.tensor_tensor(tmp[:], ps[:], ba[dd][:, sl], op=ALU.add), R=[ps, ba[dd]], W=[tmp])
                kb.op("act", lambda e: e.activation(O[:, sl], tmp[:], AF.Sigmoid), R=[tmp], P=[O])
            epi_a.dst = lambda dd: (d["AA"], d["AA"][dd])

            def epi_g(dd, nb, ps, O, tmp):
                sl = slice(nb * 512, (nb + 1) * 512)
                kb.op("act", lambda e: e.activation(O[:, sl], ps[:], AF.Copy), R=[ps], P=[O])
            epi_g.dst = lambda dd: (d["GG"], d["GG"])

            lora(1, fill2("c_w1"), AF.Tanh, fill2b("c_w2"), 2, epi_w)
            lora(4, fill2("c_a1"), None, fill2b("c_a2"), 2, epi_a)
            lora(5, lambda wt: kb.dma("sp", wt, wt[:], i["c_g1"], i["c_g1"][j].rearrange("(k p) n -> p k n", p=128), part=False),
                 AF.Sigmoid, lambda w2t: kb.dma("sp", w2t, w2t[:], i["c_g2"], i["c_g2"][j], part=False), 1, epi_g)
        if dbg == 2:
            return
        with contextlib.ExitStack() as st:
            kb.barrier()
            kkb = kb.sb(st, "kkb", [128, D])
            kab = kb.sb(st, "kab", [128, D])
            rkb = [kb.sb(st, f"rkb{k}", [128, D]) for k in range(2)]
            self.load_bc(kkb, i["c_k_k"], i["c_k_k"][j, :])
            self.load_bc(kab, i["c_k_a"], i["c_k_a"][j, :])
            for dd in range(2):
                self.load_bc(rkb[dd], i["c_r_k"], i["c_r_k"][j, dd, :])
            tk = kb.sb(st, "tk", [128, D])
            tr = kb.sb(st, "tr", [128, D])
            ta = [kb.sb(st, f"ta{k}", [128, D]) for k in range(2)]
            tw = [kb.sb(st, f"tw{k}", [128, D]) for k in range(2)]
            kk = kb.sb(st, "kk", [128, D])
            t1 = kb.sb(st, "t1", [128, D])
            t2 = kb.sb(st, "t2", [128, D])
            t3 = kb.sb(st, "t3", [128, D])
            t4 = kb.sb(st, "t4", [128, D])
            ss = kb.sb(st, "ss", [128, 16])
            rs = kb.sb(st, "rs", [128, 16])
            sc = kb.sb(st, "sc", [128, 96])
            hv = lambda b: b[:].rearrange("p (h e) -> p h e", h=16)
            for n, t in enumerate(alltiles):
                rows = slice(t * 128, (t + 1) * 128)
                kb.dma("sp", tk, tk[:], d["KK"], d["KK"][rows, :], part=False)
                kb.dma("sp", tr, tr[:], d["RR"], d["RR"][rows, :], part=False)
                for dd in range(2):
                    kb.dma("sp", ta[dd], ta[dd][:], d["AA"], d["AA"][dd, rows, :], part=False)
                    kb.dma("sp", tw[dd], tw[dd][:], d["DEC"], d["DEC"][dd, rows, :], part=False)
                kb.op("pool", lambda e: e.tensor_tensor(kk[:], tk[:], kkb[:], op=ALU.mult), R=[tk, kkb], W=[kk])
                kb.op("dve", lambda e: e.tensor_tensor(t1[:], kk[:], kk[:], op=ALU.mult), R=[kk], W=[t1])
                kb.op("dve", lambda e: e.tensor_reduce(ss[:], hv(t1), axis=AX.X, op=ALU.add), R=[t1], W=[ss])
                kb.op("dve", lambda e: e.tensor_scalar(rs[:], ss[:], 1e-24, None, op0=ALU.max), R=[ss], W=[rs])
                kb.op("act", lambda e: e.activation(rs[:], rs[:], AF.Sqrt), R=[rs], W=[rs])
                kb.op("dve", lambda e: e.reciprocal(rs[:], rs[:]), R=[rs], W=[rs])
                kb.op("dve", lambda e: e.tensor_tensor(hv(kk), hv(kk), bc_last(rs[:, :], 64), op=ALU.mult), R=[kk, rs], W=[kk])
                kb.op("act", lambda e: e.activation(t1[:], kk[:], AF.Copy, scale=-1.0), R=[kk], W=[t1])
                kb.dma("sp", d["AV"], d["AV"][rows, :], t1, t1[:])
                for dd in range(2):
                    kb.op("dve", lambda e: e.scalar_tensor_tensor(t2[:], ta[dd][:], -1.0, kab[:], op0=ALU.add, op1=ALU.mult), R=[ta[dd], kab], W=[t2])
                    kb.op("dve", lambda e: e.scalar_tensor_tensor(t2[:], t2[:], 1.0, tk[:], op0=ALU.add, op1=ALU.mult), R=[t2, tk], W=[t2])
                    kb.dma("sp", d["KD"], d["KD"][dd, rows, :], t2, t2[:])
                    kb.op("pool", lambda e: e.tensor_tensor(t3[:], kk[:], ta[dd][:], op=ALU.mult), R=[kk, ta[dd]], W=[t3])
                    kb.dma("sp", d["BB"], d["BB"][dd, rows, :], t3, t3[:])
                    kb.op("pool", lambda e: e.tensor_tensor(t4[:], tr[:], tw[dd][:], op=ALU.mult), R=[tr, tw[dd]], W=[t4])
                    kb.dma("sp", d["RP"], d["RP"][dd, rows, :], t4, t4[:])
                    kb.op("pool", lambda e: e.tensor_tensor(t3[:], t3[:], tr[:], op=ALU.mult), R=[t3, tr], W=[t3])
                    kb.op("dve", lambda e: e.tensor_reduce(sc[:, dd * 16:(dd + 1) * 16], hv(t3), axis=AX.X, op=ALU.add), R=[t3], P=[sc])
                    kb.op("pool", lambda e: e.tensor_tensor(t2[:], t2[:], tr[:], op=ALU.mult), R=[t2, tr], W=[t2])
                    kb.op("dve", lambda e: e.tensor_reduce(sc[:, 32 + dd * 16:32 + (dd + 1) * 16], hv(t2), axis=AX.X, op=ALU.add), R=[t2], P=[sc])
                    kb.op("pool", lambda e: e.tensor_tensor(t2[:], t2[:], rkb[dd][:], op=ALU.mult), R=[t2, rkb[dd]], W=[t2])
                    kb.op("dve", lambda e: e.tensor_reduce(sc[:, 64 + dd * 16:64 + (dd + 1) * 16], hv(t2), axis=AX.X, op=ALU.add), R=[t2], P=[sc])
                kb.op("dve", lambda e: e.tensor_tensor(sc[:, 64:80], sc[:, 64:80], sc[:, 80:96], op=ALU.add), R=[sc], W=[sc])
                kb.dma("sp", d["SC"], d["SC"][rows, :], sc, sc[:, 0:80])
        if dbg == 3:
            return
        with contextlib.ExitStack() as st:
            kb.barrier()
            bd = kb.sb(st, "bd", [48, 512])
            kb.dma("sp", bd, bd[:], i["k_bd"], i["k_bd"][:], part=False)
            selc = kb.sb(st, "selc", [128, 64, 16])
            kb.dma("sp", selc, selc[:], i["k_sel"], i["k_sel"][:], part=False)

            def scan_dir(dd):
                svt = [kb.sb(st, f"svt{dd}{k}", [128, 512]) for k in range(2)]
                vr = kb.sb(st, f"VR{dd}", [128, 512])
                lt = kb.sb(st, f"LT{dd}", [128, 64, 32])
                wt_ = kb.sb(st, f"WT{dd}", [128, 64, 8])
                l2 = kb.sb(st, f"L2{dd}", [48, 64, 128])
                Mr = [kb.sb(st, f"Mr{dd}{k}", [48, 512]) for k in range(2)]
                stg = [kb.sb(st, f"stg{dd}{a}", [64, D]) for a in range(3)]
                po1 = kb.ps(st, f"spo1{dd}", [128, 512])
                pu = kb.ps(st, f"spu{dd}", [128, 512])
                pt = kb.ps(st, f"spt{dd}", [128, 512])
                kb.op("pool", lambda e: e.memset(lt[:], 0.0), W=[lt])
                kb.op("pool", lambda e: e.memset(l2[:], 0.0), W=[l2])
                kb.op("pool", lambda e: e.memset(svt[0][:], 0.0), W=[svt[0]])
                yield
                p = 0
                nstep = 0
                nchunk = 0
                for (lo, hi) in [(S, T), (0, S)]:
                    c0s = list(range(lo, hi, 64))
                    if dd == 1:
                        c0s = c0s[::-1]
                    for c0 in c0s:
                        if dbg == 41 and nchunk >= 1:
                            break
                        nchunk += 1
                        srcs = [(d["AV"], d["AV"][c0:c0 + 64, :]), (d["RP"], d["RP"][dd, c0:c0 + 64, :]), (d["DEC"], d["DEC"][dd, c0:c0 + 64, :])]
                        for a, (sb_, sap) in enumerate(srcs):
                            kb.dma("sp", stg[a], stg[a][:], sb_, sap, part=False)
                        for (r0, arr) in ((0, "BB"), (32, "KD")):
                            for hg in range(2):
                                kb.dma("sp", l2, l2[r0 + hg * 8:r0 + hg * 8 + 8, :, hg * 64:(hg + 1) * 64], d[arr],
                                       d[arr][dd, c0:c0 + 64, :].rearrange("t (hp hg k) -> hg hp t k", hg=2, k=64)[hg])
                        for hg in range(2):
                            kb.dma("sp", vr, vr[hg * 64:(hg + 1) * 64, :].rearrange("t (hp v) -> t hp v", v=64), d["VV"],
                                   d["VV"][c0:c0 + 64, :].rearrange("t (hp hg v) -> hg t hp v", hg=2, v=64)[hg])
                        for a in range(3):
                            G = stg[a]
                            for hp in range(8):
                                kb.op("pe", lambda e: e.transpose(pt[:, hp * 64:(hp + 1) * 64], G[:, hp * 128:(hp + 1) * 128], self.ident[0:64, 0:64]),
                                      R=[G, self.ident], P=[pt])
                            if a < 2:
                                kb.op("act", lambda e: e.activation(lt[0:64, :, a * 16:a * 16 + 8], pt[0:64, :].rearrange("p (h t) -> p t h", h=8), AF.Copy),
                                      R=[pt], P=[lt])
                                kb.op("dve", lambda e: e.tensor_copy(lt[64:128, :, a * 16 + 8:a * 16 + 16], pt[64:128, :].rearrange("p (h t) -> p t h", h=8)),
                                      R=[pt], P=[lt])
                            else:
                                kb.op("act", lambda e: e.activation(wt_[:], pt[:].rearrange("p (h t) -> p t h", h=8), AF.Copy), R=[pt], W=[wt_])
                        yield
                        order = range(64) if dd == 0 else range(63, -1, -1)
                        for tl in order:
                            cur, nxt = p, 1 - p
                            M = Mr[nstep % 2]
                            nstep += 1
                            kb.op("pe", lambda e: e.matmul(po1[0:32, :], lt[:, tl, :], svt[cur][:], start=True, stop=True), R=[lt, svt[cur]], W=[po1])
                            kb.op("pe", lambda e: e.matmul(po1[32:48, :], selc[:, tl, :], vr[:], start=True, stop=True), R=[selc, vr], P=[po1])
                            kb.op("pool", lambda e: e.tensor_tensor(svt[nxt][:].rearrange("p (h e) -> p h e", h=8),
                                                                     svt[cur][:].rearrange("p (h e) -> p h e", h=8),
                                                                     bc_last(wt_[:, tl, :], 64), op=ALU.mult), R=[svt[cur], wt_], W=[svt[nxt]])
                            yield
                            kb.op("dve", lambda e: e.tensor_tensor(M[:], po1[0:48, :], bd[:], op=ALU.mult), R=[po1, bd], W=[M])
                            yield
                            kb.op("pe", lambda e: e.matmul(pu[:], l2[:, tl, :], M[:], start=True, stop=True), R=[l2, M], W=[pu])
                            yield
                            kb.op("dve", lambda e: e.tensor_tensor(svt[nxt][:], svt[nxt][:], pu[:], op=ALU.add), R=[svt[nxt], pu], W=[svt[nxt]])
                            tok = c0 + tl
                            qsb = d["QS"][dd][tok // 2048]
                            kb.dma("sp", qsb, qsb[tok % 2048, :].rearrange("(p n) -> p n", p=32), M, M[0:32, :])
                            p = nxt
                            yield

            gens = [scan_dir(0), scan_dir(1)]
            while gens:
                for g in list(gens):
                    try:
                        next(g)
                    except StopIteration:
                        gens.remove(g)
        if dbg in (4, 41):
            return
        tiles = list(range(NTL if last else NT))
        with contextlib.ExitStack() as st:
            kb.barrier()
            lw = kb.sb(st, "lnw", [128, D])
            lb = kb.sb(st, "lnb", [128, D])
            self.load_bc(lw, i["c_ln_w"], i["c_ln_w"][j, :])
            self.load_bc(lb, i["c_ln_b"], i["c_ln_b"][j, :])
            sa = [kb.sb(st, f"osa{k}", [128, D]) for k in range(2)]
            qq = [kb.sb(st, f"oqq{k}", [128, D]) for k in range(2)]
            tv = kb.sb(st, "otv", [128, D])
            tg = kb.sb(st, "otg", [128, D])
            sc = kb.sb(st, "osc", [128, 80])
            o = kb.sb(st, "oo", [128, D])
            t1 = kb.sb(st, "ot1", [128, D])
            m1 = kb.sb(st, "om1", [128, 16])
            m2 = kb.sb(st, "om2", [128, 16])
            hv = lambda b: b[:].rearrange("p (h e) -> p h e", h=16)
            for n, t in enumerate(tiles):
                rows = slice(t * 128, (t + 1) * 128)
                for dd in range(2):
                    qsb = d["QS"][dd][(t * 128) // 2048]
                    r0 = (t * 128) % 2048
                    qv = qsb[r0:r0 + 128, :].rearrange("t (ty g q v) -> t ty g q v", ty=2, g=2, q=64, v=64)
                    for ty, dstb in ((0, sa[dd]), (1, qq[dd])):
                        dv = dstb[:].rearrange("p (hp hg v) -> p hg hp v", hg=2, v=64)
                        for hg in range(2):
                            kb.dma("sp", dstb, dv[:, hg], qsb, qv[:, ty, hg, ::9, :])
                kb.dma("sp", tv, tv[:], d["VV"], d["VV"][rows, :], part=False)
                kb.dma("sp", tg, tg[:], d["GG"], d["GG"][rows, :], part=False)
                kb.dma("sp", sc, sc[:], d["SC"], d["SC"][rows, :], part=False)
                kb.op("pool", lambda e: e.tensor_tensor(o[:], qq[0][:], qq[1][:], op=ALU.add), R=[qq[0], qq[1]], W=[o])
                for dd in range(2):
                    kb.op("dve", lambda e: e.tensor_tensor(hv(t1), hv(sa[dd]), bc_last(sc[:, dd * 16:(dd + 1) * 16], 64), op=ALU.mult), R=[sa[dd], sc], W=[t1])
                    kb.op("pool", lambda e: e.tensor_tensor(o[:], o[:], t1[:], op=ALU.add), R=[o, t1], W=[o])
                kb.op("dve", lambda e: e.tensor_tensor(m1[:], sc[:, 32:48], sc[:, 48:64], op=ALU.add), R=[sc], W=[m1])
                kb.op("dve", lambda e: e.tensor_tensor(hv(t1), hv(tv), bc_last(m1[:, :], 64), op=ALU.mult), R=[tv, m1], W=[t1])
                kb.op("pool", lambda e: e.tensor_tensor(o[:], o[:], t1[:], op=ALU.add), R=[o, t1], W=[o])
                kb.op("dve", lambda e: e.tensor_reduce(m1[:], hv(o), axis=AX.X, op=ALU.add), R=[o], W=[m1])
                kb.op("dve", lambda e: e.tensor_scalar(m1[:], m1[:], -1.0 / 64, None, op0=ALU.mult), R=[m1], W=[m1])
                kb.op("dve", lambda e: e.tensor_tensor(hv(o), hv(o), bc_last(m1[:, :], 64), op=ALU.add), R=[o, m1], W=[o])
                kb.op("pool", lambda e: e.tensor_tensor(t1[:], o[:], o[:], op=ALU.mult), R=[o], W=[t1])
                kb.op("dve", lambda e: e.tensor_reduce(m2[:], hv(t1), axis=AX.X, op=ALU.add), R=[t1], W=[m2])
                kb.op("dve", lambda e: e.tensor_scalar(m2[:], m2[:], 1.0 / 64, 64 * 1e-5, op0=ALU.mult, op1=ALU.add), R=[m2], W=[m2])
                kb.op("act", lambda e: e.activation(m2[:], m2[:], AF.Sqrt), R=[m2], W=[m2])
                kb.op("dve", lambda e: e.reciprocal(m2[:], m2[:]), R=[m2], W=[m2])
                kb.op("dve", lambda e: e.tensor_tensor(hv(o), hv(o), bc_last(m2[:, :], 64), op=ALU.mult), R=[o, m2], W=[o])
                kb.op("pool", lambda e: e.tensor_tensor(o[:], o[:], lw[:], op=ALU.mult), R=[o, lw], W=[o])
                kb.op("dve", lambda e: e.tensor_tensor(o[:], o[:], lb[:], op=ALU.add), R=[o, lb], W=[o])
                kb.op("dve", lambda e: e.tensor_tensor(hv(t1), hv(tv), bc_last(sc[:, 64:80], 64), op=ALU.mult), R=[tv, sc], W=[t1])
                kb.op("pool", lambda e: e.tensor_tensor(o[:], o[:], t1[:], op=ALU.add), R=[o, t1], W=[o])
                kb.op("dve", lambda e: e.tensor_tensor(o[:], o[:], tg[:], op=ALU.mult), R=[o, tg], W=[o])
                kb.dma("sp", d["O"], d["O"][rows, :], o, o[:])
        self.phase_out_proj(l, i["c_w_o"], i["c_w_o"][j], last)

    def phase_moe(self, l, last):
        kb, i, d, S, C, T, FF = self.kb, self.i, self.d, self.S, self.C, self.T, self.FF
        capl, capc = 2 * S // NE, 2 * C // NE
        tiles = list(range(self.NTL if last else self.NT))
        self.phase_norm(l, 3, 4, tiles, d["H"])
        chunks = [(0, s0, min(128, capl - s0)) for s0 in range(0, capl, 128)]
        if not last:
            chunks += [(1, s0, min(128, capc - s0)) for s0 in range(0, capc, 128)]
        NCH = len(chunks)
        NSL = NCH * 128
        offs = [sum(c[2] for c in chunks[:k]) for k in range(NCH)]
        NSC = sum(c[2] for c in chunks)
        with contextlib.ExitStack() as sto:
            kb.barrier()
            idxT = kb.sb(sto, "idxT", [128, NCH, NE], I32)
            gateT = kb.sb(sto, "gateT", [128, NCH, NE])
            with contextlib.ExitStack() as st:
                kb.barrier()
                affT = kb.sb(st, "affT", [NE, T])
                mx = kb.sb(st, "rmx", [128, 1])
                sm = kb.sb(st, "rsm", [128, 1])
                ex = kb.sb(st, "rex", [128, NE])
                pa = kb.ps(st, "rpa", [NE, 128])

                def post(n, t, Y):
                    kb.op("dve", lambda e: e.tensor_reduce(mx[:], Y[:, 0:NE], axis=AX.X, op=ALU.max), R=[Y], W=[mx])
                    kb.op("dve", lambda e: e.tensor_scalar(mx[:], mx[:], -1.0, None, op0=ALU.mult), R=[mx], W=[mx])
                    kb.op("act", lambda e: e.activation(ex[:], Y[:, 0:NE], AF.Exp, bias=mx[:, 0:1], scale=1.0, accum_out=sm[:]), R=[Y, mx], W=[ex, sm])
                    kb.op("dve", lambda e: e.reciprocal(sm[:], sm[:]), R=[sm], W=[sm])
                    kb.op("dve", lambda e: e.tensor_scalar(ex[:], ex[:], sm[:, 0:1], None, op0=ALU.mult), R=[ex, sm], W=[ex])
                    kb.op("pe", lambda e: e.transpose(pa[:], ex[:], self.ident[:]), R=[ex, self.ident], W=[pa])
                    kb.op("dve", lambda e: e.tensor_copy(affT[:, t * 128:(t + 1) * 128], pa[:]), R=[pa], P=[affT])

                with contextlib.ExitStack() as st2:
                    kb.barrier()
                    self.phase_linear(st2, d["H"], i["router_w"], i["router_w"][l], NE, tiles, post)
                kb.barrier()
                vals = kb.sb(st, "tvals", [NE, NSL])
                idxf = kb.sb(st, "tidxf", [NE, NSL])
                idxu = kb.sb(st, "tidxu", [NE, 8], U32)
                kb.op("dve", lambda e: e.memset(vals[:], 0.0), W=[vals])
                kb.op("dve", lambda e: e.memset(idxf[:], 0.0), W=[idxf])
                for (isc, s0, cnt), ci in zip(chunks, range(NCH)):
                    lo, hi = (S, T) if isc else (0, S)
                    for it in range(cnt // 8):
                        col = ci * 128 + it * 8
                        work = affT[:, lo:hi]
                        kb.op("dve", lambda e: e.max(out=vals[:, col:col + 8], in_=work), R=[affT], P=[vals])
                        kb.op("dve", lambda e: e.max_index(out=idxu[:], in_max=vals[:, col:col + 8], in_values=work), R=[affT, vals], W=[idxu])
                        kb.op("dve", lambda e: e.tensor_copy(idxf[:, col:col + 8], idxu[:]), R=[idxu], P=[idxf])
                        kb.op("dve", lambda e: e.match_replace(out=work, in_to_replace=vals[:, col:col + 8], in_values=work, imm_value=-1.0),
                              R=[vals], W=[affT])
                    if isc:
                        kb.op("dve", lambda e: e.tensor_scalar(idxf[:, ci * 128:ci * 128 + cnt], idxf[:, ci * 128:ci * 128 + cnt], float(S), None, op0=ALU.add),
                              R=[idxf], W=[idxf])
                padf = kb.sb(st, "padf", [128, 1])
                kb.dma("sp", padf, padf[:], i["k_pad"], i["k_pad"][:], part=False)
                idxTf = kb.sb(st, "idxTf", [128, NCH, NE])
                kb.op("dve", lambda e: e.tensor_copy(idxTf[:].rearrange("p a b -> p (a b)"), padf[:, 0:1].to_broadcast([128, NCH * NE])), R=[padf], W=[idxTf])
                kb.op("dve", lambda e: e.memset(gateT[:], 0.0), W=[gateT])
                pt = kb.ps(st, "tpt", [128, 2 * NE])
                for (isc, s0, cnt), ci in zip(chunks, range(NCH)):
                    kb.op("pe", lambda e: e.transpose(pt[:, 0:NE], idxf[:, ci * 128:(ci + 1) * 128], self.ident[0:NE, 0:NE]), R=[idxf, self.ident], P=[pt])
                    kb.op("pe", lambda e: e.transpose(pt[:, NE:2 * NE], vals[:, ci * 128:(ci + 1) * 128], self.ident[0:NE, 0:NE]), R=[vals, self.ident], P=[pt])
                    kb.op("dve", lambda e: e.tensor_copy(idxTf[0:cnt, ci, :], pt[0:cnt, 0:NE]), R=[pt], W=[idxTf])
                    kb.op("dve", lambda e: e.tensor_copy(gateT[0:cnt, ci, :], pt[0:cnt, NE:2 * NE]), R=[pt], W=[gateT])
                kb.op("dve", lambda e: e.tensor_copy(idxT[:], idxTf[:]), R=[idxTf], W=[idxT])
            if self.cfg.get('dbg', 99) == 5:
                return
            with contextlib.ExitStack() as st:
                kb.barrier()
                NF = FF // 128
                FB = min(256, FF)
                NFB = FF // FB
                gm = {}
                for row in ((0,) if last else (0, 1)):
                    g = kb.sb(st, f"mg{row}", [128, D])
                    self.load_bc(g, d["MODV"], self.mod_row(l, row, 5))
                    gm[row] = g
                xg = [kb.sb(st, f"xg{k}", [128, D]) for k in range(2)]
                xsT = kb.sb(st, "xsT", [128, 8, NSC])
                hT = kb.sb(st, "hT", [128, NF, NSC])
                w1 = [kb.sb(st, f"w1_{k}", [128, 8, FB]) for k in range(2)]
                w3 = [kb.sb(st, f"w3_{k}", [128, 8, FB]) for k in range(2)]
                w2 = [kb.sb(st, f"w2_{k}", [128, NF, 256]) for k in range(2)]
                sg = kb.sb(st, "sg", [128, 512])
                yo = [kb.sb(st, f"yo{k}", [128, D]) for k in range(NCH)]
                xr = [kb.sb(st, f"xr{k}", [128, D]) for k in range(2)]
                ptr = [kb.ps(st, f"mpt{k}", [128, 512]) for k in range(2)]
                p1 = [kb.ps(st, f"mp1{k}", [128, 512]) for k in range(2)]
                p3 = [kb.ps(st, f"mp3{k}", [128, 512]) for k in range(2)]
                py = [kb.ps(st, f"mpy{k}", [128, 512]) for k in range(2)]
                nw = 0
                nw2 = 0
                ng = 0
                npp = 0
                cgs = [(c0, min(512, NSC - c0)) for c0 in range(0, NSC, 512)]
                for Yo in yo:
                    kb.op("pool", lambda e: e.memset(Yo[:], 0.0), W=[Yo])
                for ex in range(NE):
                    for ci in range(NCH):
                        G = xg[ng % 2]
                        ng += 1
                        kb.op("pool", lambda e: e.indirect_dma_start(out=G[:, :], out_offset=None, in_=d["H"][:, :],
                                                                      in_offset=bass.IndirectOffsetOnAxis(ap=idxT[:, ci, ex:ex + 1], axis=0)),
                              R=[d["H"], idxT], W=[G], dma=True)
                        for k in range(8):
                            p = ptr[k // 4]
                            kb.op("pe", lambda e: e.transpose(p[:, (k % 4) * 128:(k % 4 + 1) * 128], G[:, k * 128:(k + 1) * 128], self.ident[:]),
                                  R=[G, self.ident], P=[p])
                        for jj in range(2):
                            if jj == 0:
                                kb.op("act", lambda e: e.activation(xsT[:, 0:4, offs[ci]:offs[ci] + chunks[ci][2]], ptr[0][:].rearrange("p (a b) -> p a b", a=4)[:, :, 0:chunks[ci][2]], AF.Copy),
                                      R=[ptr[0]], P=[xsT])
                            else:
                                kb.op("dve", lambda e: e.tensor_copy(xsT[:, 4:8, offs[ci]:offs[ci] + chunks[ci][2]], ptr[1][:].rearrange("p (a b) -> p a b", a=4)[:, :, 0:chunks[ci][2]]),
                                      R=[ptr[1]], P=[xsT])
                    for fb in range(NFB):
                        W1, W3 = w1[nw % 2], w3[nw % 2]
                        nw += 1
                        kb.dma("sp", W1, W1[:], i["ffn_w1"], i["ffn_w1"][l, ex, :, fb * FB:(fb + 1) * FB].rearrange("(k p) f -> p k f", p=128), part=False)
                        kb.dma("sp", W3, W3[:], i["ffn_w3"], i["ffn_w3"][l, ex, :, fb * FB:(fb + 1) * FB].rearrange("(k p) f -> p k f", p=128), part=False)
                        for fc in range(FB // 128):
                            f = fb * (FB // 128) + fc
                            for (c0, cw) in cgs:
                                P1, P3 = p1[npp % 2], p3[npp % 2]
                                npp += 1
                                for k in range(8):
                                    kb.op("pe", lambda e: e.matmul(P1[:, 0:cw], W1[:, k, fc * 128:(fc + 1) * 128], xsT[:, k, c0:c0 + cw], start=(k == 0), stop=(k == 7)),
                                          R=[W1, xsT], P=[P1])
                                for k in range(8):
                                    kb.op("pe", lambda e: e.matmul(P3[:, 0:cw], W3[:, k, fc * 128:(fc + 1) * 128], xsT[:, k, c0:c0 + cw], start=(k == 0), stop=(k == 7)),
                                          R=[W3, xsT], P=[P3])
                                kb.op("act", lambda e: e.activation(sg[:, 0:cw], P1[:, 0:cw], AF.Silu), R=[P1], W=[sg])
                                kb.op("dve", lambda e: e.tensor_tensor(hT[:, f, c0:c0 + cw], sg[:, 0:cw], P3[:, 0:cw], op=ALU.mult), R=[sg, P3], P=[hT])
                    for db in range(4):
                        W2 = w2[nw2 % 2]
                        nw2 += 1
                        kb.dma("sp", W2, W2[:], i["ffn_w2"], i["ffn_w2"][l, ex, :, db * 256:(db + 1) * 256].rearrange("(f p) n -> p f n", p=128), part=False)
                        for ci in range(NCH):
                            PY = py[(db * NCH + ci) % 2]
                            Yo = yo[ci]
                            cn = chunks[ci][2]
                            for f in range(NF):
                                kb.op("pe", lambda e: e.matmul(PY[0:cn, 0:256], hT[:, f, offs[ci]:offs[ci] + cn], W2[:, f, :], start=(f == 0), stop=(f == NF - 1)),
                                      R=[hT, W2], P=[PY])
                            g = gm[chunks[ci][0]]
                            kb.op("dve", lambda e: e.scalar_tensor_tensor(Yo[0:cn, db * 256:(db + 1) * 256], PY[0:cn, 0:256], gateT[0:cn, ci, ex:ex + 1], g[0:cn, db * 256:(db + 1) * 256],
                                                                          op0=ALU.mult, op1=ALU.mult), R=[PY, gateT, g], P=[Yo])
                            if db == 3:
                                Xr = xr[ci % 2]
                                kb.op("pool", lambda e: e.indirect_dma_start(out=Xr[:, :], out_offset=None, in_=d["X"][:, :],
                                                                              in_offset=bass.IndirectOffsetOnAxis(ap=idxT[:, ci, ex:ex + 1], axis=0)),
                                      R=[d["X"], idxT], W=[Xr], dma=True)
                                kb.op("dve", lambda e: e.tensor_tensor(Xr[:], Xr[:], Yo[:], op=ALU.add), R=[Xr, Yo], W=[Xr])
                                kb.op("pool", lambda e: e.indirect_dma_start(out=d["X"][:, :], out_offset=bass.IndirectOffsetOnAxis(ap=idxT[:, ci, ex:ex + 1], axis=0),
                                                                              in_=Xr[:, :], in_offset=None),
                                      R=[Xr, idxT], W=[d["X"]], dma=True)

    def build(self):
        kb = self.kb
        self.declare()
        with contextlib.ExitStack() as gst:
            self.gst = gst
            self.phase_init()
            cnt = {0: 0, 1: 0, 2: 0}
            dbg = self.cfg.get("dbg", 99)
            for l, kind in enumerate(self.kinds):
                last = l == self.L - 1
                j = cnt[kind]
                cnt[kind] += 1
                if dbg >= 1:
                    self.phase_norm(l, 0, 1, list(range(self.NT)), self.d["H"])
                if kind == 0:
                    if dbg >= 2:
                        self.phase_qkv_A(l, j)
                    if dbg >= 3:
                        self.phase_attn_A(last)
                    if dbg >= 4:
                        self.phase_out_proj(l, self.i["a_w_o"], self.i["a_w_o"][j], last)
                elif kind == 1:
                    if dbg >= 2:
                        self.phase_qkv_B(l, j)
                    if dbg >= 3:
                        self.phase_attn_B(j, last)
                    if dbg >= 4:
                        self.phase_out_proj(l, self.i["b_w_o"], self.i["b_w_o"][j], last)
                else:
                    if dbg >= 2:
                        self.phase_rwkv(l, j, last, dbg)
                if dbg >= 5:
                    self.phase_moe(l, last)
            self.phase_norm(self.L - 1, 0, 0, list(range(self.NTL)), self.out, final=True)
            kb.finish([self.out])
        return kb.nc


def _constants(cfg):
    S, C = cfg["S"], cfg["C"]
    T = S + C
    k = {}
    k["k_ident"] = np.eye(128, dtype=np.float32)
    rows = np.repeat(np.arange(S // 64), 64).astype(np.float32)
    cols = np.tile(np.arange(64), S // 64).astype(np.float32)

    def tab(hd):
        nf = hd // 4
        inv = (np.float32(10000.0) ** (-np.arange(nf, dtype=np.float32) / np.float32(nf))).astype(np.float32)
        ang = np.concatenate([rows[:, None] * inv, cols[:, None] * inv], axis=-1).astype(np.float32)
        return np.concatenate([np.cos(ang), np.sin(ang)], axis=-1).astype(np.float32)

    k["k_ropeA"] = tab(128)
    k["k_ropeB"] = tab(64)
    jj, ii = np.meshgrid(np.arange(128), np.arange(128), indexing="ij")
    k["k_tri"] = np.stack([(jj >= ii), (jj <= ii)]).astype(np.float32)
    bd = np.zeros((48, 8, 64), np.float32)
    for r in range(48):
        bd[r, r % 8, :] = 1.0
    k["k_bd"] = bd.reshape(48, 512)
    sel = np.zeros((2, 64, 64, 2, 8), np.float32)
    for hg in range(2):
        for t in range(64):
            sel[hg, t, t, hg, :] = 1.0
    k["k_sel"] = sel.reshape(128, 64, 16)
    k["k_pad"] = (T + np.arange(128, dtype=np.float32)).reshape(128, 1)
    k["k_zero"] = np.zeros((128, D), np.float32)
    return k


_NC_CACHE = {}


def kernel(**inputs):
    cfg = CFG
    n = cfg["ncores"]
    key = repr(cfg)
    if key not in _NC_CACHE:
        _NC_CACHE[key] = Prog(cfg).build()
    nc = _NC_CACHE[key]
    consts = _constants(cfg)
    f = lambda a: np.ascontiguousarray(np.asarray(a, dtype=np.float32))
    shared = {}
    for name, a in inputs.items():
        if name in ("x", "c", "ctx"):
            continue
        a = f(a)
        if name in ("c_ctx", "final_norm"):
            a = a.reshape(1, D)
        if name == "c_r_k":
            a = a.reshape(a.shape[0], 2, D)
        shared[name] = a
    shared.update(consts)
    x, c, ctx = f(inputs["x"]), f(inputs["c"]), f(inputs["ctx"])
    in_maps = []
    for b in range(n):
        m = dict(shared)
        m["x"] = x[b]
        m["c"] = c[b:b + 1]
        m["ctx"] = ctx[b]
        in_maps.append(m)
    res = run_bass_kernel_spmd(nc, in_maps, core_ids=list(range(n)))
    return np.stack([np.asarray(r["out"]) for r in res.results], axis=0).astype(np.float32)
```

```python
import contextlib
import numpy as np
import concourse.bass as bass
import concourse.mybir as mybir
from concourse.bass_utils import run_bass_kernel_spmd

F32 = mybir.dt.float32
U32 = mybir.dt.uint32
I32 = mybir.dt.int32
ALU = mybir.AluOpType
AF = mybir.ActivationFunctionType
AX = mybir.AxisListType

D = 1024
NE = 16
EPS = 1e-6
CFG = dict(S=4096, C=256, FF=2048, kinds=[0, 1, 2, 0], ncores=8)


class Buf:
    __slots__ = ("t", "w", "r", "pr", "name")

    def __init__(self, t, name):
        self.t = t
        self.name = name
        self.w = []
        self.r = []
        self.pr = []

    def __getitem__(self, k):
        return self.t[k]


class KB:
    SEM_LIMIT = 30000
    N_DMA_SLOTS = 48

    def __init__(self):
        self.nc = bass.Bass("TRN2", target_bir_lowering=False)
        nc = self.nc
        self.es = contextlib.ExitStack()
        self.engs = {"pe": nc.tensor, "dve": nc.vector, "act": nc.scalar, "pool": nc.gpsimd, "sp": nc.sync}
        self.sem = {}
        self.cnt = {}
        self.nsem = 0
        for e in self.engs:
            self._new_sem(e)
        self.seen = {e: {} for e in self.engs}
        self.slots = []
        for i in range(self.N_DMA_SLOTS):
            s = self.es.enter_context(nc.semaphore(f"dq{i}"))
            self.slots.append([s, 0])
        self.slot_i = 0
        self.uid = 0
        self.ninst = 0

    def _new_sem(self, e):
        self.nsem += 1
        self.sem[e] = self.es.enter_context(self.nc.semaphore(f"s_{e}_{self.nsem}"))
        self.cnt[e] = 0

    def inp(self, name, shape, dtype=F32):
        return Buf(self.nc.dram_tensor(name, list(shape), dtype, kind="ExternalInput").ap(), name)

    def outp(self, name, shape, dtype=F32):
        return Buf(self.nc.dram_tensor(name, list(shape), dtype, kind="ExternalOutput").ap(), name)

    def dram(self, name, shape, dtype=F32):
        return Buf(self.nc.dram_tensor(name, list(shape), dtype, kind="Internal").ap(), name)

    def sb(self, stack, name, shape, dtype=F32):
        self.uid += 1
        t = stack.enter_context(self.nc.sbuf_tensor(f"{name}_{self.uid}", list(shape), dtype))
        return Buf(t, name)

    def ps(self, stack, name, shape, dtype=F32):
        self.uid += 1
        t = stack.enter_context(self.nc.psum_tensor(f"{name}_{self.uid}", list(shape), dtype))
        return Buf(t, name)

    def _wait(self, eng, sem, val):
        d = self.seen[eng]
        k = id(sem)
        if d.get(k, (None, 0))[1] >= val:
            return
        self.engs[eng].wait_ge(sem, val)
        d[k] = (sem, val)
        self.ninst += 1

    @staticmethod
    def _add(lst, ev):
        out = [x for x in lst if not (x[0] is ev[0] and x[1] <= ev[1])]
        out.append(ev)
        return out

    def op(self, eng, fn, R=(), W=(), P=(), dma=False):
        for b in R:
            for ev in b.w:
                self._wait(eng, ev[0], ev[1])
        for b in W:
            for ev in b.w + b.r + b.pr:
                self._wait(eng, ev[0], ev[1])
        for b in P:
            for ev in b.r + b.pr:
                self._wait(eng, ev[0], ev[1])
            for ev in b.w:
                if ev[2]:
                    self._wait(eng, ev[0], ev[1])
        if dma:
            slot = self.slots[self.slot_i]
            self.slot_i = (self.slot_i + 1) % len(self.slots)
            if slot[1] > 0:
                self._wait(eng, slot[0], slot[1])
            inst = fn(self.engs[eng])
            slot[1] += 16
            inst.then_inc(slot[0], 16)
            sem, val = slot[0], slot[1]
        else:
            if self.cnt[eng] >= self.SEM_LIMIT:
                self._new_sem(eng)
            inst = fn(self.engs[eng])
            self.cnt[eng] += 1
            sem, val = self.sem[eng], self.cnt[eng]
            inst.then_inc(sem, 1)
        self.ninst += 1
        for b in R:
            b.r = self._add(b.r, (sem, val))
        for b in W:
            b.w = [(sem, val, True)]
            b.r = []
            b.pr = []
        for b in P:
            if b.r:
                b.pr = b.r
                b.r = []
                b.w = []
            b.w = self._add(b.w, (sem, val, False))
        return (sem, val)

    def dma(self, eng, out_b, out_ap, in_b, in_ap, part=True, **kw):
        def f(e):
            return e.dma_start(out=out_ap, in_=in_ap, **kw)
        if part:
            return self.op(eng, f, R=[in_b], P=[out_b], dma=True)
        return self.op(eng, f, R=[in_b], W=[out_b], dma=True)

    def barrier(self):
        for e in self.engs:
            for e2 in self.engs:
                if e2 != e and self.cnt[e2] > 0:
                    self._wait(e, self.sem[e2], self.cnt[e2])
            for slot in self.slots:
                if slot[1] > 0:
                    self._wait(e, slot[0], slot[1])

    def finish(self, bufs):
        for b in bufs:
            for ev in b.w:
                self._wait("sp", ev[0], ev[1])
        self.es.close()
        return self.nc


def bc_last(ap, n):
    return ap.unsqueeze(2).to_broadcast([ap.shape[0], ap.shape[1], n])


def bc_mid(ap, n):
    return ap.unsqueeze(1).to_broadcast([ap.shape[0], n, ap.shape[1]])


class Prog:
    def __init__(self, cfg):
        self.cfg = cfg
        self.S, self.C, self.FF = cfg["S"], cfg["C"], cfg["FF"]
        self.T = self.S + self.C
        self.kinds = cfg["kinds"]
        self.L = len(self.kinds)
        self.NT = self.T // 128
        self.NTL = self.S // 128
        self.kb = KB()

    def rstd(self, st, ss, n, width, tmp):
        kb = self.kb
        kb.op("dve", lambda e: e.tensor_scalar(tmp[:, 0:n], ss[:, 0:n], 1.0 / width, EPS, op0=ALU.mult, op1=ALU.add),
              R=[ss], W=[tmp])
        kb.op("act", lambda e: e.activation(tmp[:, 0:n], tmp[:, 0:n], AF.Sqrt), R=[tmp], W=[tmp])
        kb.op("dve", lambda e: e.reciprocal(tmp[:, 0:n], tmp[:, 0:n]), R=[tmp], W=[tmp])

    def load_bc(self, dst, src_b, row_ap, plus_one=False):
        kb = self.kb
        kb.dma("sp", dst, dst[:], src_b, row_ap.partition_broadcast(128), part=False)
        if plus_one:
            kb.op("pool", lambda e: e.tensor_scalar(dst[:], dst[:], 1.0, None, op0=ALU.add), R=[dst], W=[dst])

    def declare(self):
        kb, S, C, T, L, FF = self.kb, self.S, self.C, self.T, self.L, self.FF
        nA, nB, nC = max(1, self.kinds.count(0)), max(1, self.kinds.count(1)), max(1, self.kinds.count(2))
        i = {}
        i["x"] = kb.inp("x", [S, D])
        i["c"] = kb.inp("c", [1, D])
        i["ctx"] = kb.inp("ctx", [C, D])
        i["c_ctx"] = kb.inp("c_ctx", [1, D])
        i["mod_w"] = kb.inp("mod_w", [L, D, 6 * D])
        i["mod_b"] = kb.inp("mod_b", [L, 6 * D])
        i["a_w_qkv"] = kb.inp("a_w_qkv", [nA, D, 1536])
        i["a_w_o"] = kb.inp("a_w_o", [nA, D, D])
        i["a_q_norm"] = kb.inp("a_q_norm", [nA, 128])
        i["a_k_norm"] = kb.inp("a_k_norm", [nA, 128])
        i["b_w_qkv"] = kb.inp("b_w_qkv", [nB, D, 1536])
        i["b_w_o"] = kb.inp("b_w_o", [nB, D, D])
        i["b_sink"] = kb.inp("b_sink", [nB, 16])
        i["c_mu"] = kb.inp("c_mu", [nC, 6, D])
        i["c_w_rkv"] = kb.inp("c_w_rkv", [nC, 3, D, D])
        i["c_w_o"] = kb.inp("c_w_o", [nC, D, D])
        i["c_w0"] = kb.inp("c_w0", [nC, 2, D])
        i["c_w1"] = kb.inp("c_w1", [nC, 2, D, 64])
        i["c_w2"] = kb.inp("c_w2", [nC, 2, 64, D])
        i["c_a0"] = kb.inp("c_a0", [nC, 2, D])
        i["c_a1"] = kb.inp("c_a1", [nC, 2, D, 64])
        i["c_a2"] = kb.inp("c_a2", [nC, 2, 64, D])
        i["c_g1"] = kb.inp("c_g1", [nC, D, 128])
        i["c_g2"] = kb.inp("c_g2", [nC, 128, D])
        i["c_k_k"] = kb.inp("c_k_k", [nC, D])
        i["c_k_a"] = kb.inp("c_k_a", [nC, D])
        i["c_r_k"] = kb.inp("c_r_k", [nC, 2, D])
        i["c_ln_w"] = kb.inp("c_ln_w", [nC, D])
        i["c_ln_b"] = kb.inp("c_ln_b", [nC, D])
        i["router_w"] = kb.inp("router_w", [L, D, NE])
        i["ffn_w1"] = kb.inp("ffn_w1", [L, NE, D, FF])
        i["ffn_w3"] = kb.inp("ffn_w3", [L, NE, D, FF])
        i["ffn_w2"] = kb.inp("ffn_w2", [L, NE, FF, D])
        i["final_norm"] = kb.inp("final_norm", [1, D])
        i["k_ident"] = kb.inp("k_ident", [128, 128])
        i["k_ropeA"] = kb.inp("k_ropeA", [S, 128])
        i["k_ropeB"] = kb.inp("k_ropeB", [S, 64])
        i["k_tri"] = kb.inp("k_tri", [2, 128, 128])
        i["k_bd"] = kb.inp("k_bd", [48, 512])
        i["k_sel"] = kb.inp("k_sel", [128, 64, 16])
        i["k_pad"] = kb.inp("k_pad", [128, 1])
        i["k_zero"] = kb.inp("k_zero", [128, D])
        self.i = i
        self.out = kb.outp("out", [S, D])
        d = {}
        d["X"] = kb.dram("X", [T + 128, D])
        d["H"] = kb.dram("H", [T + 128, D])
        d["MODV"] = kb.dram("MODV", [L, 2, 6 * D])
        d["QT"] = kb.dram("QT", [20, 128, T])
        d["V"] = kb.dram("V", [T, 256])
        d["O"] = kb.dram("O", [T, D])
        if 2 in self.kinds:
            d["XJ"] = kb.dram("XJ", [6, T, D])
            for nm in ("RR", "KK", "VV", "GG", "AV"):
                d[nm] = kb.dram(nm, [T, D])
            for nm in ("DEC", "AA", "BB", "KD", "RP"):
                d[nm] = kb.dram(nm, [2, T, D])
            d["SC"] = kb.dram("SC", [T, 80])
            self.QSB = 1024
            d["QS"] = [[kb.dram(f"QS{a}_{b}", [min(2048, T - b * 2048), 32 * 512]) for b in range((T + 2047) // 2048)] for a in range(2)]
        self.d = d

    def phase_init(self):
        kb, i, d, S, C, T = self.kb, self.i, self.d, self.S, self.C, self.T
        with contextlib.ExitStack() as st:
            kb.barrier()
            self.ident = kb.sb(self.gst, "ident", [128, 128])
            kb.dma("sp", self.ident, self.ident[:], i["k_ident"], i["k_ident"][:], part=False)
            tl = [kb.sb(st, f"cp{j}", [128, D]) for j in range(2)]
            for t in range(self.NT):
                b = tl[t % 2]
                src = i["x"][t * 128:(t + 1) * 128, :] if t < self.NTL else i["ctx"][(t - self.NTL) * 128:(t - self.NTL + 1) * 128, :]
                kb.dma("sp", b, b[:], i["x"] if t < self.NTL else i["ctx"], src, part=False)
                kb.dma("sp", d["X"], d["X"][t * 128:(t + 1) * 128, :], b, b[:])
            z = kb.sb(st, "z", [128, D])
            kb.dma("sp", z, z[:], i["k_zero"], i["k_zero"][:], part=False)
            kb.dma("sp", d["X"], d["X"][T:T + 128, :], z, z[:])
            kb.dma("sp", d["H"], d["H"][T:T + 128, :], z, z[:])
            cT = kb.sb(st, "cT", [128, 8, 2])
            kb.dma("sp", cT, cT[:, :, 0], i["c"], i["c"][0, :].rearrange("(k p) -> p k", p=128), allow_slow_non_contiguous=True)
            kb.dma("sp", cT, cT[:, :, 1], i["c_ctx"], i["c_ctx"][0, :].rearrange("(k p) -> p k", p=128), allow_slow_non_contiguous=True)
            kb.op("act", lambda e: e.activation(cT[:], cT[:], AF.Silu), R=[cT], W=[cT])
            wts = [kb.sb(st, f"mw{j}", [128, 8, 512]) for j in range(2)]
            mb = kb.sb(st, "mb", [2, 6 * D])
            mv = kb.sb(st, "mv", [2, 6 * D])
            pm = [kb.ps(st, f"pm{j}", [2, 512]) for j in range(2)]
            n = 0
            for l in range(self.L):
                kb.dma("sp", mb, mb[:], i["mod_b"], i["mod_b"][l, :].partition_broadcast(2), part=False)
                for nb in range(12):
                    wt = wts[n % 2]
                    p = pm[n % 2]
                    n += 1
                    kb.dma("sp", wt, wt[:], i["mod_w"], i["mod_w"][l, :, nb * 512:(nb + 1) * 512].rearrange("(k p) n -> p k n", p=128), part=False)
                    for k in range(8):
                        kb.op("pe", lambda e: e.matmul(p[:], cT[:, k, :], wt[:, k, :], start=(k == 0), stop=(k == 7)), R=[cT, wt], P=[p])
                    kb.op("dve", lambda e: e.tensor_tensor(mv[:, nb * 512:(nb + 1) * 512], p[:], mb[:, nb * 512:(nb + 1) * 512], op=ALU.add),
                          R=[p, mb], P=[mv])
                kb.dma("sp", d["MODV"], d["MODV"][l], mv, mv[:])

    def mod_row(self, l, row, idx):
        return self.d["MODV"][l, row, idx * D:(idx + 1) * D]

    def phase_norm(self, l, sh_idx, sc_idx, tiles, dst, final=False):
        kb, d = self.kb, self.d
        with contextlib.ExitStack() as st:
            kb.barrier()
            if final:
                fw = kb.sb(st, "fw", [128, D])
                self.load_bc(fw, self.i["final_norm"], self.i["final_norm"][0, :])
                bc = {0: (None, fw)}
            else:
                bc = {}
                for row in (0, 1):
                    sh = kb.sb(st, f"sh{row}", [128, D])
                    sc = kb.sb(st, f"sc{row}", [128, D])
                    self.load_bc(sh, d["MODV"], self.mod_row(l, row, sh_idx))
                    self.load_bc(sc, d["MODV"], self.mod_row(l, row, sc_idx), plus_one=True)
                    bc[row] = (sh, sc)
            xt = [kb.sb(st, f"nx{j}", [128, D]) for j in range(2)]
            ht = [kb.sb(st, f"nh{j}", [128, D]) for j in range(2)]
            sqs = [kb.sb(st, f"nsq{j}", [128, D]) for j in range(2)]
            sss = [kb.sb(st, f"nss{j}", [128, 1]) for j in range(2)]
            rss = [kb.sb(st, f"nrs{j}", [128, 1]) for j in range(2)]
            if tiles:
                kb.dma("sp", xt[0], xt[0][:], d["X"], d["X"][tiles[0] * 128:(tiles[0] + 1) * 128, :], part=False)
            for n, t in enumerate(tiles):
                X, Hh = xt[n % 2], ht[n % 2]
                sh, sc = bc[0 if t < self.NTL else 1]
                if n + 1 < len(tiles):
                    tn = tiles[n + 1]
                    Xn = xt[(n + 1) % 2]
                    kb.dma("sp", Xn, Xn[:], d["X"], d["X"][tn * 128:(tn + 1) * 128, :], part=False)
                sq, ss, rs = sqs[n % 2], sss[n % 2], rss[n % 2]
                kb.op("act", lambda e: e.activation(sq[:], X[:], AF.Square, accum_out=ss[:]), R=[X], W=[sq, ss])
                self.rstd(st, ss, 1, D, rs)
                kb.op("dve", lambda e: e.scalar_tensor_tensor(Hh[:], X[:], rs[:, 0:1], sc[:], op0=ALU.mult, op1=ALU.mult),
                      R=[X, rs, sc], W=[Hh])
                if sh is not None:
                    kb.op("pool", lambda e: e.tensor_tensor(Hh[:], Hh[:], sh[:], op=ALU.add), R=[Hh, sh], W=[Hh])
                kb.dma("sp", dst, dst[t * 128:(t + 1) * 128, :], Hh, Hh[:])

    def phase_linear(self, st, src, w_b, w_ap, N, tiles, post, wt=None, src_ap=None):
        kb = self.kb
        nb = (N + 511) // 512
        if wt is None:
            wt = kb.sb(st, "lw", [128, 8, N])
            kb.dma("sp", wt, wt[:], w_b, w_ap.rearrange("(k p) n -> p k n", p=128), part=False)
        xin = [kb.sb(st, f"lx{j}", [128, D]) for j in range(3)]
        xTs = [kb.sb(st, f"lxT{j}", [128, 8, 128]) for j in range(2)]
        ys = [kb.sb(st, f"ly{j}", [128, N]) for j in range(2)]
        ptr = [kb.ps(st, f"lpt{j}", [128, 512]) for j in range(2)]
        po = [kb.ps(st, f"lpo{j}", [128, 512]) for j in range(nb)]
        sap = src_ap if src_ap is not None else src.t
        for n0 in range(min(2, len(tiles))):
            kb.dma("sp", xin[n0], xin[n0][:], src, sap[tiles[n0] * 128:(tiles[n0] + 1) * 128, :], part=False)
        if tiles:
            self.transpose8(xin[0], xTs[0], ptr)
        for n, t in enumerate(tiles):
            Y = ys[n % 2]
            xT = xTs[n % 2]
            if n + 2 < len(tiles):
                tn = tiles[n + 2]
                Xn = xin[(n + 2) % 3]
                kb.dma("sp", Xn, Xn[:], src, sap[tn * 128:(tn + 1) * 128, :], part=False)
            if n + 1 < len(tiles):
                self.transpose8(xin[(n + 1) % 3], xTs[(n + 1) % 2], ptr)
            for j in range(nb):
                w = min(512, N - j * 512)
                for k in range(8):
                    kb.op("pe", lambda e: e.matmul(po[j][:, 0:w], xT[:, k, :], wt[:, k, j * 512:j * 512 + w], start=(k == 0), stop=(k == 7)),
                          R=[xT, wt], P=[po[j]])
                eng = "act" if j % 2 == 0 else "dve"
                if eng == "act":
                    kb.op("act", lambda e: e.activation(Y[:, j * 512:j * 512 + w], po[j][:, 0:w], AF.Copy), R=[po[j]], P=[Y])
                else:
                    kb.op("dve", lambda e: e.tensor_copy(Y[:, j * 512:j * 512 + w], po[j][:, 0:w]), R=[po[j]], P=[Y])
            post(n, t, Y)

    def transpose8(self, X, xT, ptr, nblk=8):
        kb = self.kb
        for k in range(nblk):
            p = ptr[k // 4]
            kb.op("pe", lambda e: e.transpose(p[:, (k % 4) * 128:(k % 4 + 1) * 128], X[:, k * 128:(k + 1) * 128], self.ident[:]),
                  R=[X, self.ident], P=[p])
        for j in range((nblk + 3) // 4):
            nn = min(4, nblk - j * 4)
            if j % 2 == 0:
                kb.op("act", lambda e: e.activation(xT[:, j * 4:j * 4 + nn, :], ptr[j][:, 0:nn * 128].rearrange("p (a b) -> p a b", a=nn), AF.Copy),
                      R=[ptr[j]], P=[xT])
            else:
                kb.op("dve", lambda e: e.tensor_copy(xT[:, j * 4:j * 4 + nn, :], ptr[j][:, 0:nn * 128].rearrange("p (a b) -> p a b", a=nn)),
                      R=[ptr[j]], P=[xT])

    def phase_qkv_A(self, l, j):
        kb, i, d = self.kb, self.i, self.d
        with contextlib.ExitStack() as st:
            kb.barrier()
            gq = kb.sb(st, "gq", [128, 128])
            gk = kb.sb(st, "gk", [128, 128])
            self.load_bc(gq, i["a_q_norm"], i["a_q_norm"][j, :])
            self.load_bc(gk, i["a_k_norm"], i["a_k_norm"][j, :])
            sq = kb.sb(st, "asq", [128, 1280])
            ss = kb.sb(st, "ass", [128, 10])
            rs = kb.sb(st, "ars", [128, 10])
            cs = [kb.sb(st, f"acs{k}", [128, 128]) for k in range(2)]
            t1 = kb.sb(st, "at1", [128, 10, 64])
            t2 = kb.sb(st, "at2", [128, 10, 64])
            yr = kb.sb(st, "ayr", [128, 1280])
            qT = kb.sb(st, "aqT", [128, 10, 128])
            ptr = [kb.ps(st, f"apt{k}", [128, 512]) for k in range(3)]

            def post(n, t, Y):
                lat = t < self.NTL
                yv = Y[:, 0:1280].rearrange("p (h e) -> p h e", h=10)
                kb.op("pool", lambda e: e.tensor_tensor(sq[:], Y[:, 0:1280], Y[:, 0:1280], op=ALU.mult), R=[Y], W=[sq])
                kb.op("dve", lambda e: e.tensor_reduce(ss[:], sq[:].rearrange("p (h e) -> p h e", h=10), axis=AX.X, op=ALU.add), R=[sq], W=[ss])
                self.rstd(st, ss, 10, 128, rs)
                kb.op("dve", lambda e: e.tensor_tensor(yv, yv, bc_last(rs[:, :], 128), op=ALU.mult), R=[Y, rs], W=[Y])
                kb.op("pool", lambda e: e.tensor_tensor(yv[:, 0:8, :], yv[:, 0:8, :], bc_mid(gq[:, :], 8), op=ALU.mult), R=[Y, gq], W=[Y])
                kb.op("pool", lambda e: e.tensor_tensor(yv[:, 8:10, :], yv[:, 8:10, :], bc_mid(gk[:, :], 2), op=ALU.mult), R=[Y, gk], W=[Y])
                if lat:
                    c_ = cs[n % 2]
                    kb.dma("sp", c_, c_[:], i["k_ropeA"], i["k_ropeA"][t * 128:(t + 1) * 128, :], part=False)
                    x1, x2 = yv[:, :, 0:64], yv[:, :, 64:128]
                    yrv = yr[:].rearrange("p (h e) -> p h e", h=10)
                    cosb, sinb = bc_mid(c_[:, 0:64], 10), bc_mid(c_[:, 64:128], 10)
                    kb.op("dve", lambda e: e.tensor_tensor(t1[:], x1, cosb, op=ALU.mult), R=[Y, c_], W=[t1])
                    kb.op("pool", lambda e: e.tensor_tensor(t2[:], x2, sinb, op=ALU.mult), R=[Y, c_], W=[t2])
                    kb.op("dve", lambda e: e.tensor_tensor(yrv[:, :, 0:64], t1[:], t2[:], op=ALU.subtract), R=[t1, t2], W=[yr])
                    kb.op("pool", lambda e: e.tensor_tensor(t1[:], x1, sinb, op=ALU.mult), R=[Y, c_], W=[t1])
                    kb.op("dve", lambda e: e.tensor_tensor(t2[:], x2, cosb, op=ALU.mult), R=[Y, c_], W=[t2])
                    kb.op("pool", lambda e: e.tensor_tensor(yrv[:, :, 64:128], t1[:], t2[:], op=ALU.add), R=[t1, t2], P=[yr])
                    src = yr
                else:
                    src = Y
                self.transpose8(src, qT, ptr, nblk=10)
                kb.dma("sp", d["QT"], d["QT"][0:10, :, t * 128:(t + 1) * 128].rearrange("h p t -> p h t"), qT, qT[:])
                kb.dma("sp", d["V"], d["V"][t * 128:(t + 1) * 128, :], Y, Y[:, 1280:1536])

            self.phase_linear(st, d["H"], i["a_w_qkv"], i["a_w_qkv"][j], 1536, list(range(self.NT)), post)

    def phase_attn_A(self, last):
        kb, d, T, S, C = self.kb, self.d, self.T, self.S, self.C
        scale = 128 ** -0.5
        with contextlib.ExitStack() as st:
            kb.barrier()
            kT = kb.sb(st, "kT", [128, T])
            vx = kb.sb(st, "vx", [128, self.NT, 129])
            qb = [kb.sb(st, f"qb{k}", [128, 512]) for k in range(2)]
            pT = [kb.sb(st, f"pT{k}", [128, 512]) for k in range(3)]
            ob = [kb.sb(st, f"ob{k}", [128, 128]) for k in range(2)]
            rc = kb.sb(st, "rc", [128, 1])
            psS = [kb.ps(st, f"psS{k}", [128, 512]) for k in range(2)]
            psO = [kb.ps(st, f"psO{k}", [128, 512]) for k in range(4)]
            nq = 0
            nch = 0
            for g in range(2):
                kb.dma("sp", kT, kT[:], d["QT"], d["QT"][8 + g], part=False)
                kb.op("pool", lambda e: e.memset(vx[:, :, 128:129], 1.0), W=[vx])
                kb.dma("sp", vx, vx[:, :, 0:128], d["V"], d["V"][:, g * 128:(g + 1) * 128].rearrange("(c p) e -> p c e", p=128))
                blocks = [(q0, min(512, S - q0), list(range(self.NT))) for q0 in range(0, S, 512)]
                if not last:
                    blocks += [(q0, min(512, T - q0), list(range(self.NTL, self.NT))) for q0 in range(S, T, 512)]
                items = [(h, q0, nqq, chunks) for h in range(4 * g, 4 * g + 4) for (q0, nqq, chunks) in blocks]
                kb.dma("sp", qb[nq % 2], qb[nq % 2][:, 0:items[0][2]], d["QT"], d["QT"][items[0][0], :, items[0][1]:items[0][1] + items[0][2]], part=False)
                for ii, (h, q0, nqq, chunks) in enumerate(items):
                    if True:
                        Q = qb[nq % 2]
                        nq += 1
                        if ii + 1 < len(items):
                            hn_, q0n, nqn, _ = items[ii + 1]
                            Qn = qb[nq % 2]
                            kb.dma("sp", Qn, Qn[:, 0:nqn], d["QT"], d["QT"][hn_, :, q0n:q0n + nqn], part=False)
                        nsub = nqq // 128
                        def emit_s(ci):
                            pS = psS[(nch + ci) % 2]
                            ch = chunks[ci]
                            kb.op("pe", lambda e: e.matmul(pS[:, 0:nqq], kT[:, ch * 128:(ch + 1) * 128], Q[:, 0:nqq], start=True, stop=True),
                                  R=[kT, Q], P=[pS])
                        emit_s(0)
                        for ci, ch in enumerate(chunks):
                            pS = psS[(nch + ci) % 2]
                            P_ = pT[(nch + ci) % 3]
                            if ci + 1 < len(chunks):
                                emit_s(ci + 1)
                            kb.op("act", lambda e: e.activation(P_[:, 0:nqq], pS[:, 0:nqq], AF.Exp, scale=scale), R=[pS], W=[P_])
                            for s in range(nsub):
                                kb.op("pe", lambda e: e.matmul(psO[s][:, 0:129], P_[:, s * 128:(s + 1) * 128], vx[:, ch, :],
                                                               start=(ci == 0), stop=(ci == len(chunks) - 1)), R=[P_, vx], P=[psO[s]])
                        nch += len(chunks)
                        for s in range(nsub):
                            O = ob[s % 2]
                            kb.op("dve", lambda e: e.reciprocal(rc[:], psO[s][:, 128:129]), R=[psO[s]], W=[rc])
                            kb.op("dve", lambda e: e.tensor_scalar(O[:], psO[s][:, 0:128], rc[:, 0:1], None, op0=ALU.mult), R=[psO[s], rc], W=[O])
                            r0 = q0 + s * 128
                            kb.dma("sp", d["O"], d["O"][r0:r0 + 128, h * 128:(h + 1) * 128], O, O[:])

    def phase_out_proj(self, l, w_b, w_ap, last, src=None):
        kb, d = self.kb, self.d
        src = src if src is not None else d["O"]
        with contextlib.ExitStack() as st:
            kb.barrier()
            gt = {}
            for row in ((0,) if last else (0, 1)):
                g = kb.sb(st, f"og{row}", [128, D])
                self.load_bc(g, d["MODV"], self.mod_row(l, row, 2))
                gt[row] = g
            xt = [kb.sb(st, f"ox{k}", [128, D]) for k in range(2)]

            def post(n, t, Y):
                Xt = xt[n % 2]
                g = gt[0 if t < self.NTL else 1]
                kb.dma("sp", Xt, Xt[:], d["X"], d["X"][t * 128:(t + 1) * 128, :], part=False)
                kb.op("pool", lambda e: e.tensor_tensor(Y[:], Y[:], g[:], op=ALU.mult), R=[Y, g], W=[Y])
                kb.op("dve", lambda e: e.tensor_tensor(Xt[:], Xt[:], Y[:], op=ALU.add), R=[Xt, Y], W=[Xt])
                kb.dma("sp", d["X"], d["X"][t * 128:(t + 1) * 128, :], Xt, Xt[:])

            tiles = list(range(self.NTL if last else self.NT))
            self.phase_linear(st, src, w_b, w_ap, D, tiles, post)


    def phase_qkv_B(self, l, j):
        kb, i, d = self.kb, self.i, self.d
        with contextlib.ExitStack() as st:
            kb.barrier()
            cs = [kb.sb(st, f"bcs{k}", [128, 64]) for k in range(2)]
            t1 = kb.sb(st, "bt1", [128, 20, 32])
            t2 = kb.sb(st, "bt2", [128, 20, 32])
            yr = kb.sb(st, "byr", [128, 1280])
            qT = kb.sb(st, "bqT", [128, 10, 128])
            ptr = [kb.ps(st, f"bpt{k}", [128, 512]) for k in range(3)]

            def post(n, t, Y):
                lat = t < self.NTL
                if lat:
                    yv = Y[:, 0:1280].rearrange("p (h e) -> p h e", h=20)
                    c_ = cs[n % 2]
                    kb.dma("sp", c_, c_[:], i["k_ropeB"], i["k_ropeB"][t * 128:(t + 1) * 128, :], part=False)
                    x1, x2 = yv[:, :, 0:32], yv[:, :, 32:64]
                    yrv = yr[:].rearrange("p (h e) -> p h e", h=20)
                    cosb, sinb = bc_mid(c_[:, 0:32], 20), bc_mid(c_[:, 32:64], 20)
                    kb.op("dve", lambda e: e.tensor_tensor(t1[:], x1, cosb, op=ALU.mult), R=[Y, c_], W=[t1])
                    kb.op("pool", lambda e: e.tensor_tensor(t2[:], x2, sinb, op=ALU.mult), R=[Y, c_], W=[t2])
                    kb.op("dve", lambda e: e.tensor_tensor(yrv[:, :, 0:32], t1[:], t2[:], op=ALU.subtract), R=[t1, t2], W=[yr])
                    kb.op("pool", lambda e: e.tensor_tensor(t1[:], x1, sinb, op=ALU.mult), R=[Y, c_], W=[t1])
                    kb.op("dve", lambda e: e.tensor_tensor(t2[:], x2, cosb, op=ALU.mult), R=[Y, c_], W=[t2])
                    kb.op("pool", lambda e: e.tensor_tensor(yrv[:, :, 32:64], t1[:], t2[:], op=ALU.add), R=[t1, t2], P=[yr])
                    src = yr
                else:
                    src = Y
                self.transpose8(src, qT, ptr, nblk=10)
                for half in range(2):
                    dst = d["QT"][:, 0:64, t * 128:(t + 1) * 128].rearrange("(b two) p t -> two p b t", two=2)[half]
                    kb.dma("sp", d["QT"], dst, qT, qT[half * 64:(half + 1) * 64, :, :])
                kb.dma("sp", d["V"], d["V"][t * 128:(t + 1) * 128, :], Y, Y[:, 1280:1536])

            self.phase_linear(st, d["H"], i["b_w_qkv"], i["b_w_qkv"][j], 1536, list(range(self.NT)), post)

    def phase_attn_B(self, j, last):
        kb, i, d, T, S, C = self.kb, self.i, self.d, self.T, self.S, self.C
        scale = 64 ** -0.5
        NT, NTL = self.NT, self.NTL
        with contextlib.ExitStack() as st:
            kb.barrier()
            kT = kb.sb(st, "bkT", [64, 4, T])
            vx = kb.sb(st, "bvx", [128, NT, 4, 65])
            es = kb.sb(st, "bes", [128, 16])
            tri = kb.sb(st, "btri", [128, 2, 128])
            kb.dma("sp", kT, kT[:], d["QT"], d["QT"][16:20, 0:64, :].rearrange("g p t -> p g t"), part=False)
            kb.op("pool", lambda e: e.memset(vx[:, :, :, 64:65], 1.0), W=[vx])
            for g in range(4):
                kb.dma("sp", vx, vx[:, :, g, 0:64], d["V"], d["V"][:, g * 64:(g + 1) * 64].rearrange("(c p) e -> p c e", p=128))
            self.load_bc(es, i["b_sink"], i["b_sink"][j, :])
            kb.op("act", lambda e: e.activation(es[:], es[:], AF.Exp), R=[es], W=[es])
            kb.dma("sp", tri, tri[:], i["k_tri"], i["k_tri"][:].rearrange("a p q -> p a q"), part=False)
            qa = [kb.sb(st, f"bqa{k}", [64, 16, 128]) for k in range(2)]
            pT = [kb.sb(st, f"bpT{k}", [128, 512]) for k in range(3)]
            ot = [kb.sb(st, f"bot{k}", [128, D]) for k in range(2)]
            den = kb.sb(st, "bden", [128, 1])
            psS = [kb.ps(st, f"bpsS{k}", [128, 512]) for k in range(2)]
            psO = [kb.ps(st, f"bpsO{k}", [128, 512]) for k in range(4)]
            nch = 0
            tiles = list(range(NTL if last else NT))
            for n, t in enumerate(tiles):
                if t < NTL:
                    chunks = []
                    if t - 1 >= 0:
                        chunks.append((t - 1, 0))
                    chunks.append((t, None))
                    if t + 1 < NTL:
                        chunks.append((t + 1, 1))
                    chunks += [(c_, None) for c_ in range(NTL, NT)]
                else:
                    chunks = [(c_, None) for c_ in range(NTL, NT)]
                Q = qa[n % 2]
                O = ot[n % 2]
                kb.dma("sp", Q, Q[:], d["QT"], d["QT"][0:16, 0:64, t * 128:(t + 1) * 128].rearrange("h p t -> p h t"), part=False)
                for g in range(4):
                    def emit_s(ci):
                        pS = psS[(nch + ci) % 2]
                        ch = chunks[ci][0]
                        kb.op("pe", lambda e: e.matmul(pS[:], kT[:, g, ch * 128:(ch + 1) * 128], Q[:, 4 * g:4 * g + 4, :].rearrange("p h t -> p (h t)"),
                                                       start=True, stop=True), R=[kT, Q], P=[pS])
                    emit_s(0)
                    for ci, (ch, mk) in enumerate(chunks):
                        pS = psS[(nch + ci) % 2]
                        P_ = pT[(nch + ci) % 3]
                        if ci + 1 < len(chunks):
                            emit_s(ci + 1)
                        kb.op("act", lambda e: e.activation(P_[:], pS[:], AF.Exp, scale=scale), R=[pS], W=[P_])
                        if mk is not None:
                            pv = P_[:].rearrange("p (h t) -> p h t", h=4)
                            kb.op("pool", lambda e: e.tensor_tensor(pv, pv, bc_mid(tri[:, mk, :], 4), op=ALU.mult), R=[P_, tri], W=[P_])
                        for s in range(4):
                            kb.op("pe", lambda e: e.matmul(psO[s][:, 0:65], P_[:, s * 128:(s + 1) * 128], vx[:, ch, g, :],
                                                           start=(ci == 0), stop=(ci == len(chunks) - 1)), R=[P_, vx], P=[psO[s]])
                    nch += len(chunks)
                    for s in range(4):
                        h = 4 * g + s
                        kb.op("dve", lambda e: e.tensor_tensor(den[:], psO[s][:, 64:65], es[:, h:h + 1], op=ALU.add), R=[psO[s], es], W=[den])
                        kb.op("dve", lambda e: e.reciprocal(den[:], den[:]), R=[den], W=[den])
                        kb.op("dve", lambda e: e.tensor_scalar(O[:, h * 64:(h + 1) * 64], psO[s][:, 0:64], den[:, 0:1], None, op0=ALU.mult),
                              R=[psO[s], den], P=[O])
                kb.dma("sp", d["O"], d["O"][t * 128:(t + 1) * 128, :], O, O[:])


    def phase_rwkv(self, l, j, last, dbg=99):
        kb, i, d, T, S, C = self.kb, self.i, self.d, self.T, self.S, self.C
        NT, NTL = self.NT, self.NTL
        alltiles = list(range(NT))
        with contextlib.ExitStack() as st:
            kb.barrier()
            mu = [kb.sb(st, f"mu{k}", [128, D]) for k in range(6)]
            for k in range(6):
                self.load_bc(mu[k], i["c_mu"], i["c_mu"][j, k, :])
            hh = [kb.sb(st, f"rh{k}", [128, D]) for k in range(2)]
            hp = [kb.sb(st, f"rhp{k}", [128, D]) for k in range(2)]
            hn = [kb.sb(st, f"rhn{k}", [128, D]) for k in range(2)]
            xx = kb.sb(st, "rxx", [128, D])
            xj = [kb.sb(st, f"rxj{k}", [128, D]) for k in range(3)]
            nx = 0
            for n, t in enumerate(alltiles):
                t0 = t * 128
                Hc, Hp, Hn = hh[n % 2], hp[n % 2], hn[n % 2]
                kb.dma("sp", Hc, Hc[:], d["H"], d["H"][t0:t0 + 128, :], part=False)
                if t == 0 or t == NTL:
                    kb.dma("sp", Hp, Hp[0:1, :], i["k_zero"], i["k_zero"][0:1, :], part=False)
                    kb.dma("sp", Hp, Hp[1:128, :], d["H"], d["H"][t0:t0 + 127, :])
                else:
                    kb.dma("sp", Hp, Hp[:], d["H"], d["H"][t0 - 1:t0 + 127, :], part=False)
                if t == NTL - 1 or t == NT - 1:
                    kb.dma("sp", Hn, Hn[127:128, :], i["k_zero"], i["k_zero"][0:1, :], part=False)
                    kb.dma("sp", Hn, Hn[0:127, :], d["H"], d["H"][t0 + 1:t0 + 128, :])
                else:
                    kb.dma("sp", Hn, Hn[:], d["H"], d["H"][t0 + 1:t0 + 129, :], part=False)
                kb.op("pool", lambda e: e.tensor_tensor(Hp[:], Hp[:], Hn[:], op=ALU.add), R=[Hp, Hn], W=[Hp])
                kb.op("dve", lambda e: e.scalar_tensor_tensor(xx[:], Hp[:], 0.5, Hc[:], op0=ALU.mult, op1=ALU.subtract), R=[Hp, Hc], W=[xx])
                for k in range(6):
                    X = xj[nx % 3]
                    nx += 1
                    e1, e2 = ("pool", "dve") if k % 2 == 0 else ("dve", "pool")
                    kb.op(e1, lambda e: e.tensor_tensor(X[:], xx[:], mu[k][:], op=ALU.mult), R=[xx, mu[k]], W=[X])
                    kb.op(e2, lambda e: e.tensor_tensor(X[:], X[:], Hc[:], op=ALU.add), R=[X, Hc], W=[X])
                    kb.dma("sp", d["XJ"], d["XJ"][k, t0:t0 + 128, :], X, X[:])
        for (src_k, wi, dst) in ((0, 0, "RR"), (2, 1, "KK"), (3, 2, "VV")):
            with contextlib.ExitStack() as st:
                kb.barrier()

                def post(n, t, Y, dst=dst):
                    kb.dma("sp", d[dst], d[dst][t * 128:(t + 1) * 128, :], Y, Y[:])

                self.phase_linear(st, d["XJ"], i["c_w_rkv"], i["c_w_rkv"][j, wi], D, alltiles, post, src_ap=d["XJ"][src_k])
        def lora(src_k, fill_w1, act, fill_w2, nsplit, epi):
            with contextlib.ExitStack() as st:
                kb.barrier()
                wt = kb.sb(st, "lrw1", [128, 8, 128])
                fill_w1(wt)
                w2t = kb.sb(st, "lrw2", [128, D])
                fill_w2(w2t)
                yT = kb.sb(st, "lryT", [128, 128])
                ot = [kb.sb(st, f"lro{k}", [128, D]) for k in range(2)]
                tmp = kb.sb(st, "lrtmp", [128, 512])
                pt = kb.ps(st, "lrpt", [128, 512])
                po = [kb.ps(st, f"lrpo{k}", [128, 512]) for k in range(2)]
                cnt = [0]

                def post(n, t, Y):
                    if act is not None:
                        kb.op("act", lambda e: e.activation(Y[:], Y[:], act), R=[Y], W=[Y])
                    kb.op("pe", lambda e: e.transpose(pt[:, 0:128], Y[:, 0:128], self.ident[:]), R=[Y, self.ident], W=[pt])
                    kb.op("dve", lambda e: e.tensor_copy(yT[:], pt[:, 0:128]), R=[pt], W=[yT])
                    kk = 128 // nsplit
                    for dd in range(nsplit):
                        O = ot[cnt[0] % 2]
                        cnt[0] += 1
                        for nb in range(2):
                            kb.op("pe", lambda e: e.matmul(po[nb][:], yT[dd * kk:(dd + 1) * kk, :], w2t[dd * kk:(dd + 1) * kk, nb * 512:(nb + 1) * 512],
                                                           start=True, stop=True), R=[yT, w2t], P=[po[nb]])
                            epi(dd, nb, po[nb], O, tmp)
                        self_dst = epi.dst(dd)
                        kb.dma("sp", self_dst[0], self_dst[1][t * 128:(t + 1) * 128, :], O, O[:])

                self.phase_linear(st, d["XJ"], None, None, 128, alltiles, post, wt=wt, src_ap=d["XJ"][src_k])

        def fill2(arr):
            def f(wt):
                for dd in range(2):
                    kb.dma("sp", wt, wt[:, :, dd * 64:(dd + 1) * 64], i[arr], i[arr][j, dd].rearrange("(k p) n -> p k n", p=128))
            return f

        def fill2b(arr):
            def f(w2t):
                for dd in range(2):
                    kb.dma("sp", w2t, w2t[dd * 64:(dd + 1) * 64, :], i[arr], i[arr][j, dd])
            return f

        with contextlib.ExitStack() as stb:
            kb.barrier()
            bw = [kb.sb(stb, f"bw{k}", [128, D]) for k in range(2)]
            ba = [kb.sb(stb, f"ba{k}", [128, D]) for k in range(2)]
            for dd in range(2):
                self.load_bc(bw[dd], i["c_w0"], i["c_w0"][j, dd, :])
                self.load_bc(ba[dd], i["c_a0"], i["c_a0"][j, dd, :])

            def epi_w(dd, nb, ps, O, tmp):
                sl = slice(nb * 512, (nb + 1) * 512)
                kb.op("dve", lambda e: e.tensor_tensor(tmp[:], ps[:], bw[dd][:, sl], op=ALU.add), R=[ps, bw[dd]], W=[tmp])
                kb.op("act", lambda e: e.activation(tmp[:], tmp[:], AF.Sigmoid), R=[tmp], W=[tmp])
                kb.op("act", lambda e: e.activation(O[:, sl], tmp[:], AF.Exp, scale=-float(np.exp(-0.5))), R=[tmp], P=[O])
            epi_w.dst = lambda dd: (d["DEC"], d["DEC"][dd])

            def epi_a(dd, nb, ps, O, tmp):
                sl = slice(nb * 512, (nb + 1) * 512)
                kb.op("dve", lambda e: e.tensor_tensor(tmp[:], ps[:], ba[dd][:, sl], op=ALU.add), R=[ps, ba[dd]], W=[tmp])
                kb.op("act", lambda e: e.activation(O[:, sl], tmp[:], AF.Sigmoid), R=[tmp], P=[O])
            epi_a.dst = lambda dd: (d["AA"], d["AA"][dd])

            def epi_g(dd, nb, ps, O, tmp):
                sl = slice(nb * 512, (nb + 1) * 512)
                kb.op("act", lambda e: e.activation(O[:, sl], ps[:], AF.Copy), R=[ps], P=[O])
            epi_g.dst = lambda dd: (d["GG"], d["GG"])

            lora(1, fill2("c_w1"), AF.Tanh, fill2b("c_w2"), 2, epi_w)
            lora(4, fill2("c_a1"), None, fill2b("c_a2"), 2, epi_a)
            lora(5, lambda wt: kb.dma("sp", wt, wt[:], i["c_g1"], i["c_g1"][j].rearrange("(k p) n -> p k n", p=128), part=False),
                 AF.Sigmoid, lambda w2t: kb.dma("sp", w2t, w2t[:], i["c_g2"], i["c_g2"][j], part=False), 1, epi_g)
        if dbg == 2:
            return
        with contextlib.ExitStack() as st:
            kb.barrier()
            kkb = kb.sb(st, "kkb", [128, D])
            kab = kb.sb(st, "kab", [128, D])
            rkb = [kb.sb(st, f"rkb{k}", [128, D]) for k in range(2)]
            self.load_bc(kkb, i["c_k_k"], i["c_k_k"][j, :])
            self.load_bc(kab, i["c_k_a"], i["c_k_a"][j, :])
            for dd in range(2):
                self.load_bc(rkb[dd], i["c_r_k"], i["c_r_k"][j, dd, :])
            tk = kb.sb(st, "tk", [128, D])
            tr = kb.sb(st, "tr", [128, D])
            ta = [kb.sb(st, f"ta{k}", [128, D]) for k in range(2)]
            tw = [kb.sb(st, f"tw{k}", [128, D]) for k in range(2)]
            kk = kb.sb(st, "kk", [128, D])
            t1 = kb.sb(st, "t1", [128, D])
            t2 = kb.sb(st, "t2", [128, D])
            t3 = kb.sb(st, "t3", [128, D])
            t4 = kb.sb(st, "t4", [128, D])
            ss = kb.sb(st, "ss", [128, 16])
            rs = kb.sb(st, "rs", [128, 16])
            sc = kb.sb(st, "sc", [128, 96])
            hv = lambda b: b[:].rearrange("p (h e) -> p h e", h=16)
            for n, t in enumerate(alltiles):
                rows = slice(t * 128, (t + 1) * 128)
                kb.dma("sp", tk, tk[:], d["KK"], d["KK"][rows, :], part=False)
                kb.dma("sp", tr, tr[:], d["RR"], d["RR"][rows, :], part=False)
                for dd in range(2):
                    kb.dma("sp", ta[dd], ta[dd][:], d["AA"], d["AA"][dd, rows, :], part=False)
                    kb.dma("sp", tw[dd], tw[dd][:], d["DEC"], d["DEC"][dd, rows, :], part=False)
                kb.op("pool", lambda e: e.tensor_tensor(kk[:], tk[:], kkb[:], op=ALU.mult), R=[tk, kkb], W=[kk])
                kb.op("dve", lambda e: e.tensor_tensor(t1[:], kk[:], kk[:], op=ALU.mult), R=[kk], W=[t1])
                kb.op("dve", lambda e: e.tensor_reduce(ss[:], hv(t1), axis=AX.X, op=ALU.add), R=[t1], W=[ss])
                kb.op("dve", lambda e: e.tensor_scalar(rs[:], ss[:], 1e-24, None, op0=ALU.max), R=[ss], W=[rs])
                kb.op("act", lambda e: e.activation(rs[:], rs[:], AF.Sqrt), R=[rs], W=[rs])
                kb.op("dve", lambda e: e.reciprocal(rs[:], rs[:]), R=[rs], W=[rs])
                kb.op("dve", lambda e: e.tensor_tensor(hv(kk), hv(kk), bc_last(rs[:, :], 64), op=ALU.mult), R=[kk, rs], W=[kk])
                kb.op("act", lambda e: e.activation(t1[:], kk[:], AF.Copy, scale=-1.0), R=[kk], W=[t1])
                kb.dma("sp", d["AV"], d["AV"][rows, :], t1, t1[:])
                for dd in range(2):
                    kb.op("dve", lambda e: e.scalar_tensor_tensor(t2[:], ta[dd][:], -1.0, kab[:], op0=ALU.add, op1=ALU.mult), R=[ta[dd], kab], W=[t2])
                    kb.op("dve", lambda e: e.scalar_tensor_tensor(t2[:], t2[:], 1.0, tk[:], op0=ALU.add, op1=ALU.mult), R=[t2, tk], W=[t2])
                    kb.dma("sp", d["KD"], d["KD"][dd, rows, :], t2, t2[:])
                    kb.op("pool", lambda e: e.tensor_tensor(t3[:], kk[:], ta[dd][:], op=ALU.mult), R=[kk, ta[dd]], W=[t3])
                    kb.dma("sp", d["BB"], d["BB"][dd, rows, :], t3, t3[:])
                    kb.op("pool", lambda e: e.tensor_tensor(t4[:], tr[:], tw[dd][:], op=ALU.mult), R=[tr, tw[dd]], W=[t4])
                    kb.dma("sp", d["RP"], d["RP"][dd, rows, :], t4, t4[:])
                    kb.op("pool", lambda e: e.tensor_tensor(t3[:], t3[:], tr[:], op=ALU.mult), R=[t3, tr], W=[t3])
                    kb.op("dve", lambda e: e.tensor_reduce(sc[:, dd * 16:(dd + 1) * 16], hv(t3), axis=AX.X, op=ALU.add), R=[t3], P=[sc])
                    kb.op("pool", lambda e: e.tensor_tensor(t2[:], t2[:], tr[:], op=ALU.mult), R=[t2, tr], W=[t2])
                    kb.op("dve", lambda e: e.tensor_reduce(sc[:, 32 + dd * 16:32 + (dd + 1) * 16], hv(t2), axis=AX.X, op=ALU.add), R=[t2], P=[sc])
                    kb.op("pool", lambda e: e.tensor_tensor(t2[:], t2[:], rkb[dd][:], op=ALU.mult), R=[t2, rkb[dd]], W=[t2])
                    kb.op("dve", lambda e: e.tensor_reduce(sc[:, 64 + dd * 16:64 + (dd + 1) * 16], hv(t2), axis=AX.X, op=ALU.add), R=[t2], P=[sc])
                kb.op("dve", lambda e: e.tensor_tensor(sc[:, 64:80], sc[:, 64:80], sc[:, 80:96], op=ALU.add), R=[sc], W=[sc])
                kb.dma("sp", d["SC"], d["SC"][rows, :], sc, sc[:, 0:80])
        if dbg == 3:
            return
        with contextlib.ExitStack() as st:
            kb.barrier()
            bd = kb.sb(st, "bd", [48, 512])
            kb.dma("sp", bd, bd[:], i["k_bd"], i["k_bd"][:], part=False)
            selc = kb.sb(st, "selc", [128, 64, 16])
            kb.dma("sp", selc, selc[:], i["k_sel"], i["k_sel"][:], part=False)

            def scan_dir(dd):
                svt = [kb.sb(st, f"svt{dd}{k}", [128, 512]) for k in range(2)]
                vr = kb.sb(st, f"VR{dd}", [128, 512])
                lt = kb.sb(st, f"LT{dd}", [128, 64, 32])
                wt_ = kb.sb(st, f"WT{dd}", [128, 64, 8])
                l2 = kb.sb(st, f"L2{dd}", [48, 64, 128])
                Mr = [kb.sb(st, f"Mr{dd}{k}", [48, 512]) for k in range(2)]
                stg = [kb.sb(st, f"stg{dd}{a}", [64, D]) for a in range(3)]
                po1 = kb.ps(st, f"spo1{dd}", [128, 512])
                pu = kb.ps(st, f"spu{dd}", [128, 512])
                pt0 = kb.ps(st, f"spt{dd}", [128, 512])
                pts = [pt0, pu, po1]
                kb.op("pool", lambda e: e.memset(lt[:], 0.0), W=[lt])
                kb.op("pool", lambda e: e.memset(l2[:], 0.0), W=[l2])
                kb.op("pool", lambda e: e.memset(svt[0][:], 0.0), W=[svt[0]])
                yield
                p = 0
                nstep = 0
                nchunk = 0
                for (lo, hi) in [(S, T), (0, S)]:
                    c0s = list(range(lo, hi, 64))
                    if dd == 1:
                        c0s = c0s[::-1]
                    for c0 in c0s:
                        if dbg == 41 and nchunk >= 1:
                            break
                        nchunk += 1
                        srcs = [(d["AV"], d["AV"][c0:c0 + 64, :]), (d["RP"], d["RP"][dd, c0:c0 + 64, :]), (d["DEC"], d["DEC"][dd, c0:c0 + 64, :])]
                        for a, (sb_, sap) in enumerate(srcs):
                            kb.dma("sp", stg[a], stg[a][:], sb_, sap, part=False)
                        for (r0, arr) in ((0, "BB"), (32, "KD")):
                            for hg in range(2):
                                kb.dma("sp", l2, l2[r0 + hg * 8:r0 + hg * 8 + 8, :, hg * 64:(hg + 1) * 64], d[arr],
                                       d[arr][dd, c0:c0 + 64, :].rearrange("t (hp hg k) -> hg hp t k", hg=2, k=64)[hg])
                        for hg in range(2):
                            kb.dma("sp", vr, vr[hg * 64:(hg + 1) * 64, :].rearrange("t (hp v) -> t hp v", v=64), d["VV"],
                                   d["VV"][c0:c0 + 64, :].rearrange("t (hp hg v) -> hg t hp v", hg=2, v=64)[hg])
                        for a in range(3):
                            G = stg[a]
                            pt = pts[a]
                            for hp in range(8):
                                kb.op("pe", lambda e: e.transpose(pt[:, hp * 64:(hp + 1) * 64], G[:, hp * 128:(hp + 1) * 128], self.ident[0:64, 0:64]),
                                      R=[G, self.ident], P=[pt])
                            if a < 2:
                                kb.op("act", lambda e: e.activation(lt[0:64, :, a * 16:a * 16 + 8], pt[0:64, :].rearrange("p (h t) -> p t h", h=8), AF.Copy),
                                      R=[pt], P=[lt])
                                kb.op("dve", lambda e: e.tensor_copy(lt[64:128, :, a * 16 + 8:a * 16 + 16], pt[64:128, :].rearrange("p (h t) -> p t h", h=8)),
                                      R=[pt], P=[lt])
                            else:
                                kb.op("act", lambda e: e.activation(wt_[:], pt[:].rearrange("p (h t) -> p t h", h=8), AF.Copy), R=[pt], W=[wt_])
                        yield
                        order = range(64) if dd == 0 else range(63, -1, -1)
                        for tl in order:
                            cur, nxt = p, 1 - p
                            M = Mr[nstep % 2]
                            nstep += 1
                            kb.op("pe", lambda e: e.matmul(po1[0:32, :], lt[:, tl, :], svt[cur][:], start=True, stop=True), R=[lt, svt[cur]], W=[po1])
                            kb.op("pe", lambda e: e.matmul(po1[32:48, :], selc[:, tl, :], vr[:], start=True, stop=True), R=[selc, vr], P=[po1])
                            kb.op("pool", lambda e: e.tensor_tensor(svt[nxt][:].rearrange("p (h e) -> p h e", h=8),
                                                                     svt[cur][:].rearrange("p (h e) -> p h e", h=8),
                                                                     bc_last(wt_[:, tl, :], 64), op=ALU.mult), R=[svt[cur], wt_], W=[svt[nxt]])
                            yield
                            kb.op("dve", lambda e: e.tensor_tensor(M[:], po1[0:48, :], bd[:], op=ALU.mult), R=[po1, bd], W=[M])
                            yield
                            kb.op("pe", lambda e: e.matmul(pu[:], l2[:, tl, :], M[:], start=True, stop=True), R=[l2, M], W=[pu])
                            yield
                            kb.op("dve", lambda e: e.tensor_tensor(svt[nxt][:], svt[nxt][:], pu[:], op=ALU.add), R=[svt[nxt], pu], W=[svt[nxt]])
                            tok = c0 + tl
                            qsb = d["QS"][dd][tok // 2048]
                            kb.dma("sp", qsb, qsb[tok % 2048, :].rearrange("(p n) -> p n", p=32), M, M[0:32, :])
                            p = nxt
                            yield

            gens = [scan_dir(0), scan_dir(1)]
            while gens:
                for g in list(gens):
                    try:
                        next(g)
                    except StopIteration:
                        gens.remove(g)
        if dbg in (4, 41):
            return
        tiles = list(range(NTL if last else NT))
        with contextlib.ExitStack() as st:
            kb.barrier()
            lw = kb.sb(st, "lnw", [128, D])
            lb = kb.sb(st, "lnb", [128, D])
            self.load_bc(lw, i["c_ln_w"], i["c_ln_w"][j, :])
            self.load_bc(lb, i["c_ln_b"], i["c_ln_b"][j, :])
            sa = [kb.sb(st, f"osa{k}", [128, D]) for k in range(2)]
            qq = [kb.sb(st, f"oqq{k}", [128, D]) for k in range(2)]
            tv = kb.sb(st, "otv", [128, D])
            tg = kb.sb(st, "otg", [128, D])
            sc = kb.sb(st, "osc", [128, 80])
            o = kb.sb(st, "oo", [128, D])
            t1 = kb.sb(st, "ot1", [128, D])
            m1 = kb.sb(st, "om1", [128, 16])
            m2 = kb.sb(st, "om2", [128, 16])
            hv = lambda b: b[:].rearrange("p (h e) -> p h e", h=16)
            for n, t in enumerate(tiles):
                rows = slice(t * 128, (t + 1) * 128)
                for dd in range(2):
                    qsb = d["QS"][dd][(t * 128) // 2048]
                    r0 = (t * 128) % 2048
                    qv = qsb[r0:r0 + 128, :].rearrange("t (ty g q v) -> t ty g q v", ty=2, g=2, q=64, v=64)
                    for ty, dstb in ((0, sa[dd]), (1, qq[dd])):
                        dv = dstb[:].rearrange("p (hp hg v) -> p hg hp v", hg=2, v=64)
                        for hg in range(2):
                            kb.dma("sp", dstb, dv[:, hg], qsb, qv[:, ty, hg, ::9, :])
                kb.dma("sp", tv, tv[:], d["VV"], d["VV"][rows, :], part=False)
                kb.dma("sp", tg, tg[:], d["GG"], d["GG"][rows, :], part=False)
                kb.dma("sp", sc, sc[:], d["SC"], d["SC"][rows, :], part=False)
                kb.op("pool", lambda e: e.tensor_tensor(o[:], qq[0][:], qq[1][:], op=ALU.add), R=[qq[0], qq[1]], W=[o])
                for dd in range(2):
                    kb.op("dve", lambda e: e.tensor_tensor(hv(t1), hv(sa[dd]), bc_last(sc[:, dd * 16:(dd + 1) * 16], 64), op=ALU.mult), R=[sa[dd], sc], W=[t1])
                    kb.op("pool", lambda e: e.tensor_tensor(o[:], o[:], t1[:], op=ALU.add), R=[o, t1], W=[o])
                kb.op("dve", lambda e: e.tensor_tensor(m1[:], sc[:, 32:48], sc[:, 48:64], op=ALU.add), R=[sc], W=[m1])
                kb.op("dve", lambda e: e.tensor_tensor(hv(t1), hv(tv), bc_last(m1[:, :], 64), op=ALU.mult), R=[tv, m1], W=[t1])
                kb.op("pool", lambda e: e.tensor_tensor(o[:], o[:], t1[:], op=ALU.add), R=[o, t1], W=[o])
                kb.op("dve", lambda e: e.tensor_reduce(m1[:], hv(o), axis=AX.X, op=ALU.add), R=[o], W=[m1])
                kb.op("dve", lambda e: e.tensor_scalar(m1[:], m1[:], -1.0 / 64, None, op0=ALU.mult), R=[m1], W=[m1])
                kb.op("dve", lambda e: e.tensor_tensor(hv(o), hv(o), bc_last(m1[:, :], 64), op=ALU.add), R=[o, m1], W=[o])
                kb.op("pool", lambda e: e.tensor_tensor(t1[:], o[:], o[:], op=ALU.mult), R=[o], W=[t1])
                kb.op("dve", lambda e: e.tensor_reduce(m2[:], hv(t1), axis=AX.X, op=ALU.add), R=[t1], W=[m2])
                kb.op("dve", lambda e: e.tensor_scalar(m2[:], m2[:], 1.0 / 64, 64 * 1e-5, op0=ALU.mult, op1=ALU.add), R=[m2], W=[m2])
                kb.op("act", lambda e: e.activation(m2[:], m2[:], AF.Sqrt), R=[m2], W=[m2])
                kb.op("dve", lambda e: e.reciprocal(m2[:], m2[:]), R=[m2], W=[m2])
                kb.op("dve", lambda e: e.tensor_tensor(hv(o), hv(o), bc_last(m2[:, :], 64), op=ALU.mult), R=[o, m2], W=[o])
                kb.op("pool", lambda e: e.tensor_tensor(o[:], o[:], lw[:], op=ALU.mult), R=[o, lw], W=[o])
                kb.op("dve", lambda e: e.tensor_tensor(o[:], o[:], lb[:], op=ALU.add), R=[o, lb], W=[o])
                kb.op("dve", lambda e: e.tensor_tensor(hv(t1), hv(tv), bc_last(sc[:, 64:80], 64), op=ALU.mult), R=[tv, sc], W=[t1])
                kb.op("pool", lambda e: e.tensor_tensor(o[:], o[:], t1[:], op=ALU.add), R=[o, t1], W=[o])
                kb.op("dve", lambda e: e.tensor_tensor(o[:], o[:], tg[:], op=ALU.mult), R=[o, tg], W=[o])
                kb.dma("sp", d["O"], d["O"][rows, :], o, o[:])
        self.phase_out_proj(l, i["c_w_o"], i["c_w_o"][j], last)

    def phase_moe(self, l, last):
        kb, i, d, S, C, T, FF = self.kb, self.i, self.d, self.S, self.C, self.T, self.FF
        capl, capc = 2 * S // NE, 2 * C // NE
        tiles = list(range(self.NTL if last else self.NT))
        self.phase_norm(l, 3, 4, tiles, d["H"])
        chunks = [(0, s0, min(128, capl - s0)) for s0 in range(0, capl, 128)]
        if not last:
            chunks += [(1, s0, min(128, capc - s0)) for s0 in range(0, capc, 128)]
        NCH = len(chunks)
        NSL = NCH * 128
        offs = [sum(c[2] for c in chunks[:k]) for k in range(NCH)]
        NSC = sum(c[2] for c in chunks)
        with contextlib.ExitStack() as sto:
            kb.barrier()
            idxT = kb.sb(sto, "idxT", [128, NCH, NE], I32)
            gateT = kb.sb(sto, "gateT", [128, NCH, NE])
            with contextlib.ExitStack() as st:
                kb.barrier()
                affT = kb.sb(st, "affT", [NE, T])
                mx = kb.sb(st, "rmx", [128, 1])
                sm = kb.sb(st, "rsm", [128, 1])
                ex = kb.sb(st, "rex", [128, NE])
                pa = kb.ps(st, "rpa", [NE, 128])

                def post(n, t, Y):
                    kb.op("dve", lambda e: e.tensor_reduce(mx[:], Y[:, 0:NE], axis=AX.X, op=ALU.max), R=[Y], W=[mx])
                    kb.op("dve", lambda e: e.tensor_scalar(mx[:], mx[:], -1.0, None, op0=ALU.mult), R=[mx], W=[mx])
                    kb.op("act", lambda e: e.activation(ex[:], Y[:, 0:NE], AF.Exp, bias=mx[:, 0:1], scale=1.0, accum_out=sm[:]), R=[Y, mx], W=[ex, sm])
                    kb.op("dve", lambda e: e.reciprocal(sm[:], sm[:]), R=[sm], W=[sm])
                    kb.op("dve", lambda e: e.tensor_scalar(ex[:], ex[:], sm[:, 0:1], None, op0=ALU.mult), R=[ex, sm], W=[ex])
                    kb.op("pe", lambda e: e.transpose(pa[:], ex[:], self.ident[:]), R=[ex, self.ident], W=[pa])
                    kb.op("dve", lambda e: e.tensor_copy(affT[:, t * 128:(t + 1) * 128], pa[:]), R=[pa], P=[affT])

                with contextlib.ExitStack() as st2:
                    kb.barrier()
                    self.phase_linear(st2, d["H"], i["router_w"], i["router_w"][l], NE, tiles, post)
                kb.barrier()
                vals = kb.sb(st, "tvals", [NE, NSL])
                idxf = kb.sb(st, "tidxf", [NE, NSL])
                idxu = kb.sb(st, "tidxu", [NE, 8], U32)
                kb.op("dve", lambda e: e.memset(vals[:], 0.0), W=[vals])
                kb.op("dve", lambda e: e.memset(idxf[:], 0.0), W=[idxf])
                for (isc, s0, cnt), ci in zip(chunks, range(NCH)):
                    lo, hi = (S, T) if isc else (0, S)
                    for it in range(cnt // 8):
                        col = ci * 128 + it * 8
                        work = affT[:, lo:hi]
                        kb.op("dve", lambda e: e.max(out=vals[:, col:col + 8], in_=work), R=[affT], P=[vals])
                        kb.op("dve", lambda e: e.max_index(out=idxu[:], in_max=vals[:, col:col + 8], in_values=work), R=[affT, vals], W=[idxu])
                        kb.op("dve", lambda e: e.tensor_copy(idxf[:, col:col + 8], idxu[:]), R=[idxu], P=[idxf])
                        kb.op("dve", lambda e: e.match_replace(out=work, in_to_replace=vals[:, col:col + 8], in_values=work, imm_value=-1.0),
                              R=[vals], W=[affT])
                    if isc:
                        kb.op("dve", lambda e: e.tensor_scalar(idxf[:, ci * 128:ci * 128 + cnt], idxf[:, ci * 128:ci * 128 + cnt], float(S), None, op0=ALU.add),
                              R=[idxf], W=[idxf])
                padf = kb.sb(st, "padf", [128, 1])
                kb.dma("sp", padf, padf[:], i["k_pad"], i["k_pad"][:], part=False)
                idxTf = kb.sb(st, "idxTf", [128, NCH, NE])
                kb.op("dve", lambda e: e.tensor_copy(idxTf[:].rearrange("p a b -> p (a b)"), padf[:, 0:1].to_broadcast([128, NCH * NE])), R=[padf], W=[idxTf])
                kb.op("dve", lambda e: e.memset(gateT[:], 0.0), W=[gateT])
                pt = kb.ps(st, "tpt", [128, 2 * NE])
                for (isc, s0, cnt), ci in zip(chunks, range(NCH)):
                    kb.op("pe", lambda e: e.transpose(pt[:, 0:NE], idxf[:, ci * 128:(ci + 1) * 128], self.ident[0:NE, 0:NE]), R=[idxf, self.ident], P=[pt])
                    kb.op("pe", lambda e: e.transpose(pt[:, NE:2 * NE], vals[:, ci * 128:(ci + 1) * 128], self.ident[0:NE, 0:NE]), R=[vals, self.ident], P=[pt])
                    kb.op("dve", lambda e: e.tensor_copy(idxTf[0:cnt, ci, :], pt[0:cnt, 0:NE]), R=[pt], W=[idxTf])
                    kb.op("dve", lambda e: e.tensor_copy(gateT[0:cnt, ci, :], pt[0:cnt, NE:2 * NE]), R=[pt], W=[gateT])
                kb.op("dve", lambda e: e.tensor_copy(idxT[:], idxTf[:]), R=[idxTf], W=[idxT])
            if self.cfg.get('dbg', 99) == 5:
                return
            with contextlib.ExitStack() as st:
                kb.barrier()
                NF = FF // 128
                FB = min(256, FF)
                NFB = FF // FB
                gm = {}
                for row in ((0,) if last else (0, 1)):
                    g = kb.sb(st, f"mg{row}", [128, D])
                    self.load_bc(g, d["MODV"], self.mod_row(l, row, 5))
                    gm[row] = g
                xg = [kb.sb(st, f"xg{k}", [128, D]) for k in range(2)]
                xsT = kb.sb(st, "xsT", [128, 8, NSC])
                hT = kb.sb(st, "hT", [128, NF, NSC])
                w1 = [kb.sb(st, f"w1_{k}", [128, 8, FB]) for k in range(2)]
                w3 = [kb.sb(st, f"w3_{k}", [128, 8, FB]) for k in range(2)]
                w2 = [kb.sb(st, f"w2_{k}", [128, NF, 256]) for k in range(2)]
                sg = kb.sb(st, "sg", [128, 512])
                yo = [kb.sb(st, f"yo{k}", [128, D]) for k in range(NCH)]
                xr = [kb.sb(st, f"xr{k}", [128, D]) for k in range(2)]
                ptr = [kb.ps(st, f"mpt{k}", [128, 512]) for k in range(2)]
                p1 = [kb.ps(st, f"mp1{k}", [128, 512]) for k in range(2)]
                p3 = [kb.ps(st, f"mp3{k}", [128, 512]) for k in range(2)]
                py = [kb.ps(st, f"mpy{k}", [128, 512]) for k in range(2)]
                nw = 0
                nw2 = 0
                ng = 0
                npp = 0
                cgs = [(c0, min(512, NSC - c0)) for c0 in range(0, NSC, 512)]
                for Yo in yo:
                    kb.op("pool", lambda e: e.memset(Yo[:], 0.0), W=[Yo])
                for ex in range(NE):
                    for ci in range(NCH):
                        G = xg[ng % 2]
                        ng += 1
                        kb.op("pool", lambda e: e.indirect_dma_start(out=G[:, :], out_offset=None, in_=d["H"][:, :],
                                                                      in_offset=bass.IndirectOffsetOnAxis(ap=idxT[:, ci, ex:ex + 1], axis=0)),
                              R=[d["H"], idxT], W=[G], dma=True)
                        for k in range(8):
                            p = ptr[k // 4]
                            kb.op("pe", lambda e: e.transpose(p[:, (k % 4) * 128:(k % 4 + 1) * 128], G[:, k * 128:(k + 1) * 128], self.ident[:]),
                                  R=[G, self.ident], P=[p])
                        for jj in range(2):
                            if jj == 0:
                                kb.op("act", lambda e: e.activation(xsT[:, 0:4, offs[ci]:offs[ci] + chunks[ci][2]], ptr[0][:].rearrange("p (a b) -> p a b", a=4)[:, :, 0:chunks[ci][2]], AF.Copy),
                                      R=[ptr[0]], P=[xsT])
                            else:
                                kb.op("dve", lambda e: e.tensor_copy(xsT[:, 4:8, offs[ci]:offs[ci] + chunks[ci][2]], ptr[1][:].rearrange("p (a b) -> p a b", a=4)[:, :, 0:chunks[ci][2]]),
                                      R=[ptr[1]], P=[xsT])
                    for fb in range(NFB):
                        W1, W3 = w1[nw % 2], w3[nw % 2]
                        nw += 1
                        kb.dma("sp", W1, W1[:], i["ffn_w1"], i["ffn_w1"][l, ex, :, fb * FB:(fb + 1) * FB].rearrange("(k p) f -> p k f", p=128), part=False)
                        kb.dma("sp", W3, W3[:], i["ffn_w3"], i["ffn_w3"][l, ex, :, fb * FB:(fb + 1) * FB].rearrange("(k p) f -> p k f", p=128), part=False)
                        for fc in range(FB // 128):
                            f = fb * (FB // 128) + fc
                            for (c0, cw) in cgs:
                                P1, P3 = p1[npp % 2], p3[npp % 2]
                                npp += 1
                                for k in range(8):
                                    kb.op("pe", lambda e: e.matmul(P1[:, 0:cw], W1[:, k, fc * 128:(fc + 1) * 128], xsT[:, k, c0:c0 + cw], start=(k == 0), stop=(k == 7)),
                                          R=[W1, xsT], P=[P1])
                                for k in range(8):
                                    kb.op("pe", lambda e: e.matmul(P3[:, 0:cw], W3[:, k, fc * 128:(fc + 1) * 128], xsT[:, k, c0:c0 + cw], start=(k == 0), stop=(k == 7)),
                                          R=[W3, xsT], P=[P3])
                                kb.op("act", lambda e: e.activation(sg[:, 0:cw], P1[:, 0:cw], AF.Silu), R=[P1], W=[sg])
                                kb.op("dve", lambda e: e.tensor_tensor(hT[:, f, c0:c0 + cw], sg[:, 0:cw], P3[:, 0:cw], op=ALU.mult), R=[sg, P3], P=[hT])
                    for db in range(4):
                        W2 = w2[nw2 % 2]
                        nw2 += 1
                        kb.dma("sp", W2, W2[:], i["ffn_w2"], i["ffn_w2"][l, ex, :, db * 256:(db + 1) * 256].rearrange("(f p) n -> p f n", p=128), part=False)
                        for ci in range(NCH):
                            PY = py[(db * NCH + ci) % 2]
                            Yo = yo[ci]
                            cn = chunks[ci][2]
                            for f in range(NF):
                                kb.op("pe", lambda e: e.matmul(PY[0:cn, 0:256], hT[:, f, offs[ci]:offs[ci] + cn], W2[:, f, :], start=(f == 0), stop=(f == NF - 1)),
                                      R=[hT, W2], P=[PY])
                            g = gm[chunks[ci][0]]
                            kb.op("dve", lambda e: e.scalar_tensor_tensor(Yo[0:cn, db * 256:(db + 1) * 256], PY[0:cn, 0:256], gateT[0:cn, ci, ex:ex + 1], g[0:cn, db * 256:(db + 1) * 256],
                                                                          op0=ALU.mult, op1=ALU.mult), R=[PY, gateT, g], P=[Yo])
                            if db == 3:
                                Xr = xr[ci % 2]
                                kb.op("pool", lambda e: e.indirect_dma_start(out=Xr[:, :], out_offset=None, in_=d["X"][:, :],
                                                                              in_offset=bass.IndirectOffsetOnAxis(ap=idxT[:, ci, ex:ex + 1], axis=0)),
                                      R=[d["X"], idxT], W=[Xr], dma=True)
                                kb.op("dve", lambda e: e.tensor_tensor(Xr[:], Xr[:], Yo[:], op=ALU.add), R=[Xr, Yo], W=[Xr])
                                kb.op("pool", lambda e: e.indirect_dma_start(out=d["X"][:, :], out_offset=bass.IndirectOffsetOnAxis(ap=idxT[:, ci, ex:ex + 1], axis=0),
                                                                              in_=Xr[:, :], in_offset=None),
                                      R=[Xr, idxT], W=[d["X"]], dma=True)

    def build(self):
        kb = self.kb
        self.declare()
        with contextlib.ExitStack() as gst:
            self.gst = gst
            self.phase_init()
            cnt = {0: 0, 1: 0, 2: 0}
            dbg = self.cfg.get("dbg", 99)
            for l, kind in enumerate(self.kinds):
                last = l == self.L - 1
                j = cnt[kind]
                cnt[kind] += 1
                if dbg >= 1:
                    self.phase_norm(l, 0, 1, list(range(self.NT)), self.d["H"])
                if kind == 0:
                    if dbg >= 2:
                        self.phase_qkv_A(l, j)
                    if dbg >= 3:
                        self.phase_attn_A(last)
                    if dbg >= 4:
                        self.phase_out_proj(l, self.i["a_w_o"], self.i["a_w_o"][j], last)
                elif kind == 1:
                    if dbg >= 2:
                        self.phase_qkv_B(l, j)
                    if dbg >= 3:
                        self.phase_attn_B(j, last)
                    if dbg >= 4:
                        self.phase_out_proj(l, self.i["b_w_o"], self.i["b_w_o"][j], last)
                else:
                    if dbg >= 2:
                        self.phase_rwkv(l, j, last, dbg)
                if dbg >= 5:
                    self.phase_moe(l, last)
            self.phase_norm(self.L - 1, 0, 0, list(range(self.NTL)), self.out, final=True)
            kb.finish([self.out])
        return kb.nc


def _constants(cfg):
    S, C = cfg["S"], cfg["C"]
    T = S + C
    k = {}
    k["k_ident"] = np.eye(128, dtype=np.float32)
    rows = np.repeat(np.arange(S // 64), 64).astype(np.float32)
    cols = np.tile(np.arange(64), S // 64).astype(np.float32)

    def tab(hd):
        nf = hd // 4
        inv = (np.float32(10000.0) ** (-np.arange(nf, dtype=np.float32) / np.float32(nf))).astype(np.float32)
        ang = np.concatenate([rows[:, None] * inv, cols[:, None] * inv], axis=-1).astype(np.float32)
        return np.concatenate([np.cos(ang), np.sin(ang)], axis=-1).astype(np.float32)

    k["k_ropeA"] = tab(128)
    k["k_ropeB"] = tab(64)
    jj, ii = np.meshgrid(np.arange(128), np.arange(128), indexing="ij")
    k["k_tri"] = np.stack([(jj >= ii), (jj <= ii)]).astype(np.float32)
    bd = np.zeros((48, 8, 64), np.float32)
    for r in range(48):
        bd[r, r % 8, :] = 1.0
    k["k_bd"] = bd.reshape(48, 512)
    sel = np.zeros((2, 64, 64, 2, 8), np.float32)
    for hg in range(2):
        for t in range(64):
            sel[hg, t, t, hg, :] = 1.0
    k["k_sel"] = sel.reshape(128, 64, 16)
    k["k_pad"] = (T + np.arange(128, dtype=np.float32)).reshape(128, 1)
    k["k_zero"] = np.zeros((128, D), np.float32)
    return k


_NC_CACHE = {}


def kernel(**inputs):
    cfg = CFG
    n = cfg["ncores"]
    key = repr(cfg)
    if key not in _NC_CACHE:
        _NC_CACHE[key] = Prog(cfg).build()
    nc = _NC_CACHE[key]
    consts = _constants(cfg)
    f = lambda a: np.ascontiguousarray(np.asarray(a, dtype=np.float32))
    shared = {}
    for name, a in inputs.items():
        if name in ("x", "c", "ctx"):
            continue
        a = f(a)
        if name in ("c_ctx", "final_norm"):
            a = a.reshape(1, D)
        if name == "c_r_k":
            a = a.reshape(a.shape[0], 2, D)
        shared[name] = a
    shared.update(consts)
    x, c, ctx = f(inputs["x"]), f(inputs["c"]), f(inputs["ctx"])
    in_maps = []
    for b in range(n):
        m = dict(shared)
        m["x"] = x[b]
        m["c"] = c[b:b + 1]
        m["ctx"] = ctx[b]
        in_maps.append(m)
    res = run_bass_kernel_spmd(nc, in_maps, core_ids=list(range(n)))
    return np.stack([np.asarray(r["out"]) for r in res.results], axis=0).astype(np.float32)
```
